# Optimizing a Trainium2 kernel written in Bass

```python
import jax, jax.numpy as jnp
from jax import lax
import numpy as np

D_MODEL = 1024
BATCH = 2
SEQ = 8192
DEPTH = 1

D_MIX = D_MODEL
ATT_HEADS = 8
ATT_HEAD_DIM = 64
ATT_WIDTH = ATT_HEADS * ATT_HEAD_DIM
DILATED_PATTERNS = ((128, 1), (512, 4), (2048, 16))
Q_BLOCK = 128
ROPE_THETA = 10000.0
GLA_HEADS = 4
GLA_KEY_DIM = 64
GLA_VAL_DIM = 128
GLA_KEY_WIDTH = GLA_HEADS * GLA_KEY_DIM
GLA_VAL_WIDTH = GLA_HEADS * GLA_VAL_DIM
GLA_GATE_RANK = 16
GLA_GATE_NORMALIZER = 16.0
GLA_CHUNK = 64
IN_SPLITS = (ATT_WIDTH, ATT_WIDTH, ATT_WIDTH, GLA_KEY_WIDTH, GLA_KEY_WIDTH,
             GLA_VAL_WIDTH, GLA_VAL_WIDTH, GLA_GATE_RANK)
D_IN = sum(IN_SPLITS)
N_EXPERTS = 32
TOP_K = 4
D_FF = D_MODEL
SWIGLU_LIMIT = 7.0
SWIGLU_ALPHA = 1.702
MOE_BLOCK = 128
NORM_EPS = 1e-6
N_MOD = 6

kernel_name = "hybrid_dilated_gla_moe_block"


def rms_norm(x, g):
    xf = x.astype(jnp.float32)
    y = xf * lax.rsqrt(jnp.mean(xf * xf, axis=-1, keepdims=True) + NORM_EPS)
    return (y * g.astype(jnp.float32)).astype(x.dtype)


def rope(x, positions):
    half = x.shape[-1] // 2
    inv_freq = ROPE_THETA ** (-jnp.arange(half, dtype=jnp.float32) / half)
    ang = positions.astype(jnp.float32)[:, None] * inv_freq[None, :]
    cos, sin = jnp.cos(ang), jnp.sin(ang)
    xf = x.astype(jnp.float32)
    x1, x2 = xf[..., :half], xf[..., half:]
    return jnp.concatenate([x1 * cos - x2 * sin, x2 * cos + x1 * sin], axis=-1).astype(x.dtype)


def dilated_attention(q, k, v):
    B, H, S, hd = q.shape
    n_blk = S // Q_BLOCK

    def block(i):
        q0 = i * Q_BLOCK
        qb = lax.dynamic_slice_in_dim(q, q0, Q_BLOCK, axis=2).astype(jnp.float32)
        t = q0 + jnp.arange(Q_BLOCK)
        outs, lses = [], []
        for window, dil in DILATED_PATTERNS:
            offs = jnp.arange(window // dil + 1) * dil
            idx = t[:, None] - offs[None, :]
            valid = idx >= 0
            idx = jnp.maximum(idx, 0)
            kg = jnp.take(k, idx, axis=2).astype(jnp.float32)
            vg = jnp.take(v, idx, axis=2).astype(jnp.float32)
            s = jnp.einsum('bhqd,bhqjd->bhqj', qb, kg)
            s = jnp.where(valid, s, -jnp.inf)
            m = jnp.max(s, axis=-1, keepdims=True)
            p = jnp.exp(s - m)
            l = jnp.sum(p, axis=-1, keepdims=True)
            outs.append(jnp.einsum('bhqj,bhqjd->bhqd', p, vg) / l)
            lses.append(m + jnp.log(l))
        w = jax.nn.softmax(jnp.stack(lses, axis=0), axis=0)
        return jnp.sum(w * jnp.stack(outs, axis=0), axis=0)

    o = lax.map(block, jnp.arange(n_blk))
    o = o.transpose(1, 0, 3, 2, 4).reshape(B, S, H * hd)
    return o.astype(q.dtype)


def gated_linear_attention(q, k, v, log_a):
    B, H, S, dk = q.shape
    dv = v.shape[-1]
    C = GLA_CHUNK
    N = S // C
    qf = q.astype(jnp.float32).reshape(B, H, N, C, dk) * (dk ** -0.5)
    kf = k.astype(jnp.float32).reshape(B, H, N, C, dk)
    vf = v.astype(jnp.float32).reshape(B, H, N, C, dv)
    b = jnp.cumsum(log_a.reshape(B, H, N, C, dk), axis=3)
    b_last = b[:, :, :, -1:, :]
    q_dec = qf * jnp.exp(b)
    att = jnp.einsum('bhncd,bhnjd->bhncj', q_dec, kf * jnp.exp(-b))
    causal = jnp.tril(jnp.ones((C, C), dtype=bool))
    att = jnp.where(causal, att, 0.0)
    o_intra = jnp.einsum('bhncj,bhnje->bhnce', att, vf)
    u = jnp.einsum('bhncd,bhnce->nbhde', kf * jnp.exp(b_last - b), vf)
    a = jnp.exp(b_last[:, :, :, 0, :]).transpose(2, 0, 1, 3)

    def step(state, inp):
        a_n, u_n = inp
        return a_n[..., None] * state + u_n, state

    _, s_prev = lax.scan(step, jnp.zeros((B, H, dk, dv), jnp.float32), (a, u))
    o_inter = jnp.einsum('bhncd,nbhde->bhnce', q_dec, s_prev)
    return (o_intra + o_inter).reshape(B, H, S, dv)


def hybrid_mixer(h, w_in, w_gate_lr, b_gate, g_gla, w_out):
    B, S, _ = h.shape
    proj = h @ w_in
    split_points = [int(s) for s in np.cumsum(IN_SPLITS)[:-1]]
    q_a, k_a, v_a, q_g, k_g, v_g, r_g, g_lr = jnp.split(proj, split_points, axis=-1)

    def heads(t, n):
        return t.reshape(B, S, n, -1).transpose(0, 2, 1, 3)

    pos = jnp.arange(S)
    qa = rope(heads(q_a, ATT_HEADS), pos) * (ATT_HEAD_DIM ** -0.5)
    ka = rope(heads(k_a, ATT_HEADS), pos)
    o_att = dilated_attention(qa, ka, heads(v_a, ATT_HEADS))
    gate_logit = (g_lr @ w_gate_lr + b_gate).astype(jnp.float32)
    log_a = jax.nn.log_sigmoid(gate_logit) / GLA_GATE_NORMALIZER
    o_g = gated_linear_attention(heads(q_g, GLA_HEADS), heads(k_g, GLA_HEADS),
                                 heads(v_g, GLA_HEADS), heads(log_a, GLA_HEADS))
    o_g = rms_norm(o_g, g_gla)
    o_g = o_g.transpose(0, 2, 1, 3).reshape(B, S, GLA_VAL_WIDTH).astype(h.dtype) * jax.nn.silu(r_g)
    return jnp.concatenate([o_att, o_g], axis=-1) @ w_out


def moe_ffn(h, router_w, router_b, w_gate_up, b_gate_up, w_down, b_down):
    B, S, D = h.shape
    T = B * S
    xt = h.reshape(T, D)
    logits = (xt @ router_w + router_b).astype(jnp.float32)
    top_v, top_i = lax.top_k(logits, TOP_K)
    gates = jax.nn.softmax(top_v, axis=-1)
    A = T * TOP_K
    e_flat = top_i.reshape(A)
    tok_flat = jnp.arange(A, dtype=jnp.int32) // TOP_K
    g_flat = gates.reshape(A)
    order = jnp.argsort(e_flat)
    e_sorted = e_flat[order]
    counts = jnp.bincount(e_flat, length=N_EXPERTS)
    start = jnp.cumsum(counts) - counts
    padded = (counts + MOE_BLOCK - 1) // MOE_BLOCK * MOE_BLOCK
    pend = jnp.cumsum(padded)
    pstart = pend - padded
    dest = pstart[e_sorted] + jnp.arange(A, dtype=jnp.int32) - start[e_sorted]
    n_blocks = -(-(A + N_EXPERTS * (MOE_BLOCK - 1)) // MOE_BLOCK)
    P = n_blocks * MOE_BLOCK
    tok_buf = jnp.full((P,), T, dtype=jnp.int32).at[dest].set(tok_flat[order])
    gate_buf = jnp.zeros((P,), jnp.float32).at[dest].set(g_flat[order])
    blk_e = jnp.minimum(jnp.searchsorted(pend, jnp.arange(n_blocks) * MOE_BLOCK, side='right'),
                        N_EXPERTS - 1)
    xpad = jnp.concatenate([xt, jnp.zeros((1, D), xt.dtype)], axis=0)

    def expert_block(args):
        idx, e = args
        xb = xpad[idx]
        gu = xb @ w_gate_up[e] + b_gate_up[e]
        x_glu = jnp.minimum(gu[:, ::2], SWIGLU_LIMIT)
        x_lin = jnp.clip(gu[:, 1::2], -SWIGLU_LIMIT, SWIGLU_LIMIT)
        act = x_glu * jax.nn.sigmoid(SWIGLU_ALPHA * x_glu) * (x_lin + 1.0)
        return act @ w_down[e] + b_down[e]

    out_buf = lax.map(expert_block, (tok_buf.reshape(n_blocks, MOE_BLOCK), blk_e))
    out_buf = out_buf.reshape(P, D) * gate_buf[:, None].astype(out_buf.dtype)
    out = jnp.zeros((T + 1, D), out_buf.dtype).at[tok_buf].add(out_buf)
    return out[:T].reshape(B, S, D)


def setup_inputs(seed: int = 0) -> dict:
    key = jax.random.key(seed)
    ks = jax.random.split(key, 20)
    L = DEPTH

    def nrm(k, shape, scale):
        return jax.random.normal(k, shape, jnp.float32) * scale

    return {
        "x": nrm(ks[0], (BATCH, SEQ, D_MODEL), 1.0),
        "c": nrm(ks[1], (BATCH, D_MODEL), 1.0),
        "w_mod": nrm(ks[2], (L, D_MODEL, N_MOD * D_MODEL), 0.5 * D_MODEL ** -0.5),
        "b_mod": nrm(ks[3], (L, N_MOD * D_MODEL), 0.02),
        "g_pre_mix": 1.0 + nrm(ks[4], (L, D_MODEL), 0.05),
        "w_in": nrm(ks[5], (L, D_MODEL, D_IN), D_MODEL ** -0.5),
        "w_gate_lr": nrm(ks[6], (L, GLA_GATE_RANK, GLA_KEY_WIDTH), GLA_GATE_RANK ** -0.5),
        "b_gate": nrm(ks[7], (L, GLA_KEY_WIDTH), 0.1),
        "g_gla": 1.0 + nrm(ks[8], (L, GLA_VAL_DIM), 0.05),
        "w_out": nrm(ks[9], (L, D_MIX, D_MODEL), D_MIX ** -0.5),
        "g_post_mix": 1.0 + nrm(ks[10], (L, D_MODEL), 0.05),
        "g_pre_ffn": 1.0 + nrm(ks[11], (L, D_MODEL), 0.05),
        "router_w": nrm(ks[12], (L, D_MODEL, N_EXPERTS), D_MODEL ** -0.5),
        "router_b": nrm(ks[13], (L, N_EXPERTS), 0.01),
        "w_gate_up": nrm(ks[14], (L, N_EXPERTS, D_MODEL, 2 * D_FF), D_MODEL ** -0.5),
        "b_gate_up": nrm(ks[15], (L, N_EXPERTS, 2 * D_FF), 0.02),
        "w_down": nrm(ks[16], (L, N_EXPERTS, D_FF, D_MODEL), D_FF ** -0.5),
        "b_down": nrm(ks[17], (L, N_EXPERTS, D_MODEL), 0.02),
        "g_post_ffn": 1.0 + nrm(ks[18], (L, D_MODEL), 0.05),
    }


def reference(x, c, w_mod, b_mod, g_pre_mix, w_in, w_gate_lr, b_gate, g_gla, w_out,
              g_post_mix, g_pre_ffn, router_w, router_b, w_gate_up, b_gate_up,
              w_down, b_down, g_post_ffn):
    for l in range(DEPTH):
        mod = jax.nn.silu(c) @ w_mod[l] + b_mod[l]
        shift1, scale1, gate1, shift2, scale2, gate2 = jnp.split(mod[:, None, :], N_MOD, axis=-1)
        h = rms_norm(x, g_pre_mix[l]) * (1.0 + scale1) + shift1
        y = hybrid_mixer(h, w_in[l], w_gate_lr[l], b_gate[l], g_gla[l], w_out[l])
        x = x + gate1 * rms_norm(y, g_post_mix[l])
        h = rms_norm(x, g_pre_ffn[l]) * (1.0 + scale2) + shift2
        y = moe_ffn(h, router_w[l], router_b[l], w_gate_up[l], b_gate_up[l], w_down[l], b_down[l])
        x = x + gate2 * rms_norm(y, g_post_ffn[l])
    return x
```

```python
import numpy as np
import ml_dtypes
from contextlib import ExitStack
import concourse.bass as bass
import concourse.mybir as mybir
from concourse.bass_utils import run_bass_kernel_spmd

F32 = mybir.dt.float32
BF16 = mybir.dt.bfloat16
ALU = mybir.AluOpType
AF = mybir.ActivationFunctionType

ENGS = ("pe", "act", "dve", "pool", "sp")
EPS = 1e-6
NCORES = 8


class Buf:
    __slots__ = ("name", "w", "r", "war", "excl")

    def __init__(self, name):
        self.name = name
        self.w = {}
        self.r = {}
        self.war = []
        self.excl = False


class Op:
    __slots__ = ("eng", "fn", "dma", "pos", "deps", "flag", "cnt", "semkey", "cum")

    def __init__(self, eng, fn, dma, semkey):
        self.eng = eng
        self.fn = fn
        self.dma = dma
        self.semkey = semkey
        self.deps = []
        self.flag = False
        self.cnt = 0
        self.cum = 0
        self.pos = 0


class Sched:
    def __init__(self):
        self.ops = []
        self.streams = {e: [] for e in ENGS}
        self.dma_cum = {}
        self.last_dma = {}
        self.pending_bar = {}
        self.dead = False

    def _key(self, op):
        return ("dma", op.semkey) if op.dma else op.eng

    def add(self, eng, fn, reads=(), writes=(), dma=False, semkey=None, partial=False):
        if self.dead:
            return None
        op = Op(eng, fn, dma, semkey)
        op.pos = len(self.streams[eng])
        self.streams[eng].append(op)
        self.ops.append(op)
        if dma:
            c = self.dma_cum.get(semkey, 0) + 16
            self.dma_cum[semkey] = c
            op.cum = c
            self.last_dma[semkey] = op
        deps = []
        pb = self.pending_bar.pop(eng, None)
        if pb:
            deps.extend(pb)
        for b in reads:
            deps.extend(b.w.values())
            if b.excl:
                deps.extend(o for o in b.r.values() if o.eng != eng)
        for b in writes:
            if b.r:
                b.war = list(b.r.values()) + list(b.w.values())
                deps.extend(b.war)
            elif partial:
                deps.extend(b.war)
            else:
                deps.extend(b.w.values())
        op.deps = deps
        k = self._key(op)
        for b in reads:
            b.r[k] = op
        for b in writes:
            if b.r or not partial:
                if not b.r:
                    b.war = []
                b.w = {}
                b.r = {}
            b.w[k] = op
        return op

    def barrier(self):
        self.marks = getattr(self, "marks", [])
        self.marks.append({e: len(self.streams[e]) for e in ENGS})
        deps = []
        for e in ENGS:
            if self.streams[e]:
                last = None
                for o in reversed(self.streams[e]):
                    if not o.dma:
                        last = o
                        break
                if last is not None:
                    deps.append(last)
        deps.extend(self.last_dma.values())
        for e in ENGS:
            self.pending_bar[e] = list(deps)

    def finalize(self):
        self.waits = {}
        seen = {e: {} for e in ENGS}
        for op in self.ops:
            E = op.eng
            sv = seen[E]
            need = {}
            for d in op.deps:
                if d is op:
                    continue
                if d.dma:
                    key = ("dma", d.semkey)
                    val = d.cum
                else:
                    if d.eng == E and E in ("pe", "sp"):
                        continue
                    key = d.eng
                    val = d.pos + 1
                if sv.get(key, 0) >= val:
                    continue
                if need.get(key, (0, None))[0] < val:
                    need[key] = (val, d)
            wl = []
            for key, (val, d) in need.items():
                sv[key] = val
                if not d.dma:
                    d.flag = True
                wl.append(d)
            self.waits[id(op)] = wl
        for e in ENGS:
            c = 0
            for op in self.streams[e]:
                if not op.dma and op.flag:
                    c += 1
                    op.cnt = c

    def emit(self, block, sems, dsems):
        def run(ename, e):
            for op in self.streams[ename]:
                for d in self.waits[id(op)]:
                    if d.dma:
                        e.wait_ge(dsems[d.semkey], d.cum)
                    else:
                        e.wait_ge(sems[d.eng], d.cnt)
                ins = op.fn(e)
                if op.dma:
                    ins.then_inc(dsems[op.semkey], 16)
                elif op.flag:
                    ins.then_inc(sems[ename], 1)

        @block.sync
        def _(e):
            run("sp", e)

        @block.scalar
        def _(e):
            run("act", e)

        @block.vector
        def _(e):
            run("dve", e)

        @block.gpsimd
        def _(e):
            run("pool", e)

        @block.tensor
        def _(e):
            run("pe", e)


class TT:
    def __init__(self, t, name):
        self.t = t
        self.b = Buf(name)
        self.name = name


class Arena:
    def __init__(self, big, nbytes):
        self.big = big
        self.n = nbytes
        self.live = []
        self.peak = 0

    def alloc(self, name, shape, dt):
        esz = 4 if dt == F32 else 2
        fb = esz
        for d in shape[1:]:
            fb *= d
        size = (fb + 63) // 64 * 64
        off = 0
        for (o, sz, _) in sorted(self.live):
            if off + size <= o:
                break
            off = max(off, o + sz)
        assert off + size <= self.n, "SBUF arena overflow allocating %s (%d B); live=%d" % (
            name, size, sum(x[1] for x in self.live))
        self.live.append((off, size, name))
        self.peak = max(self.peak, off + size)
        ap = self.big[0:shape[0], off // 2:(off + fb) // 2]
        if dt == F32:
            ap = ap.bitcast(F32)
        if len(shape) == 3:
            ap = ap.rearrange("p (a b) -> p a b", a=shape[1])
        elif len(shape) == 4:
            ap = ap.rearrange("p (a b c) -> p a b c", a=shape[1], b=shape[2])
        t = TT(ap, name)
        t.off = off
        return t

    def free(self, tts):
        offs = set(t.off for t in tts)
        self.live = [x for x in self.live if x[0] not in offs]


class Ring:
    def __init__(self, items):
        self.items = items
        self.i = 0

    def next(self):
        it = self.items[self.i % len(self.items)]
        self.i += 1
        return it


def _consts():
    idx = np.arange(128)
    U = (idx[:, None] <= idx[None, :]).astype(np.float32)
    UT = U.T.copy()
    ident = np.eye(128, dtype=np.float32)
    Ls = (idx[:, None] > idx[None, :]).astype(np.float32)
    negdiag = (-0.125 * ident).astype(np.float32)
    ones = np.ones((128, 128), np.float32)
    sw = (idx // 64) * 64 + ((idx % 64) + 32) % 64
    Psw = np.zeros((128, 128), np.float32)
    Psw[sw, idx] = 1.0
    cf = np.concatenate([ident, U, Ls, negdiag, ones, Psw], axis=1)
    mask4 = np.concatenate([U, UT, U, UT], axis=1)
    negsel = np.zeros((128, 128), np.float32)
    negsel[0:64, 0] = -0.125
    negsel[64:128, 1] = -0.125
    cb = np.concatenate([ident, ones, mask4, negsel], axis=1).astype(ml_dtypes.bfloat16)
    return cf, cb


def _rope_tables(end):
    pos = (end - 4096 + np.arange(4096)).astype(np.float32)
    half = 32
    inv = (np.float32(10000.0) ** (-(np.arange(half, dtype=np.float32) / np.float32(half)))).astype(np.float32)
    ang = pos[:, None] * inv[None, :]
    c = np.cos(ang).astype(np.float32).T
    s = np.sin(ang).astype(np.float32).T
    cos_t = np.tile(c, (4, 1))
    sgn = np.where((np.arange(128) % 64) < 32, -1.0, 1.0).astype(np.float32)
    sin_t = np.tile(s, (4, 1)) * sgn[:, None]
    return np.stack([cos_t, sin_t], axis=0).astype(np.float32)


SBUF_BYTES = 206 * 1024


def build(stage="full"):
    nc = bass.Bass("TRN2", target_bir_lowering=False)
    S = Sched()

    def din(name, shape, dt=F32):
        return nc.dram_tensor(name, list(shape), dt, kind="ExternalInput").ap()

    xe = din("xe", [8192, 1024])
    tv_d = din("tilevalid", [128, 64])
    cvec = din("cvec", [128, 8])
    cs_tab = din("cs_tab", [2, 128, 4096])
    cf_d = din("cst_f32", [128, 768])
    cb_d = din("cst_bf", [128, 896], BF16)
    w_mod = din("w_mod", [1024, 6144])
    b_mod = din("b_mod", [6144])
    g_pre_mix = din("g_pre_mix", [1024])
    g_post_mix = din("g_post_mix", [1024])
    g_pre_ffn = din("g_pre_ffn", [1024])
    g_post_ffn = din("g_post_ffn", [1024])
    w_in = din("w_in", [1024, 3088])
    w_gate_lr = din("w_gate_lr", [16, 256])
    b_gate = din("b_gate", [256])
    g_gla = din("g_gla", [128])
    w_out = din("w_out", [1024, 1024])
    if stage == "full":
        router_w = din("router_w", [1024, 32])
        router_b = din("router_b", [32])
        w_gate_up = din("w_gate_up", [32, 1024, 2048])
        b_gate_up = din("b_gate_up", [32, 2048])
        w_down = din("w_down", [32, 1024, 1024])
        b_down = din("b_down", [32, 1024])
    out = nc.dram_tensor("out", [2048, 1024], F32, kind="ExternalOutput").ap()
    vscr = nc.dram_tensor("vscr", [4096, 512], BF16, kind="Internal").ap()
    ogscr = nc.dram_tensor("ogscr", [4, 128, 2048], BF16, kind="Internal").ap()
    modscr = nc.dram_tensor("modscr", [4, 128, 1024], F32, kind="Internal").ap()
    B_vscr = Buf("vscr")
    B_ogscr = Buf("ogscr")
    B_modscr = Buf("modscr")
    B_out = [Buf("out%d" % i) for i in range(16)]

    es = ExitStack()
    big = es.enter_context(nc.sbuf_tensor("big", [128, SBUF_BYTES // 2], BF16))
    A = Arena(big, SBUF_BYTES)

    def sb(scope, name, shape, dt):
        t = A.alloc(name, list(shape), dt)
        if scope is not None:
            scope.append(t)
        return t

    def ring(scope, name, shape, dt, n):
        return Ring([sb(scope, "%s%d" % (name, i), shape, dt) for i in range(n)])

    def dma(q, out_ap, in_ap, reads, writes, key, partial=False):
        S.add(q, lambda e: e.dma_start(out=out_ap, in_=in_ap), reads=reads, writes=writes,
              dma=True, semkey=key, partial=partial)

    def mm(out_ap, lhsT, rhs, start, stop, reads, writes):
        S.add("pe", lambda e: e.matmul(out_ap, lhsT=lhsT, rhs=rhs, start=start, stop=stop),
              reads=reads, writes=writes)

    def tr(out_ap, in_ap, ident_ap, reads, writes):
        S.add("pe", lambda e: e.transpose(out_ap, in_ap, ident_ap), reads=reads, writes=writes)

    def act(out_ap, in_ap, func, reads, writes, bias=None, scale=None, accum=None):
        kw = {}
        if bias is not None:
            kw["bias"] = bias
        if scale is not None:
            kw["scale"] = scale
        if accum is not None:
            kw["accum_out"] = accum
        S.add("act", lambda e: e.activation(out=out_ap, in_=in_ap, func=func, **kw), reads=reads, writes=writes)

    def tt(eng, out_ap, a, b, op, reads, writes):
        S.add(eng, lambda e: e.tensor_tensor(out=out_ap, in0=a, in1=b, op=op), reads=reads, writes=writes)

    def ts(eng, out_ap, a, s1, s2, op0, op1, reads, writes):
        if op1 is None:
            S.add(eng, lambda e: e.tensor_scalar(out=out_ap, in0=a, scalar1=s1, scalar2=None, op0=op0),
                  reads=reads, writes=writes)
        else:
            S.add(eng, lambda e: e.tensor_scalar(out=out_ap, in0=a, scalar1=s1, scalar2=s2, op0=op0, op1=op1),
                  reads=reads, writes=writes)

    def stt(eng, out_ap, a, sc, b, op0, op1, reads, writes):
        S.add(eng, lambda e: e.scalar_tensor_tensor(out=out_ap, in0=a, scalar=sc, in1=b, op0=op0, op1=op1),
              reads=reads, writes=writes)

    def cp(eng, out_ap, in_ap, reads, writes):
        if eng == "act":
            S.add("act", lambda e: e.copy(out=out_ap, in_=in_ap), reads=reads, writes=writes)
        else:
            S.add(eng, lambda e: e.tensor_copy(out=out_ap, in_=in_ap), reads=reads, writes=writes)

    def recip(out_ap, in_ap, reads, writes):
        S.add("dve", lambda e: e.reciprocal(out=out_ap, in_=in_ap), reads=reads, writes=writes)

    def ttr(out_ap, a, b, accum, reads, writes):
        S.add("dve", lambda e: e.tensor_tensor_reduce(out=out_ap, in0=a, in1=b, scale=1.0, scalar=0.0,
                                                      op0=ALU.mult, op1=ALU.add, accum_out=accum),
              reads=reads, writes=writes)

    def rstd_(ap_out, ap_in, inv_n, bufs):
        act(ap_out, ap_in, AF.Ln, bufs + [epsc.b], bufs, bias=epsc.t[:, 0:1], scale=inv_n)
        act(ap_out, ap_out, AF.Exp, bufs, bufs, scale=-0.5)

    def memset(eng, ap, val, writes):
        S.add(eng, lambda e: e.memset(ap, val), writes=writes)

    psb = []
    for i in range(8):
        t = es.enter_context(nc.psum_tensor("psb%d" % i, [128, 512], F32))
        psb.append(TT(t, "psb%d" % i))
        psb[-1].b.excl = True
    PS = Ring(psb)

    def bfview(p):
        return p.t[:, :].bitcast(BF16)

    cf = sb(None, "cf", [128, 768], F32)
    cbt = sb(None, "cb", [128, 896], BF16)
    epsc = sb(None, "epsc", [128, 1], F32)
    memset("pool", epsc.t[:, :], EPS, [epsc.b])
    c14 = sb(None, "c14", [128, 1], F32)
    memset("pool", c14.t[:, :], 14.0, [c14.b])
    tv = sb(None, "tv", [128, 64], F32)
    dma("sp", cf.t[:, :], cf_d, [], [cf.b], "cf")
    dma("sp", cbt.t[:, :], cb_d, [], [cbt.b], "cb")
    dma("sp", tv.t[:, :], tv_d, [], [tv.b], "tv")
    ident_f = cf.t[:, 0:128]
    U_f = cf.t[:, 128:256]
    Ls_f = cf.t[:, 256:384]
    negdiag = cf.t[:, 384:512]
    ones_f = cf.t[:, 512:640]
    Psw_f = cf.t[:, 640:768]
    ident_b = cbt.t[:, 0:128]
    ones_b = cbt.t[:, 128:256]
    mask4 = cbt.t[:, 256:768]
    negsel_b = cbt.t[:, 768:770]
    mT = sb(None, "mT", [128, 512], BF16)
    cp("pool", mT.t[:, 0:384], cbt.t[:, 384:768], [cbt.b], [mT.b])
    cp("pool", mT.t[:, 384:512], cbt.t[:, 256:384], [cbt.b, mT.b], [mT.b])
    mask4h = sb(None, "mask4h", [128, 512], BF16)
    cp("pool", mask4h.t[:, :], mT.t[:, :], [mT.b], [mask4h.b])
    hv = tv.t[:, 32:33]
    for off in (0, 256):
        ts("dve", mask4h.t[:, off:off + 128], mask4h.t[:, off:off + 128], hv, None, ALU.mult, None,
           [mask4h.b, tv.b], [mask4h.b])

    mix = []
    qT = sb(mix, "qT", [128, 4, 2048], BF16)
    kT = sb(mix, "kT", [128, 4, 4096], BF16)

    p1 = []
    gmod1 = sb(p1, "gmod1", [128, 1024], F32)
    shift1 = sb(p1, "shift1", [128, 1024], F32)
    p0 = []
    modb = sb(p0, "modb", [128, 6144], F32)
    bmodb = sb(p0, "bmodb", [128, 6144], F32)
    gb = [sb(p0, "gb%d" % i, [128, 1024], F32) for i in range(4)]
    cv = sb(p0, "cv", [128, 8], F32)
    ecv = sb(p0, "ecv", [128, 8], F32)
    sc = sb(p0, "sc", [128, 8], F32)
    scb = sb(p0, "scb", [128, 8, 128], BF16)
    wm = ring(p0, "wm", [128, 8, 512], BF16, 2)
    tmp = ring(p0, "mtmp", [128, 1024], F32, 3)
    dma("sp", cv.t[:, :], cvec, [], [cv.b], "cv")
    dma("sp", bmodb.t[:, :], b_mod.partition_broadcast(128), [], [bmodb.b], "bmodb")
    for i, g in enumerate((g_pre_mix, g_post_mix, g_pre_ffn, g_post_ffn)):
        dma("sp", gb[i].t[:, :], g.partition_broadcast(128), [], [gb[i].b], "gb%d" % i)
    act(ecv.t[:, :], cv.t[:, :], AF.Exp, [cv.b], [ecv.b], scale=-1.0)
    ts("dve", ecv.t[:, :], ecv.t[:, :], 1.0, None, ALU.add, None, [ecv.b], [ecv.b])
    recip(ecv.t[:, :], ecv.t[:, :], [ecv.b], [ecv.b])
    tt("dve", sc.t[:, :], cv.t[:, :], ecv.t[:, :], ALU.mult, [cv.b, ecv.b], [sc.b])
    cp("dve", scb.t[:, :, :], sc.t[:, :].unsqueeze(2).to_broadcast([128, 8, 128]), [sc.b], [scb.b])
    wmv = w_mod.rearrange("(kc p) n -> p kc n", p=128)
    for blk in range(12):
        w = wm.next()
        dma("pool", w.t[:, :, :], wmv[:, :, blk * 512:(blk + 1) * 512], [], [w.b], w.name)
        ps = PS.next()
        for kc in range(8):
            mm(ps.t[:, :], scb.t[:, kc, :], w.t[:, kc, :], kc == 0, kc == 7, [scb.b, w.b], [ps.b])
        tt("dve", modb.t[:, blk * 512:(blk + 1) * 512], ps.t[:, :], bmodb.t[:, blk * 512:(blk + 1) * 512],
           ALU.add, [ps.b, bmodb.b], [modb.b])
    sl = lambda i: modb.t[:, i * 1024:(i + 1) * 1024]
    cp("pool", shift1.t[:, :], sl(0), [modb.b], [shift1.b])
    stt("dve", gmod1.t[:, :], sl(1), 1.0, gb[0].t[:, :], ALU.add, ALU.mult, [modb.b, gb[0].b], [gmod1.b])
    t0 = tmp.next()
    tt("dve", t0.t[:, :], sl(2), gb[1].t[:, :], ALU.mult, [modb.b, gb[1].b], [t0.b])
    dma("sp", modscr[0], t0.t[:, :], [t0.b], [B_modscr], "modscr", partial=True)
    dma("sp", modscr[1], sl(3), [modb.b], [B_modscr], "modscr", partial=True)
    t1 = tmp.next()
    stt("dve", t1.t[:, :], sl(4), 1.0, gb[2].t[:, :], ALU.add, ALU.mult, [modb.b, gb[2].b], [t1.b])
    dma("sp", modscr[2], t1.t[:, :], [t1.b], [B_modscr], "modscr", partial=True)
    t2 = tmp.next()
    tt("dve", t2.t[:, :], sl(5), gb[3].t[:, :], ALU.mult, [modb.b, gb[3].b], [t2.b])
    dma("sp", modscr[3], t2.t[:, :], [t2.b], [B_modscr], "modscr", partial=True)
    S.barrier()
    if stage == "p0":
        dma("sp", out[0:128, :], gmod1.t[:, :], [gmod1.b], [B_out[0]], "dbg0")
        dma("sp", out[128:256, :], shift1.t[:, :], [shift1.b], [B_out[1]], "dbg1")
        S.dead = True
    A.free(p0)

    w_in_sb = sb(p1, "w_in", [128, 8, 3088], BF16)
    winv = w_in.rearrange("(kc p) n -> p kc n", p=128)
    for kc in range(8):
        dma("pool", w_in_sb.t[:, kc, :], winv[:, kc, :], [], [w_in_sb.b], "w_in", partial=True)
    wg17 = sb(p1, "wg17", [32, 256], F32)
    memset("pool", wg17.t[:, :], 0.0, [wg17.b])
    dma("sp", wg17.t[0:16, :], w_gate_lr, [], [wg17.b], "wg17")
    dma("sp", wg17.t[16:17, :], b_gate.rearrange("(o n) -> o n", o=1), [], [wg17.b], "wg17b")
    gglab = sb(p1, "gglab", [128, 128], F32)
    dma("sp", gglab.t[:, :], g_gla.partition_broadcast(128), [], [gglab.b], "gglab")

    Sf = sb(p1, "Sf", [128, 2, 128], F32)
    Sb = sb(p1, "Sb", [128, 2, 128], BF16)
    memset("pool", Sf.t[:, :, :], 0.0, [Sf.b])
    memset("pool", Sb.t[:, :, :], 0.0, [Sb.b])

    rn = {}
    rn["x_ring"] = ring(p1, "xt", [128, 1024], F32, 2)
    rn["junk"] = sb(p1, "junk_a", [128, 1024], BF16)
    rn["ss_ring"] = ring(p1, "ss", [128, 2], F32, 4)
    rn["hb_ring"] = ring(p1, "hb", [128, 1024], BF16, 2)
    hT_ring = ring(p1, "hT", [128, 8, 512], BF16, 2)
    cs_ring = ring(p1, "csr", [128, 2, 512], F32, 1)
    qs_ring = ring(p1, "qsb", [128, 512], F32, 2)
    rp_ring = ring(p1, "rp", [128, 512], F32, 2)
    vt_ring = ring(p1, "vt", [128, 512], BF16, 2)
    glr_ring = ring(p1, "glr", [32, 512], F32, 2)
    for g in glr_ring.items:
        memset("pool", g.t[:, :], 1.0, [g.b])
    e1_ring = ring(p1, "e1", [128, 256], F32, 2)
    sp_ring = ring(p1, "spr", [128, 256], F32, 2)
    dec_ring = ring(p1, "dec", [128, 256], F32, 2)
    ku_ring = ring(p1, "ku", [128, 256], BF16, 2)
    vg_ring = ring(p1, "vg", [128, 512], BF16, 2)
    acol_ring = ring(p1, "acol", [128, 2], F32, 2)
    eb_ring = ring(p1, "eb", [128, 256], F32, 2)
    enb_ring = ring(p1, "enb", [128, 256], F32, 2)
    qd_ring = ring(p1, "qd", [128, 2, 128], BF16, 2)
    kd_ring = ring(p1, "kd", [128, 2, 128], BF16, 2)
    attm_ring = ring(p1, "attm", [128, 4, 128], BF16, 2)
    er_ring = ring(p1, "er", [128, 512], F32, 1)
    rg_ring = ring(p1, "rg", [128, 512], F32, 2)
    ssg_ring = ring(p1, "ssg", [128, 4], F32, 2)
    on_ring = ring(p1, "on", [128, 512], F32, 1)
    og_ring = ring(p1, "og", [128, 512], BF16, 2)
    ogT_ring = ring(p1, "ogT", [128, 4, 128], BF16, 2)
    junk_s = sb(p1, "junk_s", [128, 128], BF16)

    def proj_fm(col0, ncols, hTb, tsl, ps, pcols):
        for kc in range(8):
            mm(ps.t[0:ncols, pcols], w_in_sb.t[:, kc, col0:col0 + ncols], hTb.t[:, kc, tsl], kc == 0, kc == 7,
               [w_in_sb.b, hTb.b], [ps.b])

    def proj_tm(ti, col0, ncols, hTb, ps):
        for kc in range(8):
            mm(ps.t[:, 0:ncols], hTb.t[:, kc, ti * 128:(ti + 1) * 128], w_in_sb.t[:, kc, col0:col0 + ncols],
               kc == 0, kc == 7, [w_in_sb.b, hTb.b], [ps.b])

    def rmsnorm_mod_T(xt, gm, sh, dstT, dst_cols, defer=False):
        ss = rn["ss_ring"].next()
        junk = rn["junk"]
        memset("pool", ss.t[:, :], 0.0, [ss.b])
        act(junk.t[:, :], xt.t[:, :], AF.Square, [xt.b], [junk.b, ss.b], accum=ss.t[:, 0:1])
        rstd_(ss.t[:, 1:2], ss.t[:, 0:1], 1.0 / 1024.0, [ss.b])
        stt("dve", xt.t[:, :], xt.t[:, :], ss.t[:, 1:2], gm.t[:, :], ALU.mult, ALU.mult, [xt.b, ss.b, gm.b], [xt.b])
        hb = rn["hb_ring"].next()
        tt("dve", hb.t[:, :], xt.t[:, :], sh.t[:, :], ALU.add, [xt.b, sh.b], [hb.b])
        if defer:
            return hb
        rmsnorm_T2(hb, dstT, dst_cols)

    def rmsnorm_T2(hb, dstT, dst_cols):
        ps = PS.next()
        pv = bfview(ps)
        for kc in range(8):
            tr(pv[:, kc * 128:(kc + 1) * 128], hb.t[:, kc * 128:(kc + 1) * 128], ident_b, [hb.b, cbt.b], [ps.b])
        cp("act", dstT.t[:, :, dst_cols], pv.rearrange("p (a b) -> p a b", a=8), [ps.b], [dstT.b])

    def cut(name):
        if stage == name and not S.dead:
            S.barrier()
            dma("sp", out[0:128, 0:768], cf.t[:, :], [cf.b], [B_out[0]], "dbgc")
            S.dead = True

    pending = [None]

    def back(ctx):
        acol, ku, vg = ctx["acol"], ctx["ku"], ctx["vg"]
        pu = PU.next()
        for h in range(4):
            g, hh = h // 2, h % 2
            mm(pu.t[hh * 64:(hh + 1) * 64, g * 128:(g + 1) * 128], ku.t[:, h * 64:(h + 1) * 64],
               vg.t[:, h * 128:(h + 1) * 128], True, True, [ku.b, vg.b], [pu.b])
        if ctx["kind"] == "own":
            attm, qd, rg, m0, ti = ctx["attm"], ctx["qd"], ctx["rg"], ctx["m0"], ctx["ti"]
            po = PS.next()
            for h in range(4):
                g, hh = h // 2, h % 2
                mm(po.t[:, h * 128:(h + 1) * 128], attm.t[:, h, :], vg.t[:, h * 128:(h + 1) * 128],
                   True, False, [attm.b, vg.b], [po.b])
                mm(po.t[:, h * 128:(h + 1) * 128], qd.t[hh * 64:(hh + 1) * 64, g, :],
                   Sb.t[hh * 64:(hh + 1) * 64, g, :], False, True, [qd.b, Sb.b], [po.b])
            ssg = ssg_ring.next()
            memset("pool", ssg.t[:, :], 0.0, [ssg.b])
            for h in range(4):
                act(junk_s.t[:, :], po.t[:, h * 128:(h + 1) * 128], AF.Square, [po.b], [junk_s.b, ssg.b],
                    accum=ssg.t[:, h:h + 1])
            rstd_(ssg.t[:, :], ssg.t[:, :], 1.0 / 128.0, [ssg.b])
            on = on_ring.next()
            tt("dve", on.t[:, :].rearrange("p (a b) -> p a b", a=4), po.t[:, :].rearrange("p (a b) -> p a b", a=4),
               ssg.t[:, :].unsqueeze(2).to_broadcast([128, 4, 128]), ALU.mult, [po.b, ssg.b], [on.b])
            og = og_ring.next()
            tt("pool", og.t[:, :], on.t[:, :], rg.t[:, :], ALU.mult, [on.b, rg.b], [og.b])
            pt2 = PS.next()
            pv2 = bfview(pt2)
            for h in range(4):
                tr(pv2[:, h * 128:(h + 1) * 128], og.t[:, h * 128:(h + 1) * 128], ident_b, [og.b, cbt.b], [pt2.b])
            ogT = ogT_ring.next()
            cp("act", ogT.t[:, :, :], pv2[:, 0:512].rearrange("p (a b) -> p a b", a=4), [pt2.b], [ogT.b])
            dma("sp", ogscr[:, :, m0 + ti * 128:m0 + (ti + 1) * 128].rearrange("h e t -> e h t"), ogT.t[:, :, :],
                [ogT.b], [B_ogscr], ogT.name, partial=True)
        for g in range(2):
            stt("dve", Sf.t[:, g, :], Sf.t[:, g, :], acol.t[:, g:g + 1], pu.t[:, g * 128:(g + 1) * 128],
                ALU.mult, ALU.add, [Sf.b, acol.b, pu.b], [Sf.b])
        cp("pool", Sb.t[:, :, :], Sf.t[:, :, :], [Sf.b], [Sb.b])

    hTbs = {}

    pro_pending = [None]

    def prologue_flush():
        if pro_pending[0] is not None:
            hb_, tb2, ti2 = pro_pending[0]
            rmsnorm_T2(hb_, hTbs[tb2], slice(ti2 * 128, (ti2 + 1) * 128))
            pro_pending[0] = None

    def prologue_tile(tb_, ti_, immediate=False):
        t_ = tb_ * 4 + ti_
        xt = rn["x_ring"].next()
        dma("sp", xt.t[:, :], xe[t_ * 128:(t_ + 1) * 128, :], [], [xt.b], xt.name)
        hb_ = rmsnorm_mod_T(xt, gmod1, shift1, None, None, defer=True)
        prologue_flush()
        pro_pending[0] = (hb_, tb_, ti_)
        if immediate:
            prologue_flush()

    full512 = slice(0, 512)
    PU = Ring(psb[6:8])
    PS.items = psb[0:6]
    for tb in range(16):
        kind = "prefix" if tb < 8 else ("halo" if tb < 12 else "own")
        S.bmarks = getattr(S, "bmarks", []) + [len(S.streams["pe"])]
        if tb == 0:
            hTbs[0] = hT_ring.next()
            for ti in range(4):
                prologue_tile(0, ti, immediate=(ti == 3))
        hTb = hTbs[tb]
        if tb + 1 < 16:
            hTbs[tb + 1] = hT_ring.next()
        cut("c_norm")
        n0 = (tb - 8) * 512
        m0 = (tb - 12) * 512
        if kind != "prefix":
            csr = cs_ring.next()
            dma("sp", csr.t[:, :, :], cs_tab[:, :, n0:n0 + 512].rearrange("a p n -> p a n"), [], [csr.b], csr.name)
            jobs = [(512, kT, n0)]
            if kind == "own":
                jobs.append((0, qT, m0))
            for (cbase, dst, d0) in jobs:
                for hp in range(4):
                    pa = PS.next()
                    proj_fm(cbase + hp * 128, 128, hTb, full512, pa, full512)
                    qs = qs_ring.next()
                    cp("act", qs.t[:, :], pa.t[:, :], [pa.b], [qs.b])
                    pb_ = PS.next()
                    mm(pb_.t[:, :], Psw_f, qs.t[:, :], True, True, [cf.b, qs.b], [pb_.b])
                    r1 = rp_ring.next()
                    r2 = rp_ring.next()
                    tt("pool", r1.t[:, :], qs.t[:, :], csr.t[:, 0, :], ALU.mult, [qs.b, csr.b], [r1.b])
                    tt("dve", r2.t[:, :], pb_.t[:, :], csr.t[:, 1, :], ALU.mult, [pb_.b, csr.b], [r2.b])
                    tt("pool", dst.t[:, hp, d0:d0 + 512], r1.t[:, :], r2.t[:, :], ALU.add, [r1.b, r2.b], [dst.b])
        if tb == 8:
            cut("c_rope")
        glr = glr_ring.next()
        pg = PS.next()
        proj_fm(3072, 16, hTb, full512, pg, full512)
        cp("act", glr.t[0:16, :], pg.t[0:16, :], [pg.b], [glr.b])
        for ti in range(4):
            t = tb * 4 + ti
            tsl = slice(ti * 128, (ti + 1) * 128)
            if kind != "prefix":
                pvv = PS.next()
                proj_tm(ti, 1024, 512, hTb, pvv)
                vt = vt_ring.next()
                cp("act", vt.t[:, :], pvv.t[:, :], [pvv.b], [vt.b])
                dma("sp", vscr[n0 + ti * 128:n0 + (ti + 1) * 128, :], vt.t[:, :], [vt.b], [B_vscr], vt.name,
                    partial=True)
            pgl = PS.next()
            mm(pgl.t[:, 0:256], glr.t[0:17, tsl], wg17.t[0:17, :], True, True, [glr.b, wg17.b], [pgl.b])
            e1 = e1_ring.next()
            act(e1.t[:, :], pgl.t[:, 0:256], AF.Exp, [pgl.b], [e1.b], scale=-1.0)
            spt = sp_ring.next()
            act(spt.t[:, :], e1.t[:, :], AF.Ln, [e1.b], [spt.b], bias=1.0)
            pk = PS.next()
            proj_tm(ti, 1792, 256, hTb, pk)
            pvg = PS.next()
            proj_tm(ti, 2048, 512, hTb, pvg)
            vg = vg_ring.next()
            cp("act", vg.t[:, :], pvg.t[:, :], [pvg.b], [vg.b])
            paf = PS.next()
            mm(paf.t[:, 0:256], Ls_f, spt.t[:, :], True, True, [cf.b, spt.b], [paf.b])
            dec = dec_ring.next()
            act(dec.t[:, :], paf.t[:, 0:256], AF.Exp, [paf.b], [dec.b], scale=-1.0 / 16.0)
            ku = ku_ring.next()
            stt("dve", ku.t[:, :], pk.t[:, 0:256], tv.t[:, t:t + 1], dec.t[:, :], ALU.mult, ALU.mult,
                [pk.b, tv.b, dec.b], [ku.b])
            pa_ = PS.next()
            for g in range(2):
                mm(pa_.t[:, g:g + 1], spt.t[:, g * 128:(g + 1) * 128], ones_f[:, 0:1], True, True,
                   [spt.b, cf.b], [pa_.b])
            acol = acol_ring.next()
            act(acol.t[:, :], pa_.t[:, 0:2], AF.Exp, [pa_.b], [acol.b], scale=-1.0 / 16.0)
            if kind == "own":
                pcs = PS.next()
                for g in range(2):
                    mm(pcs.t[:, g * 128:(g + 1) * 128], spt.t[:, g * 128:(g + 1) * 128], U_f, True, True,
                       [spt.b, cf.b], [pcs.b])
                eb = eb_ring.next()
                enb = enb_ring.next()
                act(eb.t[:, :], pcs.t[:, 0:256], AF.Exp, [pcs.b], [eb.b], scale=-1.0 / 16.0)
                act(enb.t[:, :], pcs.t[:, 0:256], AF.Exp, [pcs.b], [enb.b], scale=1.0 / 16.0)
                pqk = PS.next()
                for g in range(2):
                    proj_fm(1536 + g * 128, 128, hTb, tsl, pqk, slice(g * 128, (g + 1) * 128))
                    proj_fm(1792 + g * 128, 128, hTb, tsl, pqk, slice(256 + g * 128, 256 + (g + 1) * 128))
                qd = qd_ring.next()
                kd = kd_ring.next()
                stt("dve", qd.t[:, :, :], pqk.t[:, 0:256].rearrange("p (a b) -> p a b", a=2), 0.125,
                    eb.t[:, :].rearrange("p (a b) -> p a b", a=2), ALU.mult, ALU.mult, [pqk.b, eb.b], [qd.b])
                tt("dve", kd.t[:, :, :], pqk.t[:, 256:512].rearrange("p (a b) -> p a b", a=2),
                   enb.t[:, :].rearrange("p (a b) -> p a b", a=2), ALU.mult, [pqk.b, enb.b], [kd.b])
                pr = PS.next()
                proj_tm(ti, 2560, 512, hTb, pr)
                er = er_ring.next()
                act(er.t[:, :], pr.t[:, :], AF.Exp, [pr.b], [er.b], scale=-1.0)
                rg = rg_ring.next()
                tt("dve", rg.t[:, :].rearrange("p (a b) -> p a b", a=4), pr.t[:, :].rearrange("p (a b) -> p a b", a=4),
                   gglab.t[:, :].unsqueeze(1).to_broadcast([128, 4, 128]), ALU.mult, [pr.b, gglab.b], [rg.b])
                ts("dve", er.t[:, :], er.t[:, :], 1.0, None, ALU.add, None, [er.b], [er.b])
                recip(er.t[:, :], er.t[:, :], [er.b], [er.b])
                tt("pool", rg.t[:, :], rg.t[:, :], er.t[:, :], ALU.mult, [rg.b, er.b], [rg.b])
                patt = [PS.next(), PS.next()]
                for h in range(4):
                    g, hh = h // 2, h % 2
                    mm(patt[hh].t[:, g * 128:(g + 1) * 128], kd.t[hh * 64:(hh + 1) * 64, g, :],
                       qd.t[hh * 64:(hh + 1) * 64, g, :], True, True, [kd.b, qd.b], [patt[hh].b])
                attm = attm_ring.next()
                for hh in range(2):
                    tt("dve", attm.t[:, hh:4:2, :], patt[hh].t[:, 0:256].rearrange("p (a b) -> p a b", a=2),
                       U_f.unsqueeze(1).to_broadcast([128, 2, 128]), ALU.mult, [patt[hh].b, cf.b], [attm.b])
            ctx = dict(kind=kind, acol=acol, ku=ku, vg=vg)
            if kind == "own":
                ctx.update(attm=attm, qd=qd, rg=rg, m0=m0, ti=ti)
            if pending[0] is not None:
                back(pending[0])
            pending[0] = ctx
            if tb + 1 < 16:
                prologue_tile(tb + 1, ti, immediate=(ti == 3))
    back(pending[0])
    PS.items = psb
    S.barrier()
    if stage == "p1":
        dbgt = sb(None, "dbgt", [128, 1024], F32)
        for i in range(4):
            cp("dve", dbgt.t[:, :], kT.t[:, i, 2048:3072], [kT.b], [dbgt.b])
            dma("sp", out[i * 128:(i + 1) * 128, :], dbgt.t[:, :], [dbgt.b], [B_out[i]], "dbg0")
        for i in range(4):
            cp("dve", dbgt.t[:, :], qT.t[:, i, 0:1024], [qT.b], [dbgt.b])
            dma("sp", out[(4 + i) * 128:(5 + i) * 128, :], dbgt.t[:, :], [dbgt.b], [B_out[4 + i]], "dbg0")
        S.dead = True
    A.free(p1)

    p23 = []
    catT = sb(p23, "catT", [128, 8, 2048], BF16)
    w_out_sb = sb(p23, "w_out", [128, 8, 1024], BF16)
    woutv = w_out.rearrange("(kc p) n -> p kc n", p=128)
    for kc in range(8):
        dma("pool", w_out_sb.t[:, kc, :], woutv[:, kc, :], [], [w_out_sb.b], "w_out", partial=True)
    for h in range(4):
        dma("sp", catT.t[:, 4 + h, :], ogscr[h], [B_ogscr], [catT.b], "catT_ld", partial=True)
    pa = []
    vp_ring = ring(pa, "vp", [128, 3, 32, 128], BF16, 2)
    acc = sb(pa, "acc", [128, 2, 2048], F32)
    nb_ring = ring(pa, "nb", [128, 2], F32, 4)
    prod = sb(pa, "prod", [128, 2048], BF16)
    P_ring = ring(pa, "P", [128, 512], BF16, 4)
    PT_ring = ring(pa, "PT", [128, 512], BF16, 4)
    rl = sb(pa, "rl", [128, 2048], F32)
    PATS = ((0, 1), (1, 4), (2, 16))
    vps = {}

    def load_vp(hp_):
        vp_ = vp_ring.next()
        for (pi, d) in PATS:
            src = vscr[:, hp_ * 128:(hp_ + 1) * 128].rearrange("(u d) c -> d u c", d=d)
            ntile = 32 // d
            for r in range(d):
                dma("sp", vp_.t[:, pi, r * ntile:(r + 1) * ntile, :],
                    src[r].rearrange("(kt p) c -> p kt c", p=128), [B_vscr], [vp_.b], vp_.name, partial=True)
        vps[hp_] = vp_

    def mk_prod(hp_):
        tt("pool", prod.t[:, :], qT.t[:, hp_, :], kT.t[:, hp_, 2048:4096], ALU.mult, [qT.b, kT.b], [prod.b])

    load_vp(0)
    load_vp(1)
    mk_prod(0)
    for hp in range(4):
        vp = vps[hp]
        if 1 <= hp < 3:
            load_vp(hp + 1)
        memset("pool", acc.t[:, :, :], 0.0, [acc.b])
        units = []
        for (pi, d) in PATS:
            ntile = 32 // d
            for r in range(d):
                for kt in range(16 // d, ntile):
                    units.append((pi, d, r, kt))

        def stageA(u):
            pi, d, r, kt = u
            ntile = 32 // d
            st_ = {}
            q0 = r + d * kt * 128 - 2048
            k0 = r + d * (kt - 1) * 128
            qs = slice(q0, q0 + d * 127 + 1, d)
            ks = slice(k0, k0 + d * 255 + 1, d)
            pS = [PS.next(), PS.next()]
            for hh in range(2):
                mm(pS[hh].t[:, 0:256], qT.t[hh * 64:(hh + 1) * 64, hp, qs],
                   kT.t[hh * 64:(hh + 1) * 64, hp, ks], True, True, [qT.b, kT.b], [pS[hh].b])
            mm(pS[0].t[:, 256:258], prod.t[:, qs], negsel_b, True, True, [prod.b, cbt.b], [pS[0].b])
            nb = nb_ring.next()
            cp("dve", nb.t[:, 0:2], pS[0].t[:, 256:258], [pS[0].b], [nb.b])
            Pt = P_ring.next()
            for hh in range(2):
                act(Pt.t[:, hh * 256:(hh + 1) * 256], pS[hh].t[:, 0:256], AF.Exp,
                    [pS[hh].b, nb.b], [Pt.b], bias=nb.t[:, hh:hh + 1], scale=0.125)
            st_.update(Pt=Pt, qs=qs, vt0=r * ntile + kt - 1, pi=pi, first=(kt == 16 // d))
            return st_

        def stageB(st_):
            Pt = st_["Pt"]
            pT = PS.next()
            pTv = bfview(pT)
            for i in range(4):
                tr(pTv[:, i * 128:(i + 1) * 128], Pt.t[:, i * 128:(i + 1) * 128], ident_b,
                   [Pt.b, cbt.b], [pT.b])
            PT = PT_ring.next()
            mk = mask4h if st_["first"] else mT
            tt("dve", PT.t[:, :], pTv[:, 0:512], mk.t[:, :], ALU.mult, [pT.b, mk.b], [PT.b])
            st_["PT"] = PT

        def stageC(st_):
            PT, qs, vt0, pi = st_["PT"], st_["qs"], st_["vt0"], st_["pi"]
            pO = PS.next()
            for hh in range(2):
                osl = pO.t[hh * 64:(hh + 1) * 64, 0:128]
                lsl = pO.t[hh * 64:(hh + 1) * 64, 128:256]
                for kk in range(2):
                    mm(osl, vp.t[:, pi, vt0 + kk, hh * 64:(hh + 1) * 64],
                       PT.t[:, (hh * 2 + kk) * 128:(hh * 2 + kk + 1) * 128], kk == 0, kk == 1,
                       [vp.b, PT.b], [pO.b])
                for kk in range(2):
                    mm(lsl, ones_b[:, 0:64], PT.t[:, (hh * 2 + kk) * 128:(hh * 2 + kk + 1) * 128],
                       kk == 0, kk == 1, [cbt.b, PT.b], [pO.b])
            tt("dve", acc.t[:, :, qs], acc.t[:, :, qs], pO.t[:, 0:256].rearrange("p (a b) -> p a b", a=2),
               ALU.add, [acc.b, pO.b], [acc.b])

        sts = []
        nu = len(units)
        for it in range(nu + 2):
            if it < nu:
                sts.append(stageA(units[it]))
            if it == nu - 1 and hp + 1 < 4:
                mk_prod(hp + 1)
            if 0 <= it - 1 < nu:
                stageB(sts[it - 1])
            if 0 <= it - 2 < nu:
                stageC(sts[it - 2])
        recip(rl.t[:, :], acc.t[:, 1, :], [acc.b], [rl.b])
        tt("pool", catT.t[:, hp, :], acc.t[:, 0, :], rl.t[:, :], ALU.mult, [acc.b, rl.b], [catT.b])
    S.barrier()
    if stage == "p2":
        dbgt = sb(None, "dbgt", [128, 1024], F32)
        for i in range(8):
            cp("dve", dbgt.t[:, :], catT.t[:, i, 0:1024], [catT.b], [dbgt.b])
            dma("sp", out[i * 128:(i + 1) * 128, :], dbgt.t[:, :], [dbgt.b], [B_out[i]], "dbg0")
        S.dead = True
    A.free(pa)
    A.free(mix)

    p34 = []
    if stage == "full":
        h2T = sb(p34, "h2T", [128, 8, 2048], BF16)
        G = sb(p34, "G", [128, 16, 32], F32)
        GT = sb(p34, "GT", [32, 2048], BF16)
        wgu_ring = Ring([sb(p34, "wgu%d" % i, [128, 8, 1024], BF16) for i in range(2)])
        wd_ring = Ring([sb(p34, "wd0", [128, 8, 1024], BF16)])
        wguv = w_gate_up.rearrange("e (kc p) n -> e p kc n", p=128)
        wdv = w_down.rearrange("e (kc p) n -> e p kc n", p=128)

        def load_gu(e_, hx):
            wg = wgu_ring.next()
            for kc in range(0, 8, 2):
                dma("pool", wg.t[:, kc:kc + 2, :], wguv[e_, :, kc:kc + 2, hx * 1024:(hx + 1) * 1024], [], [wg.b],
                    wg.name, partial=True)
            return wg

        def load_d(e_):
            wd = wd_ring.next()
            for kc in range(0, 8, 2):
                dma("pool", wd.t[:, kc:kc + 2, :], wdv[e_, :, kc:kc + 2, :], [], [wd.b], wd.name, partial=True)
            return wd

        cur0 = [load_gu(0, 0), load_gu(0, 1), load_d(0)]
        rw = sb(p23, "rw", [128, 8, 32], BF16)
        rbb = sb(p23, "rbb", [128, 32], F32)
        dma("pool", rw.t[:, :, :], router_w.rearrange("(kc p) n -> p kc n", p=128), [], [rw.b], "rw")
        dma("sp", rbb.t[:, :], router_b.partition_broadcast(128), [], [rbb.b], "rbb")
        lg_ring = ring(p23, "lg", [128, 32], F32, 2)
        m8_ring = ring(p23, "m8", [128, 8], F32, 2)
        ex_ring = ring(p23, "ex", [128, 32], F32, 2)
        msk_ring = ring(p23, "msk", [128, 32], F32, 2)
        den_ring = ring(p23, "den", [128, 2], F32, 2)
        gtb_ring = ring(p23, "gtb", [128, 32], BF16, 2)
    mods = [sb(p23, "mods%d" % i, [128, 1024], F32) for i in range(3)]
    for i in range(3):
        dma("sp", mods[i].t[:, :], modscr[i], [B_modscr], [mods[i].b], "mods%d" % i)
    gg1, shift2, gmod2 = mods
    rn["x_ring"] = ring(p23, "xt3", [128, 1024], F32, 2)
    rn["junk"] = sb(p23, "junk_a3", [128, 1024], BF16)
    rn["ss_ring"] = ring(p23, "ss3", [128, 2], F32, 4)
    rn["hb_ring"] = ring(p23, "hb3", [128, 1024], BF16, 2)
    yt_ring = ring(p23, "yt", [128, 1024], F32, 2)
    x1_ring = ring(p23, "x1", [128, 1024], F32, 2)
    junk3 = rn["junk"]

    st3 = {}

    def p3A(ti):
        tsl = slice(ti * 128, (ti + 1) * 128)
        py = [PS.next(), PS.next()]
        for half in range(2):
            for kc in range(8):
                mm(py[half].t[:, :], catT.t[:, kc, tsl], w_out_sb.t[:, kc, half * 512:(half + 1) * 512],
                   kc == 0, kc == 7, [catT.b, w_out_sb.b], [py[half].b])
        ss = rn["ss_ring"].next()
        memset("pool", ss.t[:, :], 0.0, [ss.b])
        for half in range(2):
            act(junk3.t[:, 0:512], py[half].t[:, :], AF.Square, [py[half].b], [junk3.b, ss.b],
                accum=ss.t[:, half:half + 1])
        tt("dve", ss.t[:, 0:1], ss.t[:, 0:1], ss.t[:, 1:2], ALU.add, [ss.b], [ss.b])
        rstd_(ss.t[:, 0:1], ss.t[:, 0:1], 1.0 / 1024.0, [ss.b])
        yt = yt_ring.next()
        for half in range(2):
            hs = slice(half * 512, (half + 1) * 512)
            stt("dve", yt.t[:, hs], py[half].t[:, :], ss.t[:, 0:1], gg1.t[:, hs], ALU.mult, ALU.mult,
                [py[half].b, ss.b, gg1.b], [yt.b])
        xt = rn["x_ring"].next()
        dma("sp", xt.t[:, :], xe[(48 + ti) * 128:(49 + ti) * 128, :], [], [xt.b], xt.name)
        x1 = x1_ring.next()
        tt("pool", x1.t[:, :], xt.t[:, :], yt.t[:, :], ALU.add, [xt.b, yt.b], [x1.b])
        dma("sp", out[tsl, :], x1.t[:, :], [x1.b], [B_out[ti]], "out_w%d" % (ti % 4))
        st3[ti] = dict(x1=x1)

    def p3B1(ti):
        st3[ti]["hb"] = rmsnorm_mod_T(st3[ti]["x1"], gmod2, shift2, None, None, defer=True)

    def p3B2(ti):
        tsl = slice(ti * 128, (ti + 1) * 128)
        rmsnorm_T2(st3[ti]["hb"], h2T, tsl)
        pl = PS.next()
        for kc in range(8):
            mm(pl.t[:, 0:32], h2T.t[:, kc, tsl], rw.t[:, kc, :], kc == 0, kc == 7, [h2T.b, rw.b], [pl.b])
        lg = lg_ring.next()
        tt("dve", lg.t[:, :], pl.t[:, 0:32], rbb.t[:, :], ALU.add, [pl.b, rbb.b], [lg.b])
        m8 = m8_ring.next()
        S.add("dve", (lambda m8=m8, lg=lg: (lambda e: e.max(out=m8.t[:, :], in_=lg.t[:, :])))(),
              reads=[lg.b], writes=[m8.b])
        msk = msk_ring.next()
        ts("dve", msk.t[:, :], lg.t[:, :], m8.t[:, 3:4], None, ALU.is_ge, None, [lg.b, m8.b], [msk.b])
        den = den_ring.next()
        memset("pool", den.t[:, :], 0.0, [den.b])
        ts("dve", den.t[:, 0:1], m8.t[:, 0:1], -1.0, None, ALU.mult, None, [m8.b], [den.b])
        ex = ex_ring.next()
        act(ex.t[:, :], lg.t[:, :], AF.Exp, [lg.b, den.b], [ex.b], bias=den.t[:, 0:1], scale=1.0)
        tt("dve", ex.t[:, :], ex.t[:, :], msk.t[:, :], ALU.mult, [ex.b, msk.b], [ex.b])
        S.add("dve", (lambda ex=ex, den=den: (lambda e: e.reduce_sum(out=den.t[:, 1:2], in_=ex.t[:, :],
                                                                   axis=mybir.AxisListType.X)))(),
              reads=[ex.b], writes=[den.b])
        recip(den.t[:, 1:2], den.t[:, 1:2], [den.b], [den.b])
        ts("dve", G.t[:, ti, :], ex.t[:, :], den.t[:, 1:2], None, ALU.mult, None, [ex.b, den.b], [G.b])
        gtb = gtb_ring.next()
        cp("pool", gtb.t[:, :], G.t[:, ti, :], [G.b], [gtb.b])
        pgt = PS.next()
        pgv = bfview(pgt)
        tr(pgv[0:32, 0:128], gtb.t[:, :], ident_b, [gtb.b, cbt.b], [pgt.b])
        cp("act", GT.t[:, tsl], pgv[0:32, 0:128], [pgt.b], [GT.b])

    for it in range(16 + 2):
        if it < 16:
            p3A(it)
        if stage == "full":
            if 0 <= it - 1 < 16:
                p3B1(it - 1)
            if 0 <= it - 2 < 16:
                p3B2(it - 2)
    S.barrier()
    A.free(p23)

    if stage == "full":
        p4 = []
        gg2b = sb(p4, "gg2b", [128, 1024], F32)
        dma("sp", gg2b.t[:, :], modscr[3], [B_modscr], [gg2b.b], "gg2b")
        bdn = sb(p4, "bdn", [32, 1024], BF16)
        dma("pool", bdn.t[:, :], b_down, [], [bdn.b], "bdn")
        bgu = sb(p4, "bgu", [128, 16, 32], F32)
        pb = []
        bgs = sb(pb, "bgs", [32, 2048], F32)
        dma("sp", bgs.t[:, :], b_gate_up, [], [bgs.b], "bgs")
        bview = bgs.t[:, :].rearrange("e (fb p s) -> e fb s p", fb=8, s=2)
        for fb in range(8):
            for s_ in range(2):
                pbt = PS.next()
                tr(pbt.t[:, 0:32], bview[:, fb, s_, :], ident_f[0:32, 0:32], [bgs.b, cf.b], [pbt.b])
                cp("act", bgu.t[:, fb * 2 + s_, :], pbt.t[:, 0:32], [pbt.b], [bgu.b])
        bl7 = sb(p4, "bl7", [128, 8, 32], F32)
        ts("dve", bl7.t[:, :, :], bgu.t[:, 1:16:2, :], -1.0, 7.0, ALU.mult, ALU.add, [bgu.b], [bl7.b])
        S.barrier()
        A.free(pb)
        accm = sb(p4, "accm", [128, 8, 1024], F32)
        wgu_ring.items = wgu_ring.items + [sb(p4, "wgu2", [128, 8, 1024], BF16)]
        wd_ring.items = wd_ring.items + [sb(p4, "wd1", [128, 8, 1024], BF16)]
        actT_ring = ring(p4, "actT", [128, 8, 512], BF16, 2)
        g_ring = ring(p4, "g_", [128, 512], F32, 2)
        sg_ring = ring(p4, "sg_", [128, 512], F32, 2)
        l_ring = ring(p4, "l_", [128, 512], F32, 2)
        fin_ring = ring(p4, "fin", [128, 1024], F32, 1)
        x1b_ring = ring(p4, "x1b", [128, 1024], F32, 1)
        ss4_ring = ring(p4, "ss4", [128, 2], F32, 2)
        junk4 = sb(p4, "junk4", [128, 1024], BF16)
        seq = [(hf_, e_) for hf_ in range(2) for e_ in range(32)]
        NSK = 2

        def gu_fb(e_, tok0, fb, wgA, wgB, aT):
            wg = wgA if fb < 4 else wgB
            fl = fb % 4
            pG = PS.next()
            pL = PS.next()
            for kc in range(8):
                mm(pG.t[:, :], wg.t[:, kc, fl * 256:(fl + 1) * 256:2], h2T.t[:, kc, tok0:tok0 + 512],
                   kc == 0, kc == 7, [wg.b, h2T.b], [pG.b])
            for kc in range(8):
                mm(pL.t[:, :], wg.t[:, kc, fl * 256 + 1:(fl + 1) * 256:2], h2T.t[:, kc, tok0:tok0 + 512],
                   kc == 0, kc == 7, [wg.b, h2T.b], [pL.b])
            g_ = g_ring.next()
            ts("dve", g_.t[:, :], pG.t[:, :], bgu.t[:, fb * 2, e_:e_ + 1], 7.0, ALU.add, ALU.min,
               [pG.b, bgu.b], [g_.b])
            sg = sg_ring.next()
            act(sg.t[:, :], g_.t[:, :], AF.Sigmoid, [g_.b], [sg.b], scale=1.702)
            l_ = l_ring.next()
            act(l_.t[:, :], pL.t[:, :], AF.Relu, [pL.b, bl7.b], [l_.b], bias=bl7.t[:, fb, e_:e_ + 1], scale=-1.0)
            act(l_.t[:, :], l_.t[:, :], AF.Relu, [l_.b, c14.b], [l_.b], bias=c14.t[:, 0:1], scale=-1.0)
            tt("dve", g_.t[:, :], g_.t[:, :], sg.t[:, :], ALU.mult, [g_.b, sg.b], [g_.b])
            stt("dve", aT.t[:, fb, :], l_.t[:, :], -6.0, g_.t[:, :], ALU.add, ALU.mult, [g_.b, l_.b], [aT.b])

        def down(hf_, e_, tb, aT, wd):
            for tl in range(4):
                tg = hf_ * 8 + tb * 4 + tl
                ta = tb * 4 + tl
                py = [PS.next(), PS.next()]
                for half in range(2):
                    for fb in range(8):
                        mm(py[half].t[:, :], aT.t[:, fb, tl * 128:(tl + 1) * 128],
                           wd.t[:, fb, half * 512:(half + 1) * 512], fb == 0, fb == 7, [aT.b, wd.b], [py[half].b])
                for half in range(2):
                    hs = slice(half * 512, (half + 1) * 512)
                    if e_ == 0:
                        ts("dve", accm.t[:, ta, hs], py[half].t[:, :], G.t[:, tg, e_:e_ + 1], None, ALU.mult, None,
                           [py[half].b, G.b], [accm.b])
                    else:
                        stt("dve", accm.t[:, ta, hs], py[half].t[:, :], G.t[:, tg, e_:e_ + 1], accm.t[:, ta, hs],
                            ALU.mult, ALU.add, [py[half].b, G.b, accm.b], [accm.b])

        cur = cur0
        pre = None
        for si, (hf_, e_) in enumerate(seq):
            wgA, wgB, wd = cur
            have_next = si + 1 < len(seq)
            ne = seq[si + 1][1] if have_next else None
            nxtA = load_gu(ne, 0) if have_next else None
            nxtB = None
            nxtD = load_d(ne) if have_next else None
            for tb in range(2):
                tok0 = hf_ * 1024 + tb * 512
                if pre is None:
                    aT = actT_ring.next()
                    fb0 = 0
                else:
                    aT = pre
                    fb0 = NSK
                for fb in range(fb0, 8):
                    gu_fb(e_, tok0, fb, wgA, wgB, aT)
                if tb == 1 and have_next:
                    nxtB = load_gu(ne, 1)
                pre = None
                if tb == 0:
                    pre = actT_ring.next()
                    for fb in range(NSK):
                        gu_fb(e_, hf_ * 1024 + 512, fb, wgA, wgB, pre)
                elif have_next:
                    pre = actT_ring.next()
                    for fb in range(NSK):
                        gu_fb(ne, seq[si + 1][0] * 1024, fb, nxtA, None, pre)
                down(hf_, e_, tb, aT, wd)
            cur = [nxtA, nxtB, nxtD]
            if e_ == 31:
                for ta in range(8):
                    tg = hf_ * 8 + ta
                    tsl = slice(tg * 128, (tg + 1) * 128)
                    pb2 = [PS.next(), PS.next()]
                    ss = ss4_ring.next()
                    memset("pool", ss.t[:, :], 0.0, [ss.b])
                    for half in range(2):
                        hs = slice(half * 512, (half + 1) * 512)
                        mm(pb2[half].t[:, :], GT.t[:, tsl], bdn.t[:, hs], True, True, [GT.b, bdn.b], [pb2[half].b])
                        tt("dve", accm.t[:, ta, hs], accm.t[:, ta, hs], pb2[half].t[:, :], ALU.add,
                           [accm.b, pb2[half].b], [accm.b])
                    act(junk4.t[:, :], accm.t[:, ta, :], AF.Square, [accm.b], [junk4.b, ss.b], accum=ss.t[:, 0:1])
                    rstd_(ss.t[:, 0:1], ss.t[:, 0:1], 1.0 / 1024.0, [ss.b])
                    fin = fin_ring.next()
                    stt("dve", fin.t[:, :], accm.t[:, ta, :], ss.t[:, 0:1], gg2b.t[:, :], ALU.mult, ALU.mult,
                        [accm.b, ss.b, gg2b.b], [fin.b])
                    x1b = x1b_ring.next()
                    dma("sp", x1b.t[:, :], out[tsl, :], [B_out[tg]], [x1b.b], x1b.name)
                    tt("pool", fin.t[:, :], fin.t[:, :], x1b.t[:, :], ALU.add, [fin.b, x1b.b], [fin.b])
                    dma("sp", out[tsl, :], fin.t[:, :], [fin.b], [B_out[tg]], "out_f")
        A.free(p4)
    A.free(p34)

    S.dead = False
    S.add("sp", lambda e: e.nop(), reads=B_out)

    S.finalize()
    sems = {e: es.enter_context(nc.semaphore("s_" + e)) for e in ENGS}
    dsems = {}
    for i, k in enumerate(sorted(S.dma_cum.keys())):
        dsems[k] = es.enter_context(nc.semaphore("d%d" % i))
    block = es.enter_context(nc.Block())
    S.emit(block, sems, dsems)
    es.close()
    build.info = dict(bmarks=getattr(S, "bmarks", []), marks=[m["pe"] for m in S.marks], peak=A.peak, nops=len(S.ops), ndsem=len(dsems),
                      per_eng={e: len(S.streams[e]) for e in ENGS},
                      flagged={e: sum(1 for o in S.streams[e] if o.flag) for e in ENGS})
    return nc, S


def make_in_maps(inputs, stage="full"):
    x = np.asarray(inputs["x"], np.float32)
    c = np.asarray(inputs["c"], np.float32)
    cf, cb = _consts()
    shared = {
        "cst_f32": cf, "cst_bf": cb,
        "w_mod": np.ascontiguousarray(inputs["w_mod"][0], np.float32),
        "b_mod": np.ascontiguousarray(inputs["b_mod"][0], np.float32),
        "g_pre_mix": np.ascontiguousarray(inputs["g_pre_mix"][0], np.float32),
        "g_post_mix": np.ascontiguousarray(inputs["g_post_mix"][0], np.float32),
        "g_pre_ffn": np.ascontiguousarray(inputs["g_pre_ffn"][0], np.float32),
        "g_post_ffn": np.ascontiguousarray(inputs["g_post_ffn"][0], np.float32),
        "w_in": np.ascontiguousarray(inputs["w_in"][0], np.float32),
        "w_gate_lr": np.ascontiguousarray(inputs["w_gate_lr"][0], np.float32),
        "b_gate": np.ascontiguousarray(inputs["b_gate"][0], np.float32),
        "g_gla": np.ascontiguousarray(inputs["g_gla"][0], np.float32),
        "w_out": np.ascontiguousarray(inputs["w_out"][0], np.float32),
    }
    if stage == "full":
        shared.update({
            "router_w": np.ascontiguousarray(inputs["router_w"][0], np.float32),
            "router_b": np.ascontiguousarray(inputs["router_b"][0], np.float32),
            "w_gate_up": np.ascontiguousarray(inputs["w_gate_up"][0], np.float32),
            "b_gate_up": np.ascontiguousarray(inputs["b_gate_up"][0], np.float32),
            "w_down": np.ascontiguousarray(inputs["w_down"][0], np.float32),
            "b_down": np.ascontiguousarray(inputs["b_down"][0], np.float32),
        })
    maps = []
    for core in range(NCORES):
        b, j = core // 4, core % 4
        end = (j + 1) * 2048
        start = end - 8192
        xeh = np.zeros((8192, 1024), np.float32)
        lo = max(start, 0)
        xeh[lo - start:, :] = x[b, lo:end, :]
        tvh = np.zeros((128, 64), np.float32)
        for t in range(64):
            if start + t * 128 >= 0:
                tvh[:, t] = 1.0
        m = dict(shared)
        m["xe"] = xeh
        m["tilevalid"] = tvh
        m["cvec"] = np.ascontiguousarray(c[b].reshape(8, 128).T)
        m["cs_tab"] = _rope_tables(end)
        maps.append(m)
    return maps


_CACHE = {}


def kernel(**inputs):
    if "nc" not in _CACHE:
        _CACHE["nc"] = build("full")[0]
    nc = _CACHE["nc"]
    maps = make_in_maps(inputs, "full")
    res = run_bass_kernel_spmd(nc, maps, core_ids=list(range(NCORES)))
    outp = np.zeros((2, 8192, 1024), np.float32)
    for core in range(NCORES):
        b, j = core // 4, core % 4
        outp[b, j * 2048:(j + 1) * 2048, :] = np.asarray(res.results[core]["out"], np.float32)
    return outp
```

```python
import numpy as np
import ml_dtypes
from contextlib import ExitStack
import concourse.bass as bass
import concourse.mybir as mybir
from concourse.bass_utils import run_bass_kernel_spmd

F32 = mybir.dt.float32
BF16 = mybir.dt.bfloat16
ALU = mybir.AluOpType
AF = mybir.ActivationFunctionType

ENGS = ("pe", "act", "dve", "pool", "sp")
EPS = 1e-6
NCORES = 8


class Buf:
    __slots__ = ("name", "w", "r", "war", "excl")

    def __init__(self, name):
        self.name = name
        self.w = {}
        self.r = {}
        self.war = []
        self.excl = False


class Op:
    __slots__ = ("eng", "fn", "dma", "pos", "deps", "flag", "cnt", "semkey", "cum")

    def __init__(self, eng, fn, dma, semkey):
        self.eng = eng
        self.fn = fn
        self.dma = dma
        self.semkey = semkey
        self.deps = []
        self.flag = False
        self.cnt = 0
        self.cum = 0
        self.pos = 0


class Sched:
    def __init__(self):
        self.ops = []
        self.streams = {e: [] for e in ENGS}
        self.dma_cum = {}
        self.last_dma = {}
        self.pending_bar = {}
        self.dead = False

    def _key(self, op):
        return ("dma", op.semkey) if op.dma else op.eng

    def add(self, eng, fn, reads=(), writes=(), dma=False, semkey=None, partial=False):
        if self.dead:
            return None
        op = Op(eng, fn, dma, semkey)
        op.pos = len(self.streams[eng])
        self.streams[eng].append(op)
        self.ops.append(op)
        if dma:
            c = self.dma_cum.get(semkey, 0) + 16
            self.dma_cum[semkey] = c
            op.cum = c
            self.last_dma[semkey] = op
        deps = []
        pb = self.pending_bar.pop(eng, None)
        if pb:
            deps.extend(pb)
        for b in reads:
            deps.extend(b.w.values())
            if b.excl:
                deps.extend(o for o in b.r.values() if o.eng != eng)
        for b in writes:
            if b.r:
                b.war = list(b.r.values()) + list(b.w.values())
                deps.extend(b.war)
            elif partial:
                deps.extend(b.war)
            else:
                deps.extend(b.w.values())
        op.deps = deps
        k = self._key(op)
        for b in reads:
            b.r[k] = op
        for b in writes:
            if b.r or not partial:
                if not b.r:
                    b.war = []
                b.w = {}
                b.r = {}
            b.w[k] = op
        return op

    def barrier(self):
        self.marks = getattr(self, "marks", [])
        self.marks.append({e: len(self.streams[e]) for e in ENGS})
        deps = []
        for e in ENGS:
            if self.streams[e]:
                last = None
                for o in reversed(self.streams[e]):
                    if not o.dma:
                        last = o
                        break
                if last is not None:
                    deps.append(last)
        deps.extend(self.last_dma.values())
        for e in ENGS:
            self.pending_bar[e] = list(deps)

    def finalize(self):
        self.waits = {}
        seen = {e: {} for e in ENGS}
        for op in self.ops:
            E = op.eng
            sv = seen[E]
            need = {}
            for d in op.deps:
                if d is op:
                    continue
                if d.dma:
                    key = ("dma", d.semkey)
                    val = d.cum
                else:
                    if d.eng == E and E in ("pe", "sp"):
                        continue
                    key = d.eng
                    val = d.pos + 1
                if sv.get(key, 0) >= val:
                    continue
                if need.get(key, (0, None))[0] < val:
                    need[key] = (val, d)
            wl = []
            for key, (val, d) in need.items():
                sv[key] = val
                if not d.dma:
                    d.flag = True
                wl.append(d)
            self.waits[id(op)] = wl
        for e in ENGS:
            c = 0
            for op in self.streams[e]:
                if not op.dma and op.flag:
                    c += 1
                    op.cnt = c

    def emit(self, block, sems, dsems):
        def run(ename, e):
            for op in self.streams[ename]:
                for d in self.waits[id(op)]:
                    if d.dma:
                        e.wait_ge(dsems[d.semkey], d.cum)
                    else:
                        e.wait_ge(sems[d.eng], d.cnt)
                ins = op.fn(e)
                if op.dma:
                    ins.then_inc(dsems[op.semkey], 16)
                elif op.flag:
                    ins.then_inc(sems[ename], 1)

        @block.sync
        def _(e):
            run("sp", e)

        @block.scalar
        def _(e):
            run("act", e)

        @block.vector
        def _(e):
            run("dve", e)

        @block.gpsimd
        def _(e):
            run("pool", e)

        @block.tensor
        def _(e):
            run("pe", e)


class TT:
    def __init__(self, t, name):
        self.t = t
        self.b = Buf(name)
        self.name = name


class Arena:
    def __init__(self, big, nbytes):
        self.big = big
        self.n = nbytes
        self.live = []
        self.peak = 0

    def alloc(self, name, shape, dt):
        esz = 4 if dt == F32 else 2
        fb = esz
        for d in shape[1:]:
            fb *= d
        size = (fb + 63) // 64 * 64
        off = 0
        for (o, sz, _) in sorted(self.live):
            if off + size <= o:
                break
            off = max(off, o + sz)
        assert off + size <= self.n, "SBUF arena overflow allocating %s (%d B); live=%d" % (
            name, size, sum(x[1] for x in self.live))
        self.live.append((off, size, name))
        self.peak = max(self.peak, off + size)
        ap = self.big[0:shape[0], off // 2:(off + fb) // 2]
        if dt == F32:
            ap = ap.bitcast(F32)
        if len(shape) == 3:
            ap = ap.rearrange("p (a b) -> p a b", a=shape[1])
        elif len(shape) == 4:
            ap = ap.rearrange("p (a b c) -> p a b c", a=shape[1], b=shape[2])
        t = TT(ap, name)
        t.off = off
        return t

    def free(self, tts):
        offs = set(t.off for t in tts)
        self.live = [x for x in self.live if x[0] not in offs]


class Ring:
    def __init__(self, items):
        self.items = items
        self.i = 0

    def next(self):
        it = self.items[self.i % len(self.items)]
        self.i += 1
        return it


def _consts():
    idx = np.arange(128)
    U = (idx[:, None] <= idx[None, :]).astype(np.float32)
    UT = U.T.copy()
    ident = np.eye(128, dtype=np.float32)
    Ls = (idx[:, None] > idx[None, :]).astype(np.float32)
    negdiag = (-0.125 * ident).astype(np.float32)
    ones = np.ones((128, 128), np.float32)
    sw = (idx // 64) * 64 + ((idx % 64) + 32) % 64
    Psw = np.zeros((128, 128), np.float32)
    Psw[sw, idx] = 1.0
    cf = np.concatenate([ident, U, Ls, negdiag, ones, Psw], axis=1)
    mask4 = np.concatenate([U, UT, U, UT], axis=1)
    negsel = np.zeros((128, 128), np.float32)
    negsel[0:64, 0] = -0.125
    negsel[64:128, 1] = -0.125
    cb = np.concatenate([ident, ones, mask4, negsel], axis=1).astype(ml_dtypes.bfloat16)
    return cf, cb


def _rope_tables(end):
    pos = (end - 4096 + np.arange(4096)).astype(np.float32)
    half = 32
    inv = (np.float32(10000.0) ** (-(np.arange(half, dtype=np.float32) / np.float32(half)))).astype(np.float32)
    ang = pos[:, None] * inv[None, :]
    c = np.cos(ang).astype(np.float32).T
    s = np.sin(ang).astype(np.float32).T
    cos_t = np.tile(c, (4, 1))
    sgn = np.where((np.arange(128) % 64) < 32, -1.0, 1.0).astype(np.float32)
    sin_t = np.tile(s, (4, 1)) * sgn[:, None]
    return np.stack([cos_t, sin_t], axis=0).astype(np.float32)


SBUF_BYTES = 206 * 1024


def build(stage="full"):
    nc = bass.Bass("TRN2", target_bir_lowering=False)
    S = Sched()

    def din(name, shape, dt=F32):
        return nc.dram_tensor(name, list(shape), dt, kind="ExternalInput").ap()

    xe = din("xe", [8192, 1024])
    tv_d = din("tilevalid", [128, 64])
    cvec = din("cvec", [128, 8])
    cs_tab = din("cs_tab", [2, 128, 4096])
    cf_d = din("cst_f32", [128, 768])
    cb_d = din("cst_bf", [128, 896], BF16)
    w_mod = din("w_mod", [1024, 6144])
    b_mod = din("b_mod", [6144])
    g_pre_mix = din("g_pre_mix", [1024])
    g_post_mix = din("g_post_mix", [1024])
    g_pre_ffn = din("g_pre_ffn", [1024])
    g_post_ffn = din("g_post_ffn", [1024])
    w_in = din("w_in", [1024, 3088])
    w_gate_lr = din("w_gate_lr", [16, 256])
    b_gate = din("b_gate", [256])
    g_gla = din("g_gla", [128])
    w_out = din("w_out", [1024, 1024])
    if stage == "full":
        router_w = din("router_w", [1024, 32])
        router_b = din("router_b", [32])
        w_gate_up = din("w_gate_up", [32, 1024, 2048])
        b_gate_up = din("b_gate_up", [32, 2048])
        w_down = din("w_down", [32, 1024, 1024])
        b_down = din("b_down", [32, 1024])
    out = nc.dram_tensor("out", [2048, 1024], F32, kind="ExternalOutput").ap()
    vscr = nc.dram_tensor("vscr", [4096, 512], BF16, kind="Internal").ap()
    ogscr = nc.dram_tensor("ogscr", [4, 128, 2048], BF16, kind="Internal").ap()
    modscr = nc.dram_tensor("modscr", [4, 128, 1024], F32, kind="Internal").ap()
    B_vscr = Buf("vscr")
    B_ogscr = Buf("ogscr")
    B_modscr = Buf("modscr")
    B_out = [Buf("out%d" % i) for i in range(16)]

    es = ExitStack()
    big = es.enter_context(nc.sbuf_tensor("big", [128, SBUF_BYTES // 2], BF16))
    A = Arena(big, SBUF_BYTES)

    def sb(scope, name, shape, dt):
        t = A.alloc(name, list(shape), dt)
        if scope is not None:
            scope.append(t)
        return t

    def ring(scope, name, shape, dt, n):
        return Ring([sb(scope, "%s%d" % (name, i), shape, dt) for i in range(n)])

    def dma(q, out_ap, in_ap, reads, writes, key, partial=False):
        S.add(q, lambda e: e.dma_start(out=out_ap, in_=in_ap), reads=reads, writes=writes,
              dma=True, semkey=key, partial=partial)

    def mm(out_ap, lhsT, rhs, start, stop, reads, writes):
        S.add("pe", lambda e: e.matmul(out_ap, lhsT=lhsT, rhs=rhs, start=start, stop=stop),
              reads=reads, writes=writes)

    def tr(out_ap, in_ap, ident_ap, reads, writes):
        S.add("pe", lambda e: e.transpose(out_ap, in_ap, ident_ap), reads=reads, writes=writes)

    def act(out_ap, in_ap, func, reads, writes, bias=None, scale=None, accum=None):
        kw = {}
        if bias is not None:
            kw["bias"] = bias
        if scale is not None:
            kw["scale"] = scale
        if accum is not None:
            kw["accum_out"] = accum
        S.add("act", lambda e: e.activation(out=out_ap, in_=in_ap, func=func, **kw), reads=reads, writes=writes)

    def tt(eng, out_ap, a, b, op, reads, writes):
        S.add(eng, lambda e: e.tensor_tensor(out=out_ap, in0=a, in1=b, op=op), reads=reads, writes=writes)

    def ts(eng, out_ap, a, s1, s2, op0, op1, reads, writes):
        if op1 is None:
            S.add(eng, lambda e: e.tensor_scalar(out=out_ap, in0=a, scalar1=s1, scalar2=None, op0=op0),
                  reads=reads, writes=writes)
        else:
            S.add(eng, lambda e: e.tensor_scalar(out=out_ap, in0=a, scalar1=s1, scalar2=s2, op0=op0, op1=op1),
                  reads=reads, writes=writes)

    def stt(eng, out_ap, a, sc, b, op0, op1, reads, writes):
        S.add(eng, lambda e: e.scalar_tensor_tensor(out=out_ap, in0=a, scalar=sc, in1=b, op0=op0, op1=op1),
              reads=reads, writes=writes)

    def cp(eng, out_ap, in_ap, reads, writes):
        if eng == "act":
            S.add("act", lambda e: e.copy(out=out_ap, in_=in_ap), reads=reads, writes=writes)
        else:
            S.add(eng, lambda e: e.tensor_copy(out=out_ap, in_=in_ap), reads=reads, writes=writes)

    def recip(out_ap, in_ap, reads, writes):
        S.add("dve", lambda e: e.reciprocal(out=out_ap, in_=in_ap), reads=reads, writes=writes)

    def ttr(out_ap, a, b, accum, reads, writes):
        S.add("dve", lambda e: e.tensor_tensor_reduce(out=out_ap, in0=a, in1=b, scale=1.0, scalar=0.0,
                                                      op0=ALU.mult, op1=ALU.add, accum_out=accum),
              reads=reads, writes=writes)

    def rstd_(ap_out, ap_in, inv_n, bufs):
        act(ap_out, ap_in, AF.Ln, bufs + [epsc.b], bufs, bias=epsc.t[:, 0:1], scale=inv_n)
        act(ap_out, ap_out, AF.Exp, bufs, bufs, scale=-0.5)

    def memset(eng, ap, val, writes):
        S.add(eng, lambda e: e.memset(ap, val), writes=writes)

    psb = []
    for i in range(8):
        t = es.enter_context(nc.psum_tensor("psb%d" % i, [128, 512], F32))
        psb.append(TT(t, "psb%d" % i))
        psb[-1].b.excl = True
    PS = Ring(psb)

    def bfview(p):
        return p.t[:, :].bitcast(BF16)

    cf = sb(None, "cf", [128, 768], F32)
    cbt = sb(None, "cb", [128, 896], BF16)
    epsc = sb(None, "epsc", [128, 1], F32)
    memset("pool", epsc.t[:, :], EPS, [epsc.b])
    c14 = sb(None, "c14", [128, 1], F32)
    memset("pool", c14.t[:, :], 14.0, [c14.b])
    tv = sb(None, "tv", [128, 64], F32)
    dma("sp", cf.t[:, :], cf_d, [], [cf.b], "cf")
    dma("sp", cbt.t[:, :], cb_d, [], [cbt.b], "cb")
    dma("sp", tv.t[:, :], tv_d, [], [tv.b], "tv")
    ident_f = cf.t[:, 0:128]
    U_f = cf.t[:, 128:256]
    Ls_f = cf.t[:, 256:384]
    negdiag = cf.t[:, 384:512]
    ones_f = cf.t[:, 512:640]
    Psw_f = cf.t[:, 640:768]
    ident_b = cbt.t[:, 0:128]
    ones_b = cbt.t[:, 128:256]
    mask4 = cbt.t[:, 256:768]
    negsel_b = cbt.t[:, 768:770]
    mT = sb(None, "mT", [128, 512], BF16)
    cp("pool", mT.t[:, 0:384], cbt.t[:, 384:768], [cbt.b], [mT.b])
    cp("pool", mT.t[:, 384:512], cbt.t[:, 256:384], [cbt.b, mT.b], [mT.b])
    mask4h = sb(None, "mask4h", [128, 512], BF16)
    cp("pool", mask4h.t[:, :], mT.t[:, :], [mT.b], [mask4h.b])
    hv = tv.t[:, 32:33]
    for off in (0, 256):
        ts("dve", mask4h.t[:, off:off + 128], mask4h.t[:, off:off + 128], hv, None, ALU.mult, None,
           [mask4h.b, tv.b], [mask4h.b])

    mix = []
    qT = sb(mix, "qT", [128, 4, 2048], BF16)
    kT = sb(mix, "kT", [128, 4, 4096], BF16)

    p1 = []
    gmod1 = sb(p1, "gmod1", [128, 1024], F32)
    shift1 = sb(p1, "shift1", [128, 1024], F32)
    p0 = []
    modb = sb(p0, "modb", [128, 6144], F32)
    bmodb = sb(p0, "bmodb", [128, 6144], F32)
    gb = [sb(p0, "gb%d" % i, [128, 1024], F32) for i in range(4)]
    cv = sb(p0, "cv", [128, 8], F32)
    ecv = sb(p0, "ecv", [128, 8], F32)
    sc = sb(p0, "sc", [128, 8], F32)
    scb = sb(p0, "scb", [128, 8, 128], BF16)
    wm = ring(p0, "wm", [128, 8, 512], BF16, 2)
    tmp = ring(p0, "mtmp", [128, 1024], F32, 3)
    dma("sp", cv.t[:, :], cvec, [], [cv.b], "cv")
    dma("sp", bmodb.t[:, :], b_mod.partition_broadcast(128), [], [bmodb.b], "bmodb")
    for i, g in enumerate((g_pre_mix, g_post_mix, g_pre_ffn, g_post_ffn)):
        dma("sp", gb[i].t[:, :], g.partition_broadcast(128), [], [gb[i].b], "gb%d" % i)
    act(ecv.t[:, :], cv.t[:, :], AF.Exp, [cv.b], [ecv.b], scale=-1.0)
    ts("dve", ecv.t[:, :], ecv.t[:, :], 1.0, None, ALU.add, None, [ecv.b], [ecv.b])
    recip(ecv.t[:, :], ecv.t[:, :], [ecv.b], [ecv.b])
    tt("dve", sc.t[:, :], cv.t[:, :], ecv.t[:, :], ALU.mult, [cv.b, ecv.b], [sc.b])
    cp("dve", scb.t[:, :, :], sc.t[:, :].unsqueeze(2).to_broadcast([128, 8, 128]), [sc.b], [scb.b])
    wmv = w_mod.rearrange("(kc p) n -> p kc n", p=128)
    for blk in range(12):
        w = wm.next()
        dma("pool", w.t[:, :, :], wmv[:, :, blk * 512:(blk + 1) * 512], [], [w.b], w.name)
        ps = PS.next()
        for kc in range(8):
            mm(ps.t[:, :], scb.t[:, kc, :], w.t[:, kc, :], kc == 0, kc == 7, [scb.b, w.b], [ps.b])
        tt("dve", modb.t[:, blk * 512:(blk + 1) * 512], ps.t[:, :], bmodb.t[:, blk * 512:(blk + 1) * 512],
           ALU.add, [ps.b, bmodb.b], [modb.b])
    sl = lambda i: modb.t[:, i * 1024:(i + 1) * 1024]
    cp("pool", shift1.t[:, :], sl(0), [modb.b], [shift1.b])
    stt("dve", gmod1.t[:, :], sl(1), 1.0, gb[0].t[:, :], ALU.add, ALU.mult, [modb.b, gb[0].b], [gmod1.b])
    t0 = tmp.next()
    tt("dve", t0.t[:, :], sl(2), gb[1].t[:, :], ALU.mult, [modb.b, gb[1].b], [t0.b])
    dma("sp", modscr[0], t0.t[:, :], [t0.b], [B_modscr], "modscr", partial=True)
    dma("sp", modscr[1], sl(3), [modb.b], [B_modscr], "modscr", partial=True)
    t1 = tmp.next()
    stt("dve", t1.t[:, :], sl(4), 1.0, gb[2].t[:, :], ALU.add, ALU.mult, [modb.b, gb[2].b], [t1.b])
    dma("sp", modscr[2], t1.t[:, :], [t1.b], [B_modscr], "modscr", partial=True)
    t2 = tmp.next()
    tt("dve", t2.t[:, :], sl(5), gb[3].t[:, :], ALU.mult, [modb.b, gb[3].b], [t2.b])
    dma("sp", modscr[3], t2.t[:, :], [t2.b], [B_modscr], "modscr", partial=True)
    S.barrier()
    if stage == "p0":
        dma("sp", out[0:128, :], gmod1.t[:, :], [gmod1.b], [B_out[0]], "dbg0")
        dma("sp", out[128:256, :], shift1.t[:, :], [shift1.b], [B_out[1]], "dbg1")
        S.dead = True
    A.free(p0)

    w_in_sb = sb(p1, "w_in", [128, 8, 3088], BF16)
    winv = w_in.rearrange("(kc p) n -> p kc n", p=128)
    for kc in range(8):
        dma("pool", w_in_sb.t[:, kc, :], winv[:, kc, :], [], [w_in_sb.b], "w_in", partial=True)
    wg17 = sb(p1, "wg17", [32, 256], F32)
    memset("pool", wg17.t[:, :], 0.0, [wg17.b])
    dma("sp", wg17.t[0:16, :], w_gate_lr, [], [wg17.b], "wg17")
    dma("sp", wg17.t[16:17, :], b_gate.rearrange("(o n) -> o n", o=1), [], [wg17.b], "wg17b")
    gglab = sb(p1, "gglab", [128, 128], F32)
    dma("sp", gglab.t[:, :], g_gla.partition_broadcast(128), [], [gglab.b], "gglab")

    Sf = sb(p1, "Sf", [128, 2, 128], F32)
    Sb = sb(p1, "Sb", [128, 2, 128], BF16)
    memset("pool", Sf.t[:, :, :], 0.0, [Sf.b])
    memset("pool", Sb.t[:, :, :], 0.0, [Sb.b])

    rn = {}
    rn["x_ring"] = ring(p1, "xt", [128, 1024], F32, 2)
    rn["junk"] = sb(p1, "junk_a", [128, 1024], BF16)
    rn["ss_ring"] = ring(p1, "ss", [128, 2], F32, 4)
    rn["hb_ring"] = ring(p1, "hb", [128, 1024], BF16, 2)
    hT_ring = ring(p1, "hT", [128, 8, 512], BF16, 2)
    cs_ring = ring(p1, "csr", [128, 2, 512], F32, 1)
    qs_ring = ring(p1, "qsb", [128, 512], F32, 2)
    rp_ring = ring(p1, "rp", [128, 512], F32, 2)
    vt_ring = ring(p1, "vt", [128, 512], BF16, 2)
    glr_ring = ring(p1, "glr", [32, 512], F32, 2)
    for g in glr_ring.items:
        memset("pool", g.t[:, :], 1.0, [g.b])
    e1_ring = ring(p1, "e1", [128, 256], F32, 2)
    sp_ring = ring(p1, "spr", [128, 256], F32, 2)
    dec_ring = ring(p1, "dec", [128, 256], F32, 2)
    ku_ring = ring(p1, "ku", [128, 256], BF16, 2)
    vg_ring = ring(p1, "vg", [128, 512], BF16, 2)
    acol_ring = ring(p1, "acol", [128, 2], F32, 2)
    eb_ring = ring(p1, "eb", [128, 256], F32, 2)
    enb_ring = ring(p1, "enb", [128, 256], F32, 2)
    qd_ring = ring(p1, "qd", [128, 2, 128], BF16, 2)
    kd_ring = ring(p1, "kd", [128, 2, 128], BF16, 2)
    attm_ring = ring(p1, "attm", [128, 4, 128], BF16, 2)
    er_ring = ring(p1, "er", [128, 512], F32, 1)
    rg_ring = ring(p1, "rg", [128, 512], F32, 2)
    ssg_ring = ring(p1, "ssg", [128, 4], F32, 2)
    on_ring = ring(p1, "on", [128, 512], F32, 1)
    og_ring = ring(p1, "og", [128, 512], BF16, 2)
    ogT_ring = ring(p1, "ogT", [128, 4, 128], BF16, 2)
    junk_s = sb(p1, "junk_s", [128, 128], BF16)

    def proj_fm(col0, ncols, hTb, tsl, ps, pcols):
        for kc in range(8):
            mm(ps.t[0:ncols, pcols], w_in_sb.t[:, kc, col0:col0 + ncols], hTb.t[:, kc, tsl], kc == 0, kc == 7,
               [w_in_sb.b, hTb.b], [ps.b])

    def proj_tm(ti, col0, ncols, hTb, ps):
        for kc in range(8):
            mm(ps.t[:, 0:ncols], hTb.t[:, kc, ti * 128:(ti + 1) * 128], w_in_sb.t[:, kc, col0:col0 + ncols],
               kc == 0, kc == 7, [w_in_sb.b, hTb.b], [ps.b])

    def rmsnorm_mod_T(xt, gm, sh, dstT, dst_cols, defer=False):
        ss = rn["ss_ring"].next()
        junk = rn["junk"]
        memset("pool", ss.t[:, :], 0.0, [ss.b])
        act(junk.t[:, :], xt.t[:, :], AF.Square, [xt.b], [junk.b, ss.b], accum=ss.t[:, 0:1])
        rstd_(ss.t[:, 1:2], ss.t[:, 0:1], 1.0 / 1024.0, [ss.b])
        stt("dve", xt.t[:, :], xt.t[:, :], ss.t[:, 1:2], gm.t[:, :], ALU.mult, ALU.mult, [xt.b, ss.b, gm.b], [xt.b])
        hb = rn["hb_ring"].next()
        tt("dve", hb.t[:, :], xt.t[:, :], sh.t[:, :], ALU.add, [xt.b, sh.b], [hb.b])
        if defer:
            return hb
        rmsnorm_T2(hb, dstT, dst_cols)

    def rmsnorm_T2(hb, dstT, dst_cols):
        ps = PS.next()
        pv = bfview(ps)
        for kc in range(8):
            tr(pv[:, kc * 128:(kc + 1) * 128], hb.t[:, kc * 128:(kc + 1) * 128], ident_b, [hb.b, cbt.b], [ps.b])
        cp("act", dstT.t[:, :, dst_cols], pv.rearrange("p (a b) -> p a b", a=8), [ps.b], [dstT.b])

    def cut(name):
        if stage == name and not S.dead:
            S.barrier()
            dma("sp", out[0:128, 0:768], cf.t[:, :], [cf.b], [B_out[0]], "dbgc")
            S.dead = True

    pending = [None]

    def back(ctx):
        acol, ku, vg = ctx["acol"], ctx["ku"], ctx["vg"]
        pu = PU.next()
        for h in range(4):
            g, hh = h // 2, h % 2
            mm(pu.t[hh * 64:(hh + 1) * 64, g * 128:(g + 1) * 128], ku.t[:, h * 64:(h + 1) * 64],
               vg.t[:, h * 128:(h + 1) * 128], True, True, [ku.b, vg.b], [pu.b])
        if ctx["kind"] == "own":
            attm, qd, rg, m0, ti = ctx["attm"], ctx["qd"], ctx["rg"], ctx["m0"], ctx["ti"]
            po = PS.next()
            for h in range(4):
                g, hh = h // 2, h % 2
                mm(po.t[:, h * 128:(h + 1) * 128], attm.t[:, h, :], vg.t[:, h * 128:(h + 1) * 128],
                   True, False, [attm.b, vg.b], [po.b])
                mm(po.t[:, h * 128:(h + 1) * 128], qd.t[hh * 64:(hh + 1) * 64, g, :],
                   Sb.t[hh * 64:(hh + 1) * 64, g, :], False, True, [qd.b, Sb.b], [po.b])
            ssg = ssg_ring.next()
            memset("pool", ssg.t[:, :], 0.0, [ssg.b])
            for h in range(4):
                act(junk_s.t[:, :], po.t[:, h * 128:(h + 1) * 128], AF.Square, [po.b], [junk_s.b, ssg.b],
                    accum=ssg.t[:, h:h + 1])
            rstd_(ssg.t[:, :], ssg.t[:, :], 1.0 / 128.0, [ssg.b])
            on = on_ring.next()
            tt("dve", on.t[:, :].rearrange("p (a b) -> p a b", a=4), po.t[:, :].rearrange("p (a b) -> p a b", a=4),
               ssg.t[:, :].unsqueeze(2).to_broadcast([128, 4, 128]), ALU.mult, [po.b, ssg.b], [on.b])
            og = og_ring.next()
            tt("pool", og.t[:, :], on.t[:, :], rg.t[:, :], ALU.mult, [on.b, rg.b], [og.b])
            pt2 = PS.next()
            pv2 = bfview(pt2)
            for h in range(4):
                tr(pv2[:, h * 128:(h + 1) * 128], og.t[:, h * 128:(h + 1) * 128], ident_b, [og.b, cbt.b], [pt2.b])
            ogT = ogT_ring.next()
            cp("act", ogT.t[:, :, :], pv2[:, 0:512].rearrange("p (a b) -> p a b", a=4), [pt2.b], [ogT.b])
            dma("sp", ogscr[:, :, m0 + ti * 128:m0 + (ti + 1) * 128].rearrange("h e t -> e h t"), ogT.t[:, :, :],
                [ogT.b], [B_ogscr], ogT.name, partial=True)
        for g in range(2):
            stt("dve", Sf.t[:, g, :], Sf.t[:, g, :], acol.t[:, g:g + 1], pu.t[:, g * 128:(g + 1) * 128],
                ALU.mult, ALU.add, [Sf.b, acol.b, pu.b], [Sf.b])
        cp("pool", Sb.t[:, :, :], Sf.t[:, :, :], [Sf.b], [Sb.b])

    hTbs = {}

    pro_pending = [None]

    def prologue_flush():
        if pro_pending[0] is not None:
            hb_, tb2, ti2 = pro_pending[0]
            rmsnorm_T2(hb_, hTbs[tb2], slice(ti2 * 128, (ti2 + 1) * 128))
            pro_pending[0] = None

    def prologue_tile(tb_, ti_, immediate=False):
        t_ = tb_ * 4 + ti_
        xt = rn["x_ring"].next()
        dma("sp", xt.t[:, :], xe[t_ * 128:(t_ + 1) * 128, :], [], [xt.b], xt.name)
        hb_ = rmsnorm_mod_T(xt, gmod1, shift1, None, None, defer=True)
        prologue_flush()
        pro_pending[0] = (hb_, tb_, ti_)
        if immediate:
            prologue_flush()

    full512 = slice(0, 512)
    PU = Ring(psb[6:8])
    PS.items = psb[0:6]
    for tb in range(16):
        kind = "prefix" if tb < 8 else ("halo" if tb < 12 else "own")
        S.bmarks = getattr(S, "bmarks", []) + [len(S.streams["pe"])]
        if tb == 0:
            hTbs[0] = hT_ring.next()
            for ti in range(4):
                prologue_tile(0, ti, immediate=(ti == 3))
        hTb = hTbs[tb]
        if tb + 1 < 16:
            hTbs[tb + 1] = hT_ring.next()
        cut("c_norm")
        n0 = (tb - 8) * 512
        m0 = (tb - 12) * 512
        if kind != "prefix":
            csr = cs_ring.next()
            dma("sp", csr.t[:, :, :], cs_tab[:, :, n0:n0 + 512].rearrange("a p n -> p a n"), [], [csr.b], csr.name)
            jobs = [(512, kT, n0)]
            if kind == "own":
                jobs.append((0, qT, m0))
            for (cbase, dst, d0) in jobs:
                for hp in range(4):
                    pa = PS.next()
                    proj_fm(cbase + hp * 128, 128, hTb, full512, pa, full512)
                    qs = qs_ring.next()
                    cp("act", qs.t[:, :], pa.t[:, :], [pa.b], [qs.b])
                    pb_ = PS.next()
                    mm(pb_.t[:, :], Psw_f, qs.t[:, :], True, True, [cf.b, qs.b], [pb_.b])
                    r1 = rp_ring.next()
                    r2 = rp_ring.next()
                    tt("pool", r1.t[:, :], qs.t[:, :], csr.t[:, 0, :], ALU.mult, [qs.b, csr.b], [r1.b])
                    tt("dve", r2.t[:, :], pb_.t[:, :], csr.t[:, 1, :], ALU.mult, [pb_.b, csr.b], [r2.b])
                    tt("pool", dst.t[:, hp, d0:d0 + 512], r1.t[:, :], r2.t[:, :], ALU.add, [r1.b, r2.b], [dst.b])
        if tb == 8:
            cut("c_rope")
        glr = glr_ring.next()
        pg = PS.next()
        proj_fm(3072, 16, hTb, full512, pg, full512)
        cp("act", glr.t[0:16, :], pg.t[0:16, :], [pg.b], [glr.b])
        for ti in range(4):
            t = tb * 4 + ti
            tsl = slice(ti * 128, (ti + 1) * 128)
            if kind != "prefix":
                pvv = PS.next()
                proj_tm(ti, 1024, 512, hTb, pvv)
                vt = vt_ring.next()
                cp("act", vt.t[:, :], pvv.t[:, :], [pvv.b], [vt.b])
                dma("sp", vscr[n0 + ti * 128:n0 + (ti + 1) * 128, :], vt.t[:, :], [vt.b], [B_vscr], vt.name,
                    partial=True)
            pgl = PS.next()
            mm(pgl.t[:, 0:256], glr.t[0:17, tsl], wg17.t[0:17, :], True, True, [glr.b, wg17.b], [pgl.b])
            e1 = e1_ring.next()
            act(e1.t[:, :], pgl.t[:, 0:256], AF.Exp, [pgl.b], [e1.b], scale=-1.0)
            spt = sp_ring.next()
            act(spt.t[:, :], e1.t[:, :], AF.Ln, [e1.b], [spt.b], bias=1.0)
            pk = PS.next()
            proj_tm(ti, 1792, 256, hTb, pk)
            pvg = PS.next()
            proj_tm(ti, 2048, 512, hTb, pvg)
            vg = vg_ring.next()
            cp("act", vg.t[:, :], pvg.t[:, :], [pvg.b], [vg.b])
            paf = PS.next()
            mm(paf.t[:, 0:256], Ls_f, spt.t[:, :], True, True, [cf.b, spt.b], [paf.b])
            dec = dec_ring.next()
            act(dec.t[:, :], paf.t[:, 0:256], AF.Exp, [paf.b], [dec.b], scale=-1.0 / 16.0)
            ku = ku_ring.next()
            stt("dve", ku.t[:, :], pk.t[:, 0:256], tv.t[:, t:t + 1], dec.t[:, :], ALU.mult, ALU.mult,
                [pk.b, tv.b, dec.b], [ku.b])
            pa_ = PS.next()
            for g in range(2):
                mm(pa_.t[:, g:g + 1], spt.t[:, g * 128:(g + 1) * 128], ones_f[:, 0:1], True, True,
                   [spt.b, cf.b], [pa_.b])
            acol = acol_ring.next()
            act(acol.t[:, :], pa_.t[:, 0:2], AF.Exp, [pa_.b], [acol.b], scale=-1.0 / 16.0)
            if kind == "own":
                pcs = PS.next()
                for g in range(2):
                    mm(pcs.t[:, g * 128:(g + 1) * 128], spt.t[:, g * 128:(g + 1) * 128], U_f, True, True,
                       [spt.b, cf.b], [pcs.b])
                eb = eb_ring.next()
                enb = enb_ring.next()
                act(eb.t[:, :], pcs.t[:, 0:256], AF.Exp, [pcs.b], [eb.b], scale=-1.0 / 16.0)
                act(enb.t[:, :], pcs.t[:, 0:256], AF.Exp, [pcs.b], [enb.b], scale=1.0 / 16.0)
                pqk = PS.next()
                for g in range(2):
                    proj_fm(1536 + g * 128, 128, hTb, tsl, pqk, slice(g * 128, (g + 1) * 128))
                    proj_fm(1792 + g * 128, 128, hTb, tsl, pqk, slice(256 + g * 128, 256 + (g + 1) * 128))
                qd = qd_ring.next()
                kd = kd_ring.next()
                stt("dve", qd.t[:, :, :], pqk.t[:, 0:256].rearrange("p (a b) -> p a b", a=2), 0.125,
                    eb.t[:, :].rearrange("p (a b) -> p a b", a=2), ALU.mult, ALU.mult, [pqk.b, eb.b], [qd.b])
                tt("dve", kd.t[:, :, :], pqk.t[:, 256:512].rearrange("p (a b) -> p a b", a=2),
                   enb.t[:, :].rearrange("p (a b) -> p a b", a=2), ALU.mult, [pqk.b, enb.b], [kd.b])
                pr = PS.next()
                proj_tm(ti, 2560, 512, hTb, pr)
                er = er_ring.next()
                act(er.t[:, :], pr.t[:, :], AF.Exp, [pr.b], [er.b], scale=-1.0)
                rg = rg_ring.next()
                tt("dve", rg.t[:, :].rearrange("p (a b) -> p a b", a=4), pr.t[:, :].rearrange("p (a b) -> p a b", a=4),
                   gglab.t[:, :].unsqueeze(1).to_broadcast([128, 4, 128]), ALU.mult, [pr.b, gglab.b], [rg.b])
                ts("dve", er.t[:, :], er.t[:, :], 1.0, None, ALU.add, None, [er.b], [er.b])
                recip(er.t[:, :], er.t[:, :], [er.b], [er.b])
                tt("pool", rg.t[:, :], rg.t[:, :], er.t[:, :], ALU.mult, [rg.b, er.b], [rg.b])
                patt = [PS.next(), PS.next()]
                for h in range(4):
                    g, hh = h // 2, h % 2
                    mm(patt[hh].t[:, g * 128:(g + 1) * 128], kd.t[hh * 64:(hh + 1) * 64, g, :],
                       qd.t[hh * 64:(hh + 1) * 64, g, :], True, True, [kd.b, qd.b], [patt[hh].b])
                attm = attm_ring.next()
                for hh in range(2):
                    tt("dve", attm.t[:, hh:4:2, :], patt[hh].t[:, 0:256].rearrange("p (a b) -> p a b", a=2),
                       U_f.unsqueeze(1).to_broadcast([128, 2, 128]), ALU.mult, [patt[hh].b, cf.b], [attm.b])
            ctx = dict(kind=kind, acol=acol, ku=ku, vg=vg)
            if kind == "own":
                ctx.update(attm=attm, qd=qd, rg=rg, m0=m0, ti=ti)
            if pending[0] is not None:
                back(pending[0])
            pending[0] = ctx
            if tb + 1 < 16:
                prologue_tile(tb + 1, ti, immediate=(ti == 3))
    back(pending[0])
    PS.items = psb
    S.barrier()
    if stage == "p1":
        dbgt = sb(None, "dbgt", [128, 1024], F32)
        for i in range(4):
            cp("dve", dbgt.t[:, :], kT.t[:, i, 2048:3072], [kT.b], [dbgt.b])
            dma("sp", out[i * 128:(i + 1) * 128, :], dbgt.t[:, :], [dbgt.b], [B_out[i]], "dbg0")
        for i in range(4):
            cp("dve", dbgt.t[:, :], qT.t[:, i, 0:1024], [qT.b], [dbgt.b])
            dma("sp", out[(4 + i) * 128:(5 + i) * 128, :], dbgt.t[:, :], [dbgt.b], [B_out[4 + i]], "dbg0")
        S.dead = True
    A.free(p1)

    p23 = []
    catT = sb(p23, "catT", [128, 8, 2048], BF16)
    w_out_sb = sb(p23, "w_out", [128, 8, 1024], BF16)
    woutv = w_out.rearrange("(kc p) n -> p kc n", p=128)
    for kc in range(8):
        dma("pool", w_out_sb.t[:, kc, :], woutv[:, kc, :], [], [w_out_sb.b], "w_out", partial=True)
    for h in range(4):
        dma("sp", catT.t[:, 4 + h, :], ogscr[h], [B_ogscr], [catT.b], "catT_ld", partial=True)
    pa = []
    vp_ring = ring(pa, "vp", [128, 3, 32, 128], BF16, 2)
    acc = sb(pa, "acc", [128, 2, 2048], F32)
    nb_ring = ring(pa, "nb", [128, 2], F32, 4)
    prod = sb(pa, "prod", [128, 2048], BF16)
    P_ring = ring(pa, "P", [128, 512], BF16, 4)
    PT_ring = ring(pa, "PT", [128, 512], BF16, 4)
    rl = sb(pa, "rl", [128, 2048], F32)
    PATS = ((0, 1), (1, 4), (2, 16))
    vps = {}

    def load_vp(hp_):
        vp_ = vp_ring.next()
        for (pi, d) in PATS:
            src = vscr[:, hp_ * 128:(hp_ + 1) * 128].rearrange("(u d) c -> d u c", d=d)
            ntile = 32 // d
            for r in range(d):
                dma("sp", vp_.t[:, pi, r * ntile:(r + 1) * ntile, :],
                    src[r].rearrange("(kt p) c -> p kt c", p=128), [B_vscr], [vp_.b], vp_.name, partial=True)
        vps[hp_] = vp_

    def mk_prod(hp_):
        tt("pool", prod.t[:, :], qT.t[:, hp_, :], kT.t[:, hp_, 2048:4096], ALU.mult, [qT.b, kT.b], [prod.b])

    load_vp(0)
    load_vp(1)
    mk_prod(0)
    for hp in range(4):
        vp = vps[hp]
        if 1 <= hp < 3:
            load_vp(hp + 1)
        memset("pool", acc.t[:, :, :], 0.0, [acc.b])
        units = []
        for (pi, d) in PATS:
            ntile = 32 // d
            for r in range(d):
                for kt in range(16 // d, ntile):
                    units.append((pi, d, r, kt))

        def stageA(u):
            pi, d, r, kt = u
            ntile = 32 // d
            st_ = {}
            q0 = r + d * kt * 128 - 2048
            k0 = r + d * (kt - 1) * 128
            qs = slice(q0, q0 + d * 127 + 1, d)
            ks = slice(k0, k0 + d * 255 + 1, d)
            pS = [PS.next(), PS.next()]
            for hh in range(2):
                mm(pS[hh].t[:, 0:256], qT.t[hh * 64:(hh + 1) * 64, hp, qs],
                   kT.t[hh * 64:(hh + 1) * 64, hp, ks], True, True, [qT.b, kT.b], [pS[hh].b])
            mm(pS[0].t[:, 256:258], prod.t[:, qs], negsel_b, True, True, [prod.b, cbt.b], [pS[0].b])
            nb = nb_ring.next()
            cp("dve", nb.t[:, 0:2], pS[0].t[:, 256:258], [pS[0].b], [nb.b])
            Pt = P_ring.next()
            for hh in range(2):
                act(Pt.t[:, hh * 256:(hh + 1) * 256], pS[hh].t[:, 0:256], AF.Exp,
                    [pS[hh].b, nb.b], [Pt.b], bias=nb.t[:, hh:hh + 1], scale=0.125)
            st_.update(Pt=Pt, qs=qs, vt0=r * ntile + kt - 1, pi=pi, first=(kt == 16 // d))
            return st_

        def stageB(st_):
            Pt = st_["Pt"]
            pT = PS.next()
            pTv = bfview(pT)
            for i in range(4):
                tr(pTv[:, i * 128:(i + 1) * 128], Pt.t[:, i * 128:(i + 1) * 128], ident_b,
                   [Pt.b, cbt.b], [pT.b])
            PT = PT_ring.next()
            mk = mask4h if st_["first"] else mT
            tt("dve", PT.t[:, :], pTv[:, 0:512], mk.t[:, :], ALU.mult, [pT.b, mk.b], [PT.b])
            st_["PT"] = PT

        def stageC(st_):
            PT, qs, vt0, pi = st_["PT"], st_["qs"], st_["vt0"], st_["pi"]
            pO = PS.next()
            for hh in range(2):
                osl = pO.t[hh * 64:(hh + 1) * 64, 0:128]
                lsl = pO.t[hh * 64:(hh + 1) * 64, 128:256]
                for kk in range(2):
                    mm(osl, vp.t[:, pi, vt0 + kk, hh * 64:(hh + 1) * 64],
                       PT.t[:, (hh * 2 + kk) * 128:(hh * 2 + kk + 1) * 128], kk == 0, kk == 1,
                       [vp.b, PT.b], [pO.b])
                for kk in range(2):
                    mm(lsl, ones_b[:, 0:64], PT.t[:, (hh * 2 + kk) * 128:(hh * 2 + kk + 1) * 128],
                       kk == 0, kk == 1, [cbt.b, PT.b], [pO.b])
            tt("dve", acc.t[:, :, qs], acc.t[:, :, qs], pO.t[:, 0:256].rearrange("p (a b) -> p a b", a=2),
               ALU.add, [acc.b, pO.b], [acc.b])

        sts = []
        nu = len(units)
        for it in range(nu + 2):
            if it < nu:
                sts.append(stageA(units[it]))
            if it == nu - 1 and hp + 1 < 4:
                mk_prod(hp + 1)
            if 0 <= it - 1 < nu:
                stageB(sts[it - 1])
            if 0 <= it - 2 < nu:
                stageC(sts[it - 2])
        recip(rl.t[:, :], acc.t[:, 1, :], [acc.b], [rl.b])
        tt("pool", catT.t[:, hp, :], acc.t[:, 0, :], rl.t[:, :], ALU.mult, [acc.b, rl.b], [catT.b])
    S.barrier()
    if stage == "p2":
        dbgt = sb(None, "dbgt", [128, 1024], F32)
        for i in range(8):
            cp("dve", dbgt.t[:, :], catT.t[:, i, 0:1024], [catT.b], [dbgt.b])
            dma("sp", out[i * 128:(i + 1) * 128, :], dbgt.t[:, :], [dbgt.b], [B_out[i]], "dbg0")
        S.dead = True
    A.free(pa)
    A.free(mix)

    p34 = []
    if stage == "full":
        h2T = sb(p34, "h2T", [128, 8, 2048], BF16)
        G = sb(p34, "G", [128, 16, 32], F32)
        GT = sb(p34, "GT", [32, 2048], BF16)
        wgu_ring = Ring([sb(p34, "wgu%d" % i, [128, 8, 1024], BF16) for i in range(2)])
        wd_ring = Ring([sb(p34, "wd0", [128, 8, 1024], BF16)])
        wguv = w_gate_up.rearrange("e (kc p) n -> e p kc n", p=128)
        wdv = w_down.rearrange("e (kc p) n -> e p kc n", p=128)

        def load_gu(e_, hx):
            wg = wgu_ring.next()
            for kc in range(0, 8, 2):
                dma("pool", wg.t[:, kc:kc + 2, :], wguv[e_, :, kc:kc + 2, hx * 1024:(hx + 1) * 1024], [], [wg.b],
                    wg.name, partial=True)
            return wg

        def load_d(e_):
            wd = wd_ring.next()
            for kc in range(0, 8, 2):
                dma("pool", wd.t[:, kc:kc + 2, :], wdv[e_, :, kc:kc + 2, :], [], [wd.b], wd.name, partial=True)
            return wd

        cur0 = [load_gu(0, 0), load_gu(0, 1), load_d(0)]
        rw = sb(p23, "rw", [128, 8, 32], BF16)
        rbb = sb(p23, "rbb", [128, 32], F32)
        dma("pool", rw.t[:, :, :], router_w.rearrange("(kc p) n -> p kc n", p=128), [], [rw.b], "rw")
        dma("sp", rbb.t[:, :], router_b.partition_broadcast(128), [], [rbb.b], "rbb")
        lg_ring = ring(p23, "lg", [128, 32], F32, 2)
        m8_ring = ring(p23, "m8", [128, 8], F32, 2)
        ex_ring = ring(p23, "ex", [128, 32], F32, 2)
        msk_ring = ring(p23, "msk", [128, 32], F32, 2)
        den_ring = ring(p23, "den", [128, 2], F32, 2)
        gtb_ring = ring(p23, "gtb", [128, 32], BF16, 2)
    mods = [sb(p23, "mods%d" % i, [128, 1024], F32) for i in range(3)]
    for i in range(3):
        dma("sp", mods[i].t[:, :], modscr[i], [B_modscr], [mods[i].b], "mods%d" % i)
    gg1, shift2, gmod2 = mods
    rn["x_ring"] = ring(p23, "xt3", [128, 1024], F32, 2)
    rn["junk"] = sb(p23, "junk_a3", [128, 1024], BF16)
    rn["ss_ring"] = ring(p23, "ss3", [128, 2], F32, 4)
    rn["hb_ring"] = ring(p23, "hb3", [128, 1024], BF16, 2)
    yt_ring = ring(p23, "yt", [128, 1024], F32, 2)
    x1_ring = ring(p23, "x1", [128, 1024], F32, 2)
    junk3 = rn["junk"]

    st3 = {}

    def p3A(ti):
        tsl = slice(ti * 128, (ti + 1) * 128)
        py = [PS.next(), PS.next()]
        for half in range(2):
            for kc in range(8):
                mm(py[half].t[:, :], catT.t[:, kc, tsl], w_out_sb.t[:, kc, half * 512:(half + 1) * 512],
                   kc == 0, kc == 7, [catT.b, w_out_sb.b], [py[half].b])
        ss = rn["ss_ring"].next()
        memset("pool", ss.t[:, :], 0.0, [ss.b])
        for half in range(2):
            act(junk3.t[:, 0:512], py[half].t[:, :], AF.Square, [py[half].b], [junk3.b, ss.b],
                accum=ss.t[:, half:half + 1])
        tt("dve", ss.t[:, 0:1], ss.t[:, 0:1], ss.t[:, 1:2], ALU.add, [ss.b], [ss.b])
        rstd_(ss.t[:, 0:1], ss.t[:, 0:1], 1.0 / 1024.0, [ss.b])
        yt = yt_ring.next()
        for half in range(2):
            hs = slice(half * 512, (half + 1) * 512)
            stt("dve", yt.t[:, hs], py[half].t[:, :], ss.t[:, 0:1], gg1.t[:, hs], ALU.mult, ALU.mult,
                [py[half].b, ss.b, gg1.b], [yt.b])
        xt = rn["x_ring"].next()
        dma("sp", xt.t[:, :], xe[(48 + ti) * 128:(49 + ti) * 128, :], [], [xt.b], xt.name)
        x1 = x1_ring.next()
        tt("pool", x1.t[:, :], xt.t[:, :], yt.t[:, :], ALU.add, [xt.b, yt.b], [x1.b])
        dma("sp", out[tsl, :], x1.t[:, :], [x1.b], [B_out[ti]], "out_w%d" % (ti % 4))
        st3[ti] = dict(x1=x1)

    def p3B1(ti):
        st3[ti]["hb"] = rmsnorm_mod_T(st3[ti]["x1"], gmod2, shift2, None, None, defer=True)

    def p3B2(ti):
        tsl = slice(ti * 128, (ti + 1) * 128)
        rmsnorm_T2(st3[ti]["hb"], h2T, tsl)
        pl = PS.next()
        for kc in range(8):
            mm(pl.t[:, 0:32], h2T.t[:, kc, tsl], rw.t[:, kc, :], kc == 0, kc == 7, [h2T.b, rw.b], [pl.b])
        lg = lg_ring.next()
        tt("dve", lg.t[:, :], pl.t[:, 0:32], rbb.t[:, :], ALU.add, [pl.b, rbb.b], [lg.b])
        m8 = m8_ring.next()
        S.add("dve", (lambda m8=m8, lg=lg: (lambda e: e.max(out=m8.t[:, :], in_=lg.t[:, :])))(),
              reads=[lg.b], writes=[m8.b])
        msk = msk_ring.next()
        ts("dve", msk.t[:, :], lg.t[:, :], m8.t[:, 3:4], None, ALU.is_ge, None, [lg.b, m8.b], [msk.b])
        den = den_ring.next()
        memset("pool", den.t[:, :], 0.0, [den.b])
        ts("dve", den.t[:, 0:1], m8.t[:, 0:1], -1.0, None, ALU.mult, None, [m8.b], [den.b])
        ex = ex_ring.next()
        act(ex.t[:, :], lg.t[:, :], AF.Exp, [lg.b, den.b], [ex.b], bias=den.t[:, 0:1], scale=1.0)
        tt("dve", ex.t[:, :], ex.t[:, :], msk.t[:, :], ALU.mult, [ex.b, msk.b], [ex.b])
        S.add("dve", (lambda ex=ex, den=den: (lambda e: e.reduce_sum(out=den.t[:, 1:2], in_=ex.t[:, :],
                                                                   axis=mybir.AxisListType.X)))(),
              reads=[ex.b], writes=[den.b])
        recip(den.t[:, 1:2], den.t[:, 1:2], [den.b], [den.b])
        ts("dve", G.t[:, ti, :], ex.t[:, :], den.t[:, 1:2], None, ALU.mult, None, [ex.b, den.b], [G.b])
        gtb = gtb_ring.next()
        cp("pool", gtb.t[:, :], G.t[:, ti, :], [G.b], [gtb.b])
        pgt = PS.next()
        pgv = bfview(pgt)
        tr(pgv[0:32, 0:128], gtb.t[:, :], ident_b, [gtb.b, cbt.b], [pgt.b])
        cp("act", GT.t[:, tsl], pgv[0:32, 0:128], [pgt.b], [GT.b])

    for it in range(16 + 2):
        if it < 16:
            p3A(it)
        if stage == "full":
            if 0 <= it - 1 < 16:
                p3B1(it - 1)
            if 0 <= it - 2 < 16:
                p3B2(it - 2)
    S.barrier()
    A.free(p23)

    if stage == "full":
        p4 = []
        gg2b = sb(p4, "gg2b", [128, 1024], F32)
        dma("sp", gg2b.t[:, :], modscr[3], [B_modscr], [gg2b.b], "gg2b")
        bdn = sb(p4, "bdn", [32, 1024], BF16)
        dma("pool", bdn.t[:, :], b_down, [], [bdn.b], "bdn")
        bgu = sb(p4, "bgu", [128, 16, 32], F32)
        pb = []
        bgs = sb(pb, "bgs", [32, 2048], F32)
        dma("sp", bgs.t[:, :], b_gate_up, [], [bgs.b], "bgs")
        bview = bgs.t[:, :].rearrange("e (fb p s) -> e fb s p", fb=8, s=2)
        for fb in range(8):
            for s_ in range(2):
                pbt = PS.next()
                tr(pbt.t[:, 0:32], bview[:, fb, s_, :], ident_f[0:32, 0:32], [bgs.b, cf.b], [pbt.b])
                cp("act", bgu.t[:, fb * 2 + s_, :], pbt.t[:, 0:32], [pbt.b], [bgu.b])
        bl7 = sb(p4, "bl7", [128, 8, 32], F32)
        ts("dve", bl7.t[:, :, :], bgu.t[:, 1:16:2, :], -1.0, 7.0, ALU.mult, ALU.add, [bgu.b], [bl7.b])
        S.barrier()
        A.free(pb)
        accm = sb(p4, "accm", [128, 8, 1024], F32)
        accb = [Buf("accm%d" % i) for i in range(8)]
        wgu_ring.items = wgu_ring.items + [sb(p4, "wgu2", [128, 8, 1024], BF16)]
        wd_ring.items = wd_ring.items + [sb(p4, "wd1", [128, 8, 1024], BF16)]
        actT_ring = ring(p4, "actT", [128, 8, 512], BF16, 2)
        g_ring = ring(p4, "g_", [128, 512], F32, 2)
        sg_ring = ring(p4, "sg_", [128, 512], F32, 2)
        l_ring = ring(p4, "l_", [128, 512], F32, 2)
        x1b_ring = ring(p4, "x1b", [128, 1024], F32, 2)
        ss4_ring = ring(p4, "ss4", [128, 2], F32, 4)
        junk4 = sb(p4, "junk4", [128, 1024], BF16)
        seq = [(hf_, e_) for hf_ in range(2) for e_ in range(32)]
        NSK = 2

        def gu_fb(e_, tok0, fb, wgA, wgB, aT):
            wg = wgA if fb < 4 else wgB
            fl = fb % 4
            pG = PS.next()
            pL = PS.next()
            for kc in range(8):
                mm(pG.t[:, :], wg.t[:, kc, fl * 256:(fl + 1) * 256:2], h2T.t[:, kc, tok0:tok0 + 512],
                   kc == 0, kc == 7, [wg.b, h2T.b], [pG.b])
            for kc in range(8):
                mm(pL.t[:, :], wg.t[:, kc, fl * 256 + 1:(fl + 1) * 256:2], h2T.t[:, kc, tok0:tok0 + 512],
                   kc == 0, kc == 7, [wg.b, h2T.b], [pL.b])
            g_ = g_ring.next()
            ts("dve", g_.t[:, :], pG.t[:, :], bgu.t[:, fb * 2, e_:e_ + 1], 7.0, ALU.add, ALU.min,
               [pG.b, bgu.b], [g_.b])
            sg = sg_ring.next()
            act(sg.t[:, :], g_.t[:, :], AF.Sigmoid, [g_.b], [sg.b], scale=1.702)
            l_ = l_ring.next()
            act(l_.t[:, :], pL.t[:, :], AF.Relu, [pL.b, bl7.b], [l_.b], bias=bl7.t[:, fb, e_:e_ + 1], scale=-1.0)
            act(l_.t[:, :], l_.t[:, :], AF.Relu, [l_.b, c14.b], [l_.b], bias=c14.t[:, 0:1], scale=-1.0)
            tt("dve", g_.t[:, :], g_.t[:, :], sg.t[:, :], ALU.mult, [g_.b, sg.b], [g_.b])
            stt("dve", aT.t[:, fb, :], l_.t[:, :], -6.0, g_.t[:, :], ALU.add, ALU.mult, [g_.b, l_.b], [aT.b])

        def down(hf_, e_, tb, aT, wd):
            for tl in range(4):
                tg = hf_ * 8 + tb * 4 + tl
                ta = tb * 4 + tl
                py = [PS.next(), PS.next()]
                for half in range(2):
                    for fb in range(8):
                        mm(py[half].t[:, :], aT.t[:, fb, tl * 128:(tl + 1) * 128],
                           wd.t[:, fb, half * 512:(half + 1) * 512], fb == 0, fb == 7, [aT.b, wd.b], [py[half].b])
                for half in range(2):
                    hs = slice(half * 512, (half + 1) * 512)
                    if e_ == 0:
                        S.add("dve", (lambda o=accm.t[:, ta, hs], a=py[half].t[:, :], g=G.t[:, tg, e_:e_ + 1]:
                                      (lambda e: e.tensor_scalar(out=o, in0=a, scalar1=g, scalar2=None, op0=ALU.mult)))(),
                              reads=[py[half].b, G.b], writes=[accb[ta]], partial=(half == 1))
                    else:
                        stt("dve", accm.t[:, ta, hs], py[half].t[:, :], G.t[:, tg, e_:e_ + 1], accm.t[:, ta, hs],
                            ALU.mult, ALU.add, [py[half].b, G.b, accb[ta]], [accb[ta]])

        def finalize(hf_, tas):
            sst = {}
            for ta in tas:
                tg = hf_ * 8 + ta
                tsl = slice(tg * 128, (tg + 1) * 128)
                pb2 = [PS.next(), PS.next()]
                ss = ss4_ring.next()
                memset("pool", ss.t[:, :], 0.0, [ss.b])
                for half in range(2):
                    hs = slice(half * 512, (half + 1) * 512)
                    mm(pb2[half].t[:, :], GT.t[:, tsl], bdn.t[:, hs], True, True, [GT.b, bdn.b], [pb2[half].b])
                    tt("dve", accm.t[:, ta, hs], accm.t[:, ta, hs], pb2[half].t[:, :], ALU.add,
                       [accb[ta], pb2[half].b], [accb[ta]])
                act(junk4.t[:, :], accm.t[:, ta, :], AF.Square, [accb[ta]], [junk4.b, ss.b], accum=ss.t[:, 0:1])
                sst[ta] = ss
            for ta in tas:
                ss = sst[ta]
                rstd_(ss.t[:, 0:1], ss.t[:, 0:1], 1.0 / 1024.0, [ss.b])
            for ta in tas:
                tg = hf_ * 8 + ta
                tsl = slice(tg * 128, (tg + 1) * 128)
                ss = sst[ta]
                stt("dve", accm.t[:, ta, :], accm.t[:, ta, :], ss.t[:, 0:1], gg2b.t[:, :], ALU.mult, ALU.mult,
                    [accb[ta], ss.b, gg2b.b], [accb[ta]])
                x1b = x1b_ring.next()
                dma("sp", x1b.t[:, :], out[tsl, :], [B_out[tg]], [x1b.b], x1b.name)
                tt("pool", accm.t[:, ta, :], accm.t[:, ta, :], x1b.t[:, :], ALU.add, [accb[ta], x1b.b], [accb[ta]])
                dma("sp", out[tsl, :], accm.t[:, ta, :], [accb[ta]], [B_out[tg]], "out_f")

        cur = cur0
        pre = None
        for si, (hf_, e_) in enumerate(seq):
            wgA, wgB, wd = cur
            have_next = si + 1 < len(seq)
            ne = seq[si + 1][1] if have_next else None
            nxtA = load_gu(ne, 0) if have_next else None
            nxtB = None
            nxtD = load_d(ne) if have_next else None
            for tb in range(2):
                tok0 = hf_ * 1024 + tb * 512
                if pre is None:
                    aT = actT_ring.next()
                    fb0 = 0
                else:
                    aT = pre
                    fb0 = NSK
                for fb in range(fb0, 8):
                    gu_fb(e_, tok0, fb, wgA, wgB, aT)
                if tb == 1 and have_next:
                    nxtB = load_gu(ne, 1)
                pre = None
                if tb == 0:
                    pre = actT_ring.next()
                    for fb in range(NSK):
                        gu_fb(e_, hf_ * 1024 + 512, fb, wgA, wgB, pre)
                elif have_next:
                    pre = actT_ring.next()
                    for fb in range(NSK):
                        gu_fb(ne, seq[si + 1][0] * 1024, fb, nxtA, None, pre)
                down(hf_, e_, tb, aT, wd)
                if e_ == 31:
                    finalize(hf_, range(tb * 4, tb * 4 + 4))
            cur = [nxtA, nxtB, nxtD]
        A.free(p4)
    A.free(p34)

    S.dead = False
    S.add("sp", lambda e: e.nop(), reads=B_out)

    S.finalize()
    sems = {e: es.enter_context(nc.semaphore("s_" + e)) for e in ENGS}
    dsems = {}
    for i, k in enumerate(sorted(S.dma_cum.keys())):
        dsems[k] = es.enter_context(nc.semaphore("d%d" % i))
    block = es.enter_context(nc.Block())
    S.emit(block, sems, dsems)
    es.close()
    build.info = dict(bmarks=getattr(S, "bmarks", []), marks=[m["pe"] for m in S.marks], peak=A.peak, nops=len(S.ops), ndsem=len(dsems),
                      per_eng={e: len(S.streams[e]) for e in ENGS},
                      flagged={e: sum(1 for o in S.streams[e] if o.flag) for e in ENGS})
    return nc, S


def make_in_maps(inputs, stage="full"):
    x = np.asarray(inputs["x"], np.float32)
    c = np.asarray(inputs["c"], np.float32)
    cf, cb = _consts()
    shared = {
        "cst_f32": cf, "cst_bf": cb,
        "w_mod": np.ascontiguousarray(inputs["w_mod"][0], np.float32),
        "b_mod": np.ascontiguousarray(inputs["b_mod"][0], np.float32),
        "g_pre_mix": np.ascontiguousarray(inputs["g_pre_mix"][0], np.float32),
        "g_post_mix": np.ascontiguousarray(inputs["g_post_mix"][0], np.float32),
        "g_pre_ffn": np.ascontiguousarray(inputs["g_pre_ffn"][0], np.float32),
        "g_post_ffn": np.ascontiguousarray(inputs["g_post_ffn"][0], np.float32),
        "w_in": np.ascontiguousarray(inputs["w_in"][0], np.float32),
        "w_gate_lr": np.ascontiguousarray(inputs["w_gate_lr"][0], np.float32),
        "b_gate": np.ascontiguousarray(inputs["b_gate"][0], np.float32),
        "g_gla": np.ascontiguousarray(inputs["g_gla"][0], np.float32),
        "w_out": np.ascontiguousarray(inputs["w_out"][0], np.float32),
    }
    if stage == "full":
        shared.update({
            "router_w": np.ascontiguousarray(inputs["router_w"][0], np.float32),
            "router_b": np.ascontiguousarray(inputs["router_b"][0], np.float32),
            "w_gate_up": np.ascontiguousarray(inputs["w_gate_up"][0], np.float32),
            "b_gate_up": np.ascontiguousarray(inputs["b_gate_up"][0], np.float32),
            "w_down": np.ascontiguousarray(inputs["w_down"][0], np.float32),
            "b_down": np.ascontiguousarray(inputs["b_down"][0], np.float32),
        })
    maps = []
    for core in range(NCORES):
        b, j = core // 4, core % 4
        end = (j + 1) * 2048
        start = end - 8192
        xeh = np.zeros((8192, 1024), np.float32)
        lo = max(start, 0)
        xeh[lo - start:, :] = x[b, lo:end, :]
        tvh = np.zeros((128, 64), np.float32)
        for t in range(64):
            if start + t * 128 >= 0:
                tvh[:, t] = 1.0
        m = dict(shared)
        m["xe"] = xeh
        m["tilevalid"] = tvh
        m["cvec"] = np.ascontiguousarray(c[b].reshape(8, 128).T)
        m["cs_tab"] = _rope_tables(end)
        maps.append(m)
    return maps


_CACHE = {}


def kernel(**inputs):
    if "nc" not in _CACHE:
        _CACHE["nc"] = build("full")[0]
    nc = _CACHE["nc"]
    maps = make_in_maps(inputs, "full")
    res = run_bass_kernel_spmd(nc, maps, core_ids=list(range(NCORES)))
    outp = np.zeros((2, 8192, 1024), np.float32)
    for core in range(NCORES):
        b, j = core // 4, core % 4
        outp[b, j * 2048:(j + 1) * 2048, :] = np.asarray(res.results[core]["out"], np.float32)
    return outp
```

```python
import numpy as np
import ml_dtypes
from contextlib import ExitStack
import concourse.bass as bass
import concourse.mybir as mybir
from concourse.bass_utils import run_bass_kernel_spmd

F32 = mybir.dt.float32
BF16 = mybir.dt.bfloat16
ALU = mybir.AluOpType
AF = mybir.ActivationFunctionType

ENGS = ("pe", "act", "dve", "pool", "sp")
EPS = 1e-6
NCORES = 8


class Buf:
    __slots__ = ("name", "w", "r", "war", "excl")

    def __init__(self, name):
        self.name = name
        self.w = {}
        self.r = {}
        self.war = []
        self.excl = False


class Op:
    __slots__ = ("eng", "fn", "dma", "pos", "deps", "flag", "cnt", "semkey", "cum")

    def __init__(self, eng, fn, dma, semkey):
        self.eng = eng
        self.fn = fn
        self.dma = dma
        self.semkey = semkey
        self.deps = []
        self.flag = False
        self.cnt = 0
        self.cum = 0
        self.pos = 0


class Sched:
    def __init__(self):
        self.ops = []
        self.streams = {e: [] for e in ENGS}
        self.dma_cum = {}
        self.last_dma = {}
        self.pending_bar = {}
        self.dead = False

    def _key(self, op):
        return ("dma", op.semkey) if op.dma else op.eng

    def add(self, eng, fn, reads=(), writes=(), dma=False, semkey=None, partial=False):
        if self.dead:
            return None
        op = Op(eng, fn, dma, semkey)
        op.pos = len(self.streams[eng])
        self.streams[eng].append(op)
        self.ops.append(op)
        if dma:
            c = self.dma_cum.get(semkey, 0) + 16
            self.dma_cum[semkey] = c
            op.cum = c
            self.last_dma[semkey] = op
        deps = []
        pb = self.pending_bar.pop(eng, None)
        if pb:
            deps.extend(pb)
        for b in reads:
            deps.extend(b.w.values())
            if b.excl:
                deps.extend(o for o in b.r.values() if o.eng != eng)
        for b in writes:
            if b.r:
                b.war = list(b.r.values()) + list(b.w.values())
                deps.extend(b.war)
            elif partial:
                deps.extend(b.war)
            else:
                deps.extend(b.w.values())
        op.deps = deps
        k = self._key(op)
        for b in reads:
            b.r[k] = op
        for b in writes:
            if b.r or not partial:
                if not b.r:
                    b.war = []
                b.w = {}
                b.r = {}
            b.w[k] = op
        return op

    def barrier(self):
        self.marks = getattr(self, "marks", [])
        self.marks.append({e: len(self.streams[e]) for e in ENGS})
        deps = []
        for e in ENGS:
            if self.streams[e]:
                last = None
                for o in reversed(self.streams[e]):
                    if not o.dma:
                        last = o
                        break
                if last is not None:
                    deps.append(last)
        deps.extend(self.last_dma.values())
        for e in ENGS:
            self.pending_bar[e] = list(deps)

    def finalize(self):
        self.waits = {}
        seen = {e: {} for e in ENGS}
        for op in self.ops:
            E = op.eng
            sv = seen[E]
            need = {}
            for d in op.deps:
                if d is op:
                    continue
                if d.dma:
                    key = ("dma", d.semkey)
                    val = d.cum
                else:
                    if d.eng == E and E in ("pe", "sp"):
                        continue
                    key = d.eng
                    val = d.pos + 1
                if sv.get(key, 0) >= val:
                    continue
                if need.get(key, (0, None))[0] < val:
                    need[key] = (val, d)
            wl = []
            for key, (val, d) in need.items():
                sv[key] = val
                if not d.dma:
                    d.flag = True
                wl.append(d)
            self.waits[id(op)] = wl
        for e in ENGS:
            c = 0
            for op in self.streams[e]:
                if not op.dma and op.flag:
                    c += 1
                    op.cnt = c

    def emit(self, block, sems, dsems):
        def run(ename, e):
            for op in self.streams[ename]:
                for d in self.waits[id(op)]:
                    if d.dma:
                        e.wait_ge(dsems[d.semkey], d.cum)
                    else:
                        e.wait_ge(sems[d.eng], d.cnt)
                ins = op.fn(e)
                if op.dma:
                    ins.then_inc(dsems[op.semkey], 16)
                elif op.flag:
                    ins.then_inc(sems[ename], 1)

        @block.sync
        def _(e):
            run("sp", e)

        @block.scalar
        def _(e):
            run("act", e)

        @block.vector
        def _(e):
            run("dve", e)

        @block.gpsimd
        def _(e):
            run("pool", e)

        @block.tensor
        def _(e):
            run("pe", e)


class TT:
    def __init__(self, t, name):
        self.t = t
        self.b = Buf(name)
        self.name = name


class Arena:
    def __init__(self, big, nbytes):
        self.big = big
        self.n = nbytes
        self.live = []
        self.peak = 0

    def alloc(self, name, shape, dt):
        esz = 4 if dt == F32 else 2
        fb = esz
        for d in shape[1:]:
            fb *= d
        size = (fb + 63) // 64 * 64
        off = 0
        for (o, sz, _) in sorted(self.live):
            if off + size <= o:
                break
            off = max(off, o + sz)
        assert off + size <= self.n, "SBUF arena overflow allocating %s (%d B); live=%d" % (
            name, size, sum(x[1] for x in self.live))
        self.live.append((off, size, name))
        self.peak = max(self.peak, off + size)
        ap = self.big[0:shape[0], off // 2:(off + fb) // 2]
        if dt == F32:
            ap = ap.bitcast(F32)
        if len(shape) == 3:
            ap = ap.rearrange("p (a b) -> p a b", a=shape[1])
        elif len(shape) == 4:
            ap = ap.rearrange("p (a b c) -> p a b c", a=shape[1], b=shape[2])
        t = TT(ap, name)
        t.off = off
        return t

    def free(self, tts):
        offs = set(t.off for t in tts)
        self.live = [x for x in self.live if x[0] not in offs]


class Ring:
    def __init__(self, items):
        self.items = items
        self.i = 0

    def next(self):
        it = self.items[self.i % len(self.items)]
        self.i += 1
        return it


def _consts():
    idx = np.arange(128)
    U = (idx[:, None] <= idx[None, :]).astype(np.float32)
    UT = U.T.copy()
    ident = np.eye(128, dtype=np.float32)
    Ls = (idx[:, None] > idx[None, :]).astype(np.float32)
    negdiag = (-0.125 * ident).astype(np.float32)
    ones = np.ones((128, 128), np.float32)
    sw = (idx // 64) * 64 + ((idx % 64) + 32) % 64
    Psw = np.zeros((128, 128), np.float32)
    Psw[sw, idx] = 1.0
    cf = np.concatenate([ident, U, Ls, negdiag, ones, Psw], axis=1)
    mask4 = np.concatenate([U, UT, U, UT], axis=1)
    negsel = np.zeros((128, 128), np.float32)
    negsel[0:64, 0] = -0.125
    negsel[64:128, 1] = -0.125
    cb = np.concatenate([ident, ones, mask4, negsel], axis=1).astype(ml_dtypes.bfloat16)
    return cf, cb


def _rope_tables(end):
    pos = (end - 4096 + np.arange(4096)).astype(np.float32)
    half = 32
    inv = (np.float32(10000.0) ** (-(np.arange(half, dtype=np.float32) / np.float32(half)))).astype(np.float32)
    ang = pos[:, None] * inv[None, :]
    c = np.cos(ang).astype(np.float32).T
    s = np.sin(ang).astype(np.float32).T
    cos_t = np.tile(c, (4, 1))
    sgn = np.where((np.arange(128) % 64) < 32, -1.0, 1.0).astype(np.float32)
    sin_t = np.tile(s, (4, 1)) * sgn[:, None]
    return np.stack([cos_t, sin_t], axis=0).astype(np.float32)


SBUF_BYTES = 206 * 1024


def build(stage="full"):
    nc = bass.Bass("TRN2", target_bir_lowering=False)
    S = Sched()

    def din(name, shape, dt=F32):
        return nc.dram_tensor(name, list(shape), dt, kind="ExternalInput").ap()

    xe = din("xe", [8192, 1024])
    tv_d = din("tilevalid", [128, 64])
    cvec = din("cvec", [128, 8])
    cs_tab = din("cs_tab", [2, 128, 4096])
    cf_d = din("cst_f32", [128, 768])
    cb_d = din("cst_bf", [128, 896], BF16)
    w_mod = din("w_mod", [1024, 6144])
    b_mod = din("b_mod", [6144])
    g_pre_mix = din("g_pre_mix", [1024])
    g_post_mix = din("g_post_mix", [1024])
    g_pre_ffn = din("g_pre_ffn", [1024])
    g_post_ffn = din("g_post_ffn", [1024])
    w_in = din("w_in", [1024, 3088])
    w_gate_lr = din("w_gate_lr", [16, 256])
    b_gate = din("b_gate", [256])
    g_gla = din("g_gla", [128])
    w_out = din("w_out", [1024, 1024])
    if stage == "full":
        router_w = din("router_w", [1024, 32])
        router_b = din("router_b", [32])
        w_gate_up = din("w_gate_up", [32, 1024, 2048])
        b_gate_up = din("b_gate_up", [32, 2048])
        w_down = din("w_down", [32, 1024, 1024])
        b_down = din("b_down", [32, 1024])
    out = nc.dram_tensor("out", [2048, 1024], F32, kind="ExternalOutput").ap()
    vscr = nc.dram_tensor("vscr", [4096, 512], BF16, kind="Internal").ap()
    ogscr = nc.dram_tensor("ogscr", [4, 128, 2048], BF16, kind="Internal").ap()
    modscr = nc.dram_tensor("modscr", [4, 128, 1024], F32, kind="Internal").ap()
    B_vscr = Buf("vscr")
    B_ogscr = Buf("ogscr")
    B_modscr = Buf("modscr")
    B_out = [Buf("out%d" % i) for i in range(16)]

    es = ExitStack()
    big = es.enter_context(nc.sbuf_tensor("big", [128, SBUF_BYTES // 2], BF16))
    A = Arena(big, SBUF_BYTES)

    def sb(scope, name, shape, dt):
        t = A.alloc(name, list(shape), dt)
        if scope is not None:
            scope.append(t)
        return t

    def ring(scope, name, shape, dt, n):
        return Ring([sb(scope, "%s%d" % (name, i), shape, dt) for i in range(n)])

    def dma(q, out_ap, in_ap, reads, writes, key, partial=False):
        S.add(q, lambda e: e.dma_start(out=out_ap, in_=in_ap), reads=reads, writes=writes,
              dma=True, semkey=key, partial=partial)

    def mm(out_ap, lhsT, rhs, start, stop, reads, writes):
        S.add("pe", lambda e: e.matmul(out_ap, lhsT=lhsT, rhs=rhs, start=start, stop=stop),
              reads=reads, writes=writes)

    def tr(out_ap, in_ap, ident_ap, reads, writes):
        S.add("pe", lambda e: e.transpose(out_ap, in_ap, ident_ap), reads=reads, writes=writes)

    def act(out_ap, in_ap, func, reads, writes, bias=None, scale=None, accum=None):
        kw = {}
        if bias is not None:
            kw["bias"] = bias
        if scale is not None:
            kw["scale"] = scale
        if accum is not None:
            kw["accum_out"] = accum
        S.add("act", lambda e: e.activation(out=out_ap, in_=in_ap, func=func, **kw), reads=reads, writes=writes)

    def tt(eng, out_ap, a, b, op, reads, writes):
        S.add(eng, lambda e: e.tensor_tensor(out=out_ap, in0=a, in1=b, op=op), reads=reads, writes=writes)

    def ts(eng, out_ap, a, s1, s2, op0, op1, reads, writes):
        if op1 is None:
            S.add(eng, lambda e: e.tensor_scalar(out=out_ap, in0=a, scalar1=s1, scalar2=None, op0=op0),
                  reads=reads, writes=writes)
        else:
            S.add(eng, lambda e: e.tensor_scalar(out=out_ap, in0=a, scalar1=s1, scalar2=s2, op0=op0, op1=op1),
                  reads=reads, writes=writes)

    def stt(eng, out_ap, a, sc, b, op0, op1, reads, writes):
        S.add(eng, lambda e: e.scalar_tensor_tensor(out=out_ap, in0=a, scalar=sc, in1=b, op0=op0, op1=op1),
              reads=reads, writes=writes)

    def cp(eng, out_ap, in_ap, reads, writes):
        if eng == "act":
            S.add("act", lambda e: e.copy(out=out_ap, in_=in_ap), reads=reads, writes=writes)
        else:
            S.add(eng, lambda e: e.tensor_copy(out=out_ap, in_=in_ap), reads=reads, writes=writes)

    def recip(out_ap, in_ap, reads, writes):
        S.add("dve", lambda e: e.reciprocal(out=out_ap, in_=in_ap), reads=reads, writes=writes)

    def ttr(out_ap, a, b, accum, reads, writes):
        S.add("dve", lambda e: e.tensor_tensor_reduce(out=out_ap, in0=a, in1=b, scale=1.0, scalar=0.0,
                                                      op0=ALU.mult, op1=ALU.add, accum_out=accum),
              reads=reads, writes=writes)

    def rstd_(ap_out, ap_in, inv_n, bufs):
        act(ap_out, ap_in, AF.Ln, bufs + [epsc.b], bufs, bias=epsc.t[:, 0:1], scale=inv_n)
        act(ap_out, ap_out, AF.Exp, bufs, bufs, scale=-0.5)

    def memset(eng, ap, val, writes):
        S.add(eng, lambda e: e.memset(ap, val), writes=writes)

    psb = []
    for i in range(8):
        t = es.enter_context(nc.psum_tensor("psb%d" % i, [128, 512], F32))
        psb.append(TT(t, "psb%d" % i))
        psb[-1].b.excl = True
    PS = Ring(psb)

    def bfview(p):
        return p.t[:, :].bitcast(BF16)

    cf = sb(None, "cf", [128, 768], F32)
    cbt = sb(None, "cb", [128, 896], BF16)
    epsc = sb(None, "epsc", [128, 1], F32)
    memset("pool", epsc.t[:, :], EPS, [epsc.b])
    c14 = sb(None, "c14", [128, 1], F32)
    memset("pool", c14.t[:, :], 14.0, [c14.b])
    tv = sb(None, "tv", [128, 64], F32)
    dma("sp", cf.t[:, :], cf_d, [], [cf.b], "cf")
    dma("sp", cbt.t[:, :], cb_d, [], [cbt.b], "cb")
    dma("sp", tv.t[:, :], tv_d, [], [tv.b], "tv")
    ident_f = cf.t[:, 0:128]
    U_f = cf.t[:, 128:256]
    Ls_f = cf.t[:, 256:384]
    negdiag = cf.t[:, 384:512]
    ones_f = cf.t[:, 512:640]
    Psw_f = cf.t[:, 640:768]
    ident_b = cbt.t[:, 0:128]
    ones_b = cbt.t[:, 128:256]
    mask4 = cbt.t[:, 256:768]
    negsel_b = cbt.t[:, 768:770]
    mT = sb(None, "mT", [128, 512], BF16)
    cp("pool", mT.t[:, 0:384], cbt.t[:, 384:768], [cbt.b], [mT.b])
    cp("pool", mT.t[:, 384:512], cbt.t[:, 256:384], [cbt.b, mT.b], [mT.b])
    mask4h = sb(None, "mask4h", [128, 512], BF16)
    cp("pool", mask4h.t[:, :], mT.t[:, :], [mT.b], [mask4h.b])
    hv = tv.t[:, 32:33]
    for off in (0, 256):
        ts("dve", mask4h.t[:, off:off + 128], mask4h.t[:, off:off + 128], hv, None, ALU.mult, None,
           [mask4h.b, tv.b], [mask4h.b])

    mix = []
    qT = sb(mix, "qT", [128, 4, 2048], BF16)
    kT = sb(mix, "kT", [128, 4, 4096], BF16)

    p1 = []
    gmod1 = sb(p1, "gmod1", [128, 1024], F32)
    shift1 = sb(p1, "shift1", [128, 1024], F32)
    p0 = []
    modb = sb(p0, "modb", [128, 6144], F32)
    bmodb = sb(p0, "bmodb", [128, 6144], F32)
    gb = [sb(p0, "gb%d" % i, [128, 1024], F32) for i in range(4)]
    cv = sb(p0, "cv", [128, 8], F32)
    ecv = sb(p0, "ecv", [128, 8], F32)
    sc = sb(p0, "sc", [128, 8], F32)
    scb = sb(p0, "scb", [128, 8, 128], BF16)
    wm = ring(p0, "wm", [128, 8, 512], BF16, 2)
    tmp = ring(p0, "mtmp", [128, 1024], F32, 3)
    dma("sp", cv.t[:, :], cvec, [], [cv.b], "cv")
    dma("sp", bmodb.t[:, :], b_mod.partition_broadcast(128), [], [bmodb.b], "bmodb")
    for i, g in enumerate((g_pre_mix, g_post_mix, g_pre_ffn, g_post_ffn)):
        dma("sp", gb[i].t[:, :], g.partition_broadcast(128), [], [gb[i].b], "gb%d" % i)
    act(ecv.t[:, :], cv.t[:, :], AF.Exp, [cv.b], [ecv.b], scale=-1.0)
    ts("dve", ecv.t[:, :], ecv.t[:, :], 1.0, None, ALU.add, None, [ecv.b], [ecv.b])
    recip(ecv.t[:, :], ecv.t[:, :], [ecv.b], [ecv.b])
    tt("dve", sc.t[:, :], cv.t[:, :], ecv.t[:, :], ALU.mult, [cv.b, ecv.b], [sc.b])
    cp("dve", scb.t[:, :, :], sc.t[:, :].unsqueeze(2).to_broadcast([128, 8, 128]), [sc.b], [scb.b])
    wmv = w_mod.rearrange("(kc p) n -> p kc n", p=128)
    for blk in range(12):
        w = wm.next()
        dma("pool", w.t[:, :, :], wmv[:, :, blk * 512:(blk + 1) * 512], [], [w.b], w.name)
        ps = PS.next()
        for kc in range(8):
            mm(ps.t[:, :], scb.t[:, kc, :], w.t[:, kc, :], kc == 0, kc == 7, [scb.b, w.b], [ps.b])
        tt("dve", modb.t[:, blk * 512:(blk + 1) * 512], ps.t[:, :], bmodb.t[:, blk * 512:(blk + 1) * 512],
           ALU.add, [ps.b, bmodb.b], [modb.b])
    sl = lambda i: modb.t[:, i * 1024:(i + 1) * 1024]
    cp("pool", shift1.t[:, :], sl(0), [modb.b], [shift1.b])
    stt("dve", gmod1.t[:, :], sl(1), 1.0, gb[0].t[:, :], ALU.add, ALU.mult, [modb.b, gb[0].b], [gmod1.b])
    t0 = tmp.next()
    tt("dve", t0.t[:, :], sl(2), gb[1].t[:, :], ALU.mult, [modb.b, gb[1].b], [t0.b])
    dma("sp", modscr[0], t0.t[:, :], [t0.b], [B_modscr], "modscr", partial=True)
    dma("sp", modscr[1], sl(3), [modb.b], [B_modscr], "modscr", partial=True)
    t1 = tmp.next()
    stt("dve", t1.t[:, :], sl(4), 1.0, gb[2].t[:, :], ALU.add, ALU.mult, [modb.b, gb[2].b], [t1.b])
    dma("sp", modscr[2], t1.t[:, :], [t1.b], [B_modscr], "modscr", partial=True)
    t2 = tmp.next()
    tt("dve", t2.t[:, :], sl(5), gb[3].t[:, :], ALU.mult, [modb.b, gb[3].b], [t2.b])
    dma("sp", modscr[3], t2.t[:, :], [t2.b], [B_modscr], "modscr", partial=True)
    S.barrier()
    if stage == "p0":
        dma("sp", out[0:128, :], gmod1.t[:, :], [gmod1.b], [B_out[0]], "dbg0")
        dma("sp", out[128:256, :], shift1.t[:, :], [shift1.b], [B_out[1]], "dbg1")
        S.dead = True
    A.free(p0)

    w_in_sb = sb(p1, "w_in", [128, 8, 3088], BF16)
    winv = w_in.rearrange("(kc p) n -> p kc n", p=128)
    for kc in range(8):
        dma("pool", w_in_sb.t[:, kc, :], winv[:, kc, :], [], [w_in_sb.b], "w_in", partial=True)
    wg17 = sb(p1, "wg17", [32, 256], F32)
    memset("pool", wg17.t[:, :], 0.0, [wg17.b])
    dma("sp", wg17.t[0:16, :], w_gate_lr, [], [wg17.b], "wg17")
    dma("sp", wg17.t[16:17, :], b_gate.rearrange("(o n) -> o n", o=1), [], [wg17.b], "wg17b")
    gglab = sb(p1, "gglab", [128, 128], F32)
    dma("sp", gglab.t[:, :], g_gla.partition_broadcast(128), [], [gglab.b], "gglab")

    Sf = sb(p1, "Sf", [128, 2, 128], F32)
    Sb = sb(p1, "Sb", [128, 2, 128], BF16)
    memset("pool", Sf.t[:, :, :], 0.0, [Sf.b])
    memset("pool", Sb.t[:, :, :], 0.0, [Sb.b])

    rn = {}
    rn["x_ring"] = ring(p1, "xt", [128, 1024], F32, 2)
    rn["junk"] = sb(p1, "junk_a", [128, 1024], BF16)
    rn["ss_ring"] = ring(p1, "ss", [128, 2], F32, 4)
    rn["hb_ring"] = ring(p1, "hb", [128, 1024], BF16, 2)
    hT_ring = ring(p1, "hT", [128, 8, 512], BF16, 2)
    cs_ring = ring(p1, "csr", [128, 2, 512], F32, 1)
    qs_ring = ring(p1, "qsb", [128, 512], F32, 2)
    rp_ring = ring(p1, "rp", [128, 512], F32, 2)
    vt_ring = ring(p1, "vt", [128, 512], BF16, 2)
    glr_ring = ring(p1, "glr", [32, 512], F32, 2)
    for g in glr_ring.items:
        memset("pool", g.t[:, :], 1.0, [g.b])
    e1_ring = ring(p1, "e1", [128, 256], F32, 2)
    sp_ring = ring(p1, "spr", [128, 256], F32, 2)
    dec_ring = ring(p1, "dec", [128, 256], F32, 2)
    ku_ring = ring(p1, "ku", [128, 256], BF16, 2)
    vg_ring = ring(p1, "vg", [128, 512], BF16, 2)
    acol_ring = ring(p1, "acol", [128, 2], F32, 2)
    eb_ring = ring(p1, "eb", [128, 256], F32, 2)
    enb_ring = ring(p1, "enb", [128, 256], F32, 2)
    qd_ring = ring(p1, "qd", [128, 2, 128], BF16, 2)
    kd_ring = ring(p1, "kd", [128, 2, 128], BF16, 2)
    attm_ring = ring(p1, "attm", [128, 4, 128], BF16, 2)
    er_ring = ring(p1, "er", [128, 512], F32, 1)
    rg_ring = ring(p1, "rg", [128, 512], F32, 2)
    ssg_ring = ring(p1, "ssg", [128, 4], F32, 2)
    on_ring = ring(p1, "on", [128, 512], F32, 1)
    og_ring = ring(p1, "og", [128, 512], BF16, 2)
    ogT_ring = ring(p1, "ogT", [128, 4, 128], BF16, 2)
    junk_s = sb(p1, "junk_s", [128, 128], BF16)

    def proj_fm(col0, ncols, hTb, tsl, ps, pcols):
        for kc in range(8):
            mm(ps.t[0:ncols, pcols], w_in_sb.t[:, kc, col0:col0 + ncols], hTb.t[:, kc, tsl], kc == 0, kc == 7,
               [w_in_sb.b, hTb.b], [ps.b])

    def proj_tm(ti, col0, ncols, hTb, ps):
        for kc in range(8):
            mm(ps.t[:, 0:ncols], hTb.t[:, kc, ti * 128:(ti + 1) * 128], w_in_sb.t[:, kc, col0:col0 + ncols],
               kc == 0, kc == 7, [w_in_sb.b, hTb.b], [ps.b])

    def rmsnorm_mod_T(xt, gm, sh, dstT, dst_cols, defer=False):
        ss = rn["ss_ring"].next()
        junk = rn["junk"]
        memset("pool", ss.t[:, :], 0.0, [ss.b])
        act(junk.t[:, :], xt.t[:, :], AF.Square, [xt.b], [junk.b, ss.b], accum=ss.t[:, 0:1])
        rstd_(ss.t[:, 1:2], ss.t[:, 0:1], 1.0 / 1024.0, [ss.b])
        stt("dve", xt.t[:, :], xt.t[:, :], ss.t[:, 1:2], gm.t[:, :], ALU.mult, ALU.mult, [xt.b, ss.b, gm.b], [xt.b])
        hb = rn["hb_ring"].next()
        tt("dve", hb.t[:, :], xt.t[:, :], sh.t[:, :], ALU.add, [xt.b, sh.b], [hb.b])
        if defer:
            return hb
        rmsnorm_T2(hb, dstT, dst_cols)

    def rmsnorm_T2(hb, dstT, dst_cols):
        ps = PS.next()
        pv = bfview(ps)
        for kc in range(8):
            tr(pv[:, kc * 128:(kc + 1) * 128], hb.t[:, kc * 128:(kc + 1) * 128], ident_b, [hb.b, cbt.b], [ps.b])
        cp("act", dstT.t[:, :, dst_cols], pv.rearrange("p (a b) -> p a b", a=8), [ps.b], [dstT.b])

    def cut(name):
        if stage == name and not S.dead:
            S.barrier()
            dma("sp", out[0:128, 0:768], cf.t[:, :], [cf.b], [B_out[0]], "dbgc")
            S.dead = True

    pending = [None]

    def back(ctx):
        acol, ku, vg = ctx["acol"], ctx["ku"], ctx["vg"]
        pu = PU.next()
        for h in range(4):
            g, hh = h // 2, h % 2
            mm(pu.t[hh * 64:(hh + 1) * 64, g * 128:(g + 1) * 128], ku.t[:, h * 64:(h + 1) * 64],
               vg.t[:, h * 128:(h + 1) * 128], True, True, [ku.b, vg.b], [pu.b])
        if ctx["kind"] == "own":
            attm, qd, rg, m0, ti = ctx["attm"], ctx["qd"], ctx["rg"], ctx["m0"], ctx["ti"]
            po = PS.next()
            for h in range(4):
                g, hh = h // 2, h % 2
                mm(po.t[:, h * 128:(h + 1) * 128], attm.t[:, h, :], vg.t[:, h * 128:(h + 1) * 128],
                   True, False, [attm.b, vg.b], [po.b])
                mm(po.t[:, h * 128:(h + 1) * 128], qd.t[hh * 64:(hh + 1) * 64, g, :],
                   Sb.t[hh * 64:(hh + 1) * 64, g, :], False, True, [qd.b, Sb.b], [po.b])
            ssg = ssg_ring.next()
            memset("pool", ssg.t[:, :], 0.0, [ssg.b])
            for h in range(4):
                act(junk_s.t[:, :], po.t[:, h * 128:(h + 1) * 128], AF.Square, [po.b], [junk_s.b, ssg.b],
                    accum=ssg.t[:, h:h + 1])
            rstd_(ssg.t[:, :], ssg.t[:, :], 1.0 / 128.0, [ssg.b])
            on = on_ring.next()
            tt("dve", on.t[:, :].rearrange("p (a b) -> p a b", a=4), po.t[:, :].rearrange("p (a b) -> p a b", a=4),
               ssg.t[:, :].unsqueeze(2).to_broadcast([128, 4, 128]), ALU.mult, [po.b, ssg.b], [on.b])
            og = og_ring.next()
            tt("pool", og.t[:, :], on.t[:, :], rg.t[:, :], ALU.mult, [on.b, rg.b], [og.b])
            pt2 = PS.next()
            pv2 = bfview(pt2)
            for h in range(4):
                tr(pv2[:, h * 128:(h + 1) * 128], og.t[:, h * 128:(h + 1) * 128], ident_b, [og.b, cbt.b], [pt2.b])
            ogT = ogT_ring.next()
            cp("act", ogT.t[:, :, :], pv2[:, 0:512].rearrange("p (a b) -> p a b", a=4), [pt2.b], [ogT.b])
            dma("sp", ogscr[:, :, m0 + ti * 128:m0 + (ti + 1) * 128].rearrange("h e t -> e h t"), ogT.t[:, :, :],
                [ogT.b], [B_ogscr], ogT.name, partial=True)
        for g in range(2):
            stt("dve", Sf.t[:, g, :], Sf.t[:, g, :], acol.t[:, g:g + 1], pu.t[:, g * 128:(g + 1) * 128],
                ALU.mult, ALU.add, [Sf.b, acol.b, pu.b], [Sf.b])
        cp("pool", Sb.t[:, :, :], Sf.t[:, :, :], [Sf.b], [Sb.b])

    hTbs = {}

    pro_pending = [None]

    def prologue_flush():
        if pro_pending[0] is not None:
            hb_, tb2, ti2 = pro_pending[0]
            rmsnorm_T2(hb_, hTbs[tb2], slice(ti2 * 128, (ti2 + 1) * 128))
            pro_pending[0] = None

    def prologue_tile(tb_, ti_, immediate=False):
        t_ = tb_ * 4 + ti_
        xt = rn["x_ring"].next()
        dma("sp", xt.t[:, :], xe[t_ * 128:(t_ + 1) * 128, :], [], [xt.b], xt.name)
        hb_ = rmsnorm_mod_T(xt, gmod1, shift1, None, None, defer=True)
        prologue_flush()
        pro_pending[0] = (hb_, tb_, ti_)
        if immediate:
            prologue_flush()

    full512 = slice(0, 512)
    PU = Ring(psb[6:8])
    PS.items = psb[0:6]
    for tb in range(16):
        kind = "prefix" if tb < 8 else ("halo" if tb < 12 else "own")
        S.bmarks = getattr(S, "bmarks", []) + [len(S.streams["pe"])]
        if tb == 0:
            hTbs[0] = hT_ring.next()
            for ti in range(4):
                prologue_tile(0, ti, immediate=(ti == 3))
        hTb = hTbs[tb]
        if tb + 1 < 16:
            hTbs[tb + 1] = hT_ring.next()
        cut("c_norm")
        n0 = (tb - 8) * 512
        m0 = (tb - 12) * 512
        if kind != "prefix":
            csr = cs_ring.next()
            dma("sp", csr.t[:, :, :], cs_tab[:, :, n0:n0 + 512].rearrange("a p n -> p a n"), [], [csr.b], csr.name)
            jobs = [(512, kT, n0)]
            if kind == "own":
                jobs.append((0, qT, m0))
            for (cbase, dst, d0) in jobs:
                for hp in range(4):
                    pa = PS.next()
                    proj_fm(cbase + hp * 128, 128, hTb, full512, pa, full512)
                    qs = qs_ring.next()
                    cp("act", qs.t[:, :], pa.t[:, :], [pa.b], [qs.b])
                    pb_ = PS.next()
                    mm(pb_.t[:, :], Psw_f, qs.t[:, :], True, True, [cf.b, qs.b], [pb_.b])
                    r1 = rp_ring.next()
                    r2 = rp_ring.next()
                    tt("pool", r1.t[:, :], qs.t[:, :], csr.t[:, 0, :], ALU.mult, [qs.b, csr.b], [r1.b])
                    tt("dve", r2.t[:, :], pb_.t[:, :], csr.t[:, 1, :], ALU.mult, [pb_.b, csr.b], [r2.b])
                    tt("pool", dst.t[:, hp, d0:d0 + 512], r1.t[:, :], r2.t[:, :], ALU.add, [r1.b, r2.b], [dst.b])
        if tb == 8:
            cut("c_rope")
        glr = glr_ring.next()
        pg = PS.next()
        proj_fm(3072, 16, hTb, full512, pg, full512)
        cp("act", glr.t[0:16, :], pg.t[0:16, :], [pg.b], [glr.b])
        for ti in range(4):
            t = tb * 4 + ti
            tsl = slice(ti * 128, (ti + 1) * 128)
            if kind != "prefix":
                pvv = PS.next()
                proj_tm(ti, 1024, 512, hTb, pvv)
                vt = vt_ring.next()
                cp("act", vt.t[:, :], pvv.t[:, :], [pvv.b], [vt.b])
                dma("sp", vscr[n0 + ti * 128:n0 + (ti + 1) * 128, :], vt.t[:, :], [vt.b], [B_vscr], vt.name,
                    partial=True)
            pgl = PS.next()
            mm(pgl.t[:, 0:256], glr.t[0:17, tsl], wg17.t[0:17, :], True, True, [glr.b, wg17.b], [pgl.b])
            e1 = e1_ring.next()
            act(e1.t[:, :], pgl.t[:, 0:256], AF.Exp, [pgl.b], [e1.b], scale=-1.0)
            spt = sp_ring.next()
            act(spt.t[:, :], e1.t[:, :], AF.Ln, [e1.b], [spt.b], bias=1.0)
            pk = PS.next()
            proj_tm(ti, 1792, 256, hTb, pk)
            pvg = PS.next()
            proj_tm(ti, 2048, 512, hTb, pvg)
            vg = vg_ring.next()
            cp("act", vg.t[:, :], pvg.t[:, :], [pvg.b], [vg.b])
            paf = PS.next()
            mm(paf.t[:, 0:256], Ls_f, spt.t[:, :], True, True, [cf.b, spt.b], [paf.b])
            dec = dec_ring.next()
            act(dec.t[:, :], paf.t[:, 0:256], AF.Exp, [paf.b], [dec.b], scale=-1.0 / 16.0)
            ku = ku_ring.next()
            stt("dve", ku.t[:, :], pk.t[:, 0:256], tv.t[:, t:t + 1], dec.t[:, :], ALU.mult, ALU.mult,
                [pk.b, tv.b, dec.b], [ku.b])
            pa_ = PS.next()
            for g in range(2):
                mm(pa_.t[:, g:g + 1], spt.t[:, g * 128:(g + 1) * 128], ones_f[:, 0:1], True, True,
                   [spt.b, cf.b], [pa_.b])
            acol = acol_ring.next()
            act(acol.t[:, :], pa_.t[:, 0:2], AF.Exp, [pa_.b], [acol.b], scale=-1.0 / 16.0)
            if kind == "own":
                pcs = PS.next()
                for g in range(2):
                    mm(pcs.t[:, g * 128:(g + 1) * 128], spt.t[:, g * 128:(g + 1) * 128], U_f, True, True,
                       [spt.b, cf.b], [pcs.b])
                eb = eb_ring.next()
                enb = enb_ring.next()
                act(eb.t[:, :], pcs.t[:, 0:256], AF.Exp, [pcs.b], [eb.b], scale=-1.0 / 16.0)
                act(enb.t[:, :], pcs.t[:, 0:256], AF.Exp, [pcs.b], [enb.b], scale=1.0 / 16.0)
                pqk = PS.next()
                for g in range(2):
                    proj_fm(1536 + g * 128, 128, hTb, tsl, pqk, slice(g * 128, (g + 1) * 128))
                    proj_fm(1792 + g * 128, 128, hTb, tsl, pqk, slice(256 + g * 128, 256 + (g + 1) * 128))
                qd = qd_ring.next()
                kd = kd_ring.next()
                stt("dve", qd.t[:, :, :], pqk.t[:, 0:256].rearrange("p (a b) -> p a b", a=2), 0.125,
                    eb.t[:, :].rearrange("p (a b) -> p a b", a=2), ALU.mult, ALU.mult, [pqk.b, eb.b], [qd.b])
                tt("dve", kd.t[:, :, :], pqk.t[:, 256:512].rearrange("p (a b) -> p a b", a=2),
                   enb.t[:, :].rearrange("p (a b) -> p a b", a=2), ALU.mult, [pqk.b, enb.b], [kd.b])
                pr = PS.next()
                proj_tm(ti, 2560, 512, hTb, pr)
                er = er_ring.next()
                act(er.t[:, :], pr.t[:, :], AF.Exp, [pr.b], [er.b], scale=-1.0)
                rg = rg_ring.next()
                tt("dve", rg.t[:, :].rearrange("p (a b) -> p a b", a=4), pr.t[:, :].rearrange("p (a b) -> p a b", a=4),
                   gglab.t[:, :].unsqueeze(1).to_broadcast([128, 4, 128]), ALU.mult, [pr.b, gglab.b], [rg.b])
                ts("dve", er.t[:, :], er.t[:, :], 1.0, None, ALU.add, None, [er.b], [er.b])
                recip(er.t[:, :], er.t[:, :], [er.b], [er.b])
                tt("pool", rg.t[:, :], rg.t[:, :], er.t[:, :], ALU.mult, [rg.b, er.b], [rg.b])
                patt = [PS.next(), PS.next()]
                for h in range(4):
                    g, hh = h // 2, h % 2
                    mm(patt[hh].t[:, g * 128:(g + 1) * 128], kd.t[hh * 64:(hh + 1) * 64, g, :],
                       qd.t[hh * 64:(hh + 1) * 64, g, :], True, True, [kd.b, qd.b], [patt[hh].b])
                attm = attm_ring.next()
                for hh in range(2):
                    tt("dve", attm.t[:, hh:4:2, :], patt[hh].t[:, 0:256].rearrange("p (a b) -> p a b", a=2),
                       U_f.unsqueeze(1).to_broadcast([128, 2, 128]), ALU.mult, [patt[hh].b, cf.b], [attm.b])
            ctx = dict(kind=kind, acol=acol, ku=ku, vg=vg)
            if kind == "own":
                ctx.update(attm=attm, qd=qd, rg=rg, m0=m0, ti=ti)
            if pending[0] is not None:
                back(pending[0])
            pending[0] = ctx
            if tb + 1 < 16:
                prologue_tile(tb + 1, ti, immediate=(ti == 3))
    back(pending[0])
    PS.items = psb
    S.barrier()
    if stage == "p1":
        dbgt = sb(None, "dbgt", [128, 1024], F32)
        for i in range(4):
            cp("dve", dbgt.t[:, :], kT.t[:, i, 2048:3072], [kT.b], [dbgt.b])
            dma("sp", out[i * 128:(i + 1) * 128, :], dbgt.t[:, :], [dbgt.b], [B_out[i]], "dbg0")
        for i in range(4):
            cp("dve", dbgt.t[:, :], qT.t[:, i, 0:1024], [qT.b], [dbgt.b])
            dma("sp", out[(4 + i) * 128:(5 + i) * 128, :], dbgt.t[:, :], [dbgt.b], [B_out[4 + i]], "dbg0")
        S.dead = True
    A.free(p1)

    p23 = []
    catT = sb(p23, "catT", [128, 8, 2048], BF16)
    w_out_sb = sb(p23, "w_out", [128, 8, 1024], BF16)
    woutv = w_out.rearrange("(kc p) n -> p kc n", p=128)
    for kc in range(8):
        dma("pool", w_out_sb.t[:, kc, :], woutv[:, kc, :], [], [w_out_sb.b], "w_out", partial=True)
    for h in range(4):
        dma("sp", catT.t[:, 4 + h, :], ogscr[h], [B_ogscr], [catT.b], "catT_ld", partial=True)
    pa = []
    vp_ring = ring(pa, "vp", [128, 3, 32, 128], BF16, 2)
    acc = sb(pa, "acc", [128, 2, 2048], F32)
    nb_ring = ring(pa, "nb", [128, 2], F32, 4)
    prod = sb(pa, "prod", [128, 2048], BF16)
    P_ring = ring(pa, "P", [128, 512], BF16, 4)
    PT_ring = ring(pa, "PT", [128, 512], BF16, 4)
    rl = sb(pa, "rl", [128, 2048], F32)
    PATS = ((0, 1), (1, 4), (2, 16))
    vps = {}

    def load_vp(hp_):
        vp_ = vp_ring.next()
        for (pi, d) in PATS:
            src = vscr[:, hp_ * 128:(hp_ + 1) * 128].rearrange("(u d) c -> d u c", d=d)
            ntile = 32 // d
            for r in range(d):
                dma("sp", vp_.t[:, pi, r * ntile:(r + 1) * ntile, :],
                    src[r].rearrange("(kt p) c -> p kt c", p=128), [B_vscr], [vp_.b], vp_.name, partial=True)
        vps[hp_] = vp_

    def mk_prod(hp_):
        tt("pool", prod.t[:, :], qT.t[:, hp_, :], kT.t[:, hp_, 2048:4096], ALU.mult, [qT.b, kT.b], [prod.b])

    load_vp(0)
    load_vp(1)
    mk_prod(0)
    for hp in range(4):
        vp = vps[hp]
        if 1 <= hp < 3:
            load_vp(hp + 1)
        memset("pool", acc.t[:, :, :], 0.0, [acc.b])
        units = []
        for (pi, d) in PATS:
            ntile = 32 // d
            for r in range(d):
                for kt in range(16 // d, ntile):
                    units.append((pi, d, r, kt))

        def stageA(u):
            pi, d, r, kt = u
            ntile = 32 // d
            st_ = {}
            q0 = r + d * kt * 128 - 2048
            k0 = r + d * (kt - 1) * 128
            qs = slice(q0, q0 + d * 127 + 1, d)
            ks = slice(k0, k0 + d * 255 + 1, d)
            pS = [PS.next(), PS.next()]
            for hh in range(2):
                mm(pS[hh].t[:, 0:256], qT.t[hh * 64:(hh + 1) * 64, hp, qs],
                   kT.t[hh * 64:(hh + 1) * 64, hp, ks], True, True, [qT.b, kT.b], [pS[hh].b])
            mm(pS[0].t[:, 256:258], prod.t[:, qs], negsel_b, True, True, [prod.b, cbt.b], [pS[0].b])
            nb = nb_ring.next()
            cp("dve", nb.t[:, 0:2], pS[0].t[:, 256:258], [pS[0].b], [nb.b])
            Pt = P_ring.next()
            for hh in range(2):
                act(Pt.t[:, hh * 256:(hh + 1) * 256], pS[hh].t[:, 0:256], AF.Exp,
                    [pS[hh].b, nb.b], [Pt.b], bias=nb.t[:, hh:hh + 1], scale=0.125)
            st_.update(Pt=Pt, qs=qs, vt0=r * ntile + kt - 1, pi=pi, first=(kt == 16 // d))
            return st_

        def stageB(st_):
            Pt = st_["Pt"]
            pT = PS.next()
            pTv = bfview(pT)
            for i in range(4):
                tr(pTv[:, i * 128:(i + 1) * 128], Pt.t[:, i * 128:(i + 1) * 128], ident_b,
                   [Pt.b, cbt.b], [pT.b])
            PT = PT_ring.next()
            mk = mask4h if st_["first"] else mT
            tt("dve", PT.t[:, :], pTv[:, 0:512], mk.t[:, :], ALU.mult, [pT.b, mk.b], [PT.b])
            st_["PT"] = PT

        def stageC(st_):
            PT, qs, vt0, pi = st_["PT"], st_["qs"], st_["vt0"], st_["pi"]
            pO = PS.next()
            for hh in range(2):
                osl = pO.t[hh * 64:(hh + 1) * 64, 0:128]
                lsl = pO.t[hh * 64:(hh + 1) * 64, 128:256]
                for kk in range(2):
                    mm(osl, vp.t[:, pi, vt0 + kk, hh * 64:(hh + 1) * 64],
                       PT.t[:, (hh * 2 + kk) * 128:(hh * 2 + kk + 1) * 128], kk == 0, kk == 1,
                       [vp.b, PT.b], [pO.b])
                for kk in range(2):
                    mm(lsl, ones_b[:, 0:64], PT.t[:, (hh * 2 + kk) * 128:(hh * 2 + kk + 1) * 128],
                       kk == 0, kk == 1, [cbt.b, PT.b], [pO.b])
            tt("dve", acc.t[:, :, qs], acc.t[:, :, qs], pO.t[:, 0:256].rearrange("p (a b) -> p a b", a=2),
               ALU.add, [acc.b, pO.b], [acc.b])

        sts = []
        nu = len(units)
        for it in range(nu + 2):
            if it < nu:
                sts.append(stageA(units[it]))
            if it == nu - 1 and hp + 1 < 4:
                mk_prod(hp + 1)
            if 0 <= it - 1 < nu:
                stageB(sts[it - 1])
            if 0 <= it - 2 < nu:
                stageC(sts[it - 2])
        recip(rl.t[:, :], acc.t[:, 1, :], [acc.b], [rl.b])
        tt("pool", catT.t[:, hp, :], acc.t[:, 0, :], rl.t[:, :], ALU.mult, [acc.b, rl.b], [catT.b])
    S.barrier()
    if stage == "p2":
        dbgt = sb(None, "dbgt", [128, 1024], F32)
        for i in range(8):
            cp("dve", dbgt.t[:, :], catT.t[:, i, 0:1024], [catT.b], [dbgt.b])
            dma("sp", out[i * 128:(i + 1) * 128, :], dbgt.t[:, :], [dbgt.b], [B_out[i]], "dbg0")
        S.dead = True
    A.free(pa)
    A.free(mix)

    p34 = []
    if stage == "full":
        h2T = sb(p34, "h2T", [128, 8, 2048], BF16)
        G = sb(p34, "G", [128, 16, 32], F32)
        GT = sb(p34, "GT", [32, 2048], BF16)
        wgu_ring = Ring([sb(p34, "wgu%d" % i, [128, 8, 1024], BF16) for i in range(2)])
        wd_ring = Ring([sb(p34, "wd0", [128, 8, 1024], BF16)])
        wguv = w_gate_up.rearrange("e (kc p) n -> e p kc n", p=128)
        wdv = w_down.rearrange("e (kc p) n -> e p kc n", p=128)

        def load_gu(e_, hx):
            wg = wgu_ring.next()
            for kc in range(0, 8, 2):
                dma("pool", wg.t[:, kc:kc + 2, :], wguv[e_, :, kc:kc + 2, hx * 1024:(hx + 1) * 1024], [], [wg.b],
                    wg.name, partial=True)
            return wg

        def load_d(e_):
            wd = wd_ring.next()
            for kc in range(0, 8, 2):
                dma("pool", wd.t[:, kc:kc + 2, :], wdv[e_, :, kc:kc + 2, :], [], [wd.b], wd.name, partial=True)
            return wd

        cur0 = [load_gu(0, 0), load_gu(0, 1), load_d(0)]
        rw = sb(p23, "rw", [128, 8, 32], BF16)
        rbb = sb(p23, "rbb", [128, 32], F32)
        dma("pool", rw.t[:, :, :], router_w.rearrange("(kc p) n -> p kc n", p=128), [], [rw.b], "rw")
        dma("sp", rbb.t[:, :], router_b.partition_broadcast(128), [], [rbb.b], "rbb")
        lg_ring = ring(p23, "lg", [128, 32], F32, 2)
        m8_ring = ring(p23, "m8", [128, 8], F32, 2)
        ex_ring = ring(p23, "ex", [128, 32], F32, 2)
        msk_ring = ring(p23, "msk", [128, 32], F32, 2)
        den_ring = ring(p23, "den", [128, 2], F32, 2)
        gtb_ring = ring(p23, "gtb", [128, 32], BF16, 2)
    mods = [sb(p23, "mods%d" % i, [128, 1024], F32) for i in range(3)]
    for i in range(3):
        dma("sp", mods[i].t[:, :], modscr[i], [B_modscr], [mods[i].b], "mods%d" % i)
    gg1, shift2, gmod2 = mods
    rn["x_ring"] = ring(p23, "xt3", [128, 1024], F32, 2)
    rn["junk"] = sb(p23, "junk_a3", [128, 1024], BF16)
    rn["ss_ring"] = ring(p23, "ss3", [128, 2], F32, 4)
    rn["hb_ring"] = ring(p23, "hb3", [128, 1024], BF16, 2)
    yt_ring = ring(p23, "yt", [128, 1024], F32, 2)
    x1_ring = ring(p23, "x1", [128, 1024], F32, 2)
    junk3 = rn["junk"]

    st3 = {}

    def p3A(ti):
        tsl = slice(ti * 128, (ti + 1) * 128)
        py = [PS.next(), PS.next()]
        for half in range(2):
            for kc in range(8):
                mm(py[half].t[:, :], catT.t[:, kc, tsl], w_out_sb.t[:, kc, half * 512:(half + 1) * 512],
                   kc == 0, kc == 7, [catT.b, w_out_sb.b], [py[half].b])
        ss = rn["ss_ring"].next()
        memset("pool", ss.t[:, :], 0.0, [ss.b])
        for half in range(2):
            act(junk3.t[:, 0:512], py[half].t[:, :], AF.Square, [py[half].b], [junk3.b, ss.b],
                accum=ss.t[:, half:half + 1])
        tt("dve", ss.t[:, 0:1], ss.t[:, 0:1], ss.t[:, 1:2], ALU.add, [ss.b], [ss.b])
        rstd_(ss.t[:, 0:1], ss.t[:, 0:1], 1.0 / 1024.0, [ss.b])
        yt = yt_ring.next()
        for half in range(2):
            hs = slice(half * 512, (half + 1) * 512)
            stt("dve", yt.t[:, hs], py[half].t[:, :], ss.t[:, 0:1], gg1.t[:, hs], ALU.mult, ALU.mult,
                [py[half].b, ss.b, gg1.b], [yt.b])
        xt = rn["x_ring"].next()
        dma("sp", xt.t[:, :], xe[(48 + ti) * 128:(49 + ti) * 128, :], [], [xt.b], xt.name)
        x1 = x1_ring.next()
        tt("pool", x1.t[:, :], xt.t[:, :], yt.t[:, :], ALU.add, [xt.b, yt.b], [x1.b])
        dma("sp", out[tsl, :], x1.t[:, :], [x1.b], [B_out[ti]], "out_w%d" % (ti % 4))
        st3[ti] = dict(x1=x1)

    def p3B1(ti):
        st3[ti]["hb"] = rmsnorm_mod_T(st3[ti]["x1"], gmod2, shift2, None, None, defer=True)

    def p3B2(ti):
        tsl = slice(ti * 128, (ti + 1) * 128)
        rmsnorm_T2(st3[ti]["hb"], h2T, tsl)

    def p3C(ti):
        tsl = slice(ti * 128, (ti + 1) * 128)
        pl = PS.next()
        for kc in range(8):
            mm(pl.t[:, 0:32], h2T.t[:, kc, tsl], rw.t[:, kc, :], kc == 0, kc == 7, [h2T.b, rw.b], [pl.b])
        lg = lg_ring.next()
        tt("dve", lg.t[:, :], pl.t[:, 0:32], rbb.t[:, :], ALU.add, [pl.b, rbb.b], [lg.b])
        m8 = m8_ring.next()
        S.add("dve", (lambda m8=m8, lg=lg: (lambda e: e.max(out=m8.t[:, :], in_=lg.t[:, :])))(),
              reads=[lg.b], writes=[m8.b])
        msk = msk_ring.next()
        ts("dve", msk.t[:, :], lg.t[:, :], m8.t[:, 3:4], None, ALU.is_ge, None, [lg.b, m8.b], [msk.b])
        den = den_ring.next()
        memset("pool", den.t[:, :], 0.0, [den.b])
        ts("dve", den.t[:, 0:1], m8.t[:, 0:1], -1.0, None, ALU.mult, None, [m8.b], [den.b])
        ex = ex_ring.next()
        act(ex.t[:, :], lg.t[:, :], AF.Exp, [lg.b, den.b], [ex.b], bias=den.t[:, 0:1], scale=1.0)
        tt("dve", ex.t[:, :], ex.t[:, :], msk.t[:, :], ALU.mult, [ex.b, msk.b], [ex.b])
        S.add("dve", (lambda ex=ex, den=den: (lambda e: e.reduce_sum(out=den.t[:, 1:2], in_=ex.t[:, :],
                                                                   axis=mybir.AxisListType.X)))(),
              reads=[ex.b], writes=[den.b])
        recip(den.t[:, 1:2], den.t[:, 1:2], [den.b], [den.b])
        ts("dve", G.t[:, ti, :], ex.t[:, :], den.t[:, 1:2], None, ALU.mult, None, [ex.b, den.b], [G.b])
        gtb = gtb_ring.next()
        cp("pool", gtb.t[:, :], G.t[:, ti, :], [G.b], [gtb.b])
        st3[ti]["gtb"] = gtb

    def p3D(ti):
        tsl = slice(ti * 128, (ti + 1) * 128)
        gtb = st3[ti]["gtb"]
        pgt = PS.next()
        pgv = bfview(pgt)
        tr(pgv[0:32, 0:128], gtb.t[:, :], ident_b, [gtb.b, cbt.b], [pgt.b])
        cp("act", GT.t[:, tsl], pgv[0:32, 0:128], [pgt.b], [GT.b])

    for it in range(16 + 4):
        if it < 16:
            p3A(it)
        if stage == "full":
            if 0 <= it - 1 < 16:
                p3B1(it - 1)
            if 0 <= it - 2 < 16:
                p3B2(it - 2)
            if 0 <= it - 3 < 16:
                p3C(it - 3)
            if 0 <= it - 4 < 16:
                p3D(it - 4)
    S.barrier()
    A.free(p23)

    if stage == "full":
        p4 = []
        gg2b = sb(p4, "gg2b", [128, 1024], F32)
        dma("sp", gg2b.t[:, :], modscr[3], [B_modscr], [gg2b.b], "gg2b")
        bdn = sb(p4, "bdn", [32, 1024], BF16)
        dma("pool", bdn.t[:, :], b_down, [], [bdn.b], "bdn")
        bgu = sb(p4, "bgu", [128, 16, 32], F32)
        pb = []
        bgs = sb(pb, "bgs", [32, 2048], F32)
        dma("sp", bgs.t[:, :], b_gate_up, [], [bgs.b], "bgs")
        bview = bgs.t[:, :].rearrange("e (fb p s) -> e fb s p", fb=8, s=2)
        for fb in range(8):
            for s_ in range(2):
                pbt = PS.next()
                tr(pbt.t[:, 0:32], bview[:, fb, s_, :], ident_f[0:32, 0:32], [bgs.b, cf.b], [pbt.b])
                cp("act", bgu.t[:, fb * 2 + s_, :], pbt.t[:, 0:32], [pbt.b], [bgu.b])
        bl7 = sb(p4, "bl7", [128, 8, 32], F32)
        ts("dve", bl7.t[:, :, :], bgu.t[:, 1:16:2, :], -1.0, 7.0, ALU.mult, ALU.add, [bgu.b], [bl7.b])
        S.barrier()
        A.free(pb)
        accm = sb(p4, "accm", [128, 8, 1024], F32)
        accb = [Buf("accm%d" % i) for i in range(8)]
        wgu_ring.items = wgu_ring.items + [sb(p4, "wgu2", [128, 8, 1024], BF16)]
        wd_ring.items = wd_ring.items + [sb(p4, "wd1", [128, 8, 1024], BF16)]
        actT_ring = ring(p4, "actT", [128, 8, 512], BF16, 2)
        g_ring = ring(p4, "g_", [128, 512], F32, 2)
        sg_ring = ring(p4, "sg_", [128, 512], F32, 2)
        l_ring = ring(p4, "l_", [128, 512], F32, 2)
        x1b_ring = ring(p4, "x1b", [128, 1024], F32, 2)
        ss4_ring = ring(p4, "ss4", [128, 2], F32, 4)
        junk4 = sb(p4, "junk4", [128, 1024], BF16)
        seq = [(hf_, e_) for hf_ in range(2) for e_ in range(32)]
        NSK = 2

        def gu_fb(e_, tok0, fb, wgA, wgB, aT):
            wg = wgA if fb < 4 else wgB
            fl = fb % 4
            pG = PS.next()
            pL = PS.next()
            for kc in range(8):
                mm(pG.t[:, :], wg.t[:, kc, fl * 256:(fl + 1) * 256:2], h2T.t[:, kc, tok0:tok0 + 512],
                   kc == 0, kc == 7, [wg.b, h2T.b], [pG.b])
            for kc in range(8):
                mm(pL.t[:, :], wg.t[:, kc, fl * 256 + 1:(fl + 1) * 256:2], h2T.t[:, kc, tok0:tok0 + 512],
                   kc == 0, kc == 7, [wg.b, h2T.b], [pL.b])
            g_ = g_ring.next()
            ts("dve", g_.t[:, :], pG.t[:, :], bgu.t[:, fb * 2, e_:e_ + 1], 7.0, ALU.add, ALU.min,
               [pG.b, bgu.b], [g_.b])
            sg = sg_ring.next()
            act(sg.t[:, :], g_.t[:, :], AF.Sigmoid, [g_.b], [sg.b], scale=1.702)
            l_ = l_ring.next()
            act(l_.t[:, :], pL.t[:, :], AF.Relu, [pL.b, bl7.b], [l_.b], bias=bl7.t[:, fb, e_:e_ + 1], scale=-1.0)
            act(l_.t[:, :], l_.t[:, :], AF.Relu, [l_.b, c14.b], [l_.b], bias=c14.t[:, 0:1], scale=-1.0)
            tt("dve", g_.t[:, :], g_.t[:, :], sg.t[:, :], ALU.mult, [g_.b, sg.b], [g_.b])
            stt("dve", aT.t[:, fb, :], l_.t[:, :], -6.0, g_.t[:, :], ALU.add, ALU.mult, [g_.b, l_.b], [aT.b])

        def down(hf_, e_, tb, aT, wd):
            for tl in range(4):
                tg = hf_ * 8 + tb * 4 + tl
                ta = tb * 4 + tl
                py = [PS.next(), PS.next()]
                for half in range(2):
                    for fb in range(8):
                        mm(py[half].t[:, :], aT.t[:, fb, tl * 128:(tl + 1) * 128],
                           wd.t[:, fb, half * 512:(half + 1) * 512], fb == 0, fb == 7, [aT.b, wd.b], [py[half].b])
                for half in range(2):
                    hs = slice(half * 512, (half + 1) * 512)
                    if e_ == 0:
                        S.add("dve", (lambda o=accm.t[:, ta, hs], a=py[half].t[:, :], g=G.t[:, tg, e_:e_ + 1]:
                                      (lambda e: e.tensor_scalar(out=o, in0=a, scalar1=g, scalar2=None, op0=ALU.mult)))(),
                              reads=[py[half].b, G.b], writes=[accb[ta]], partial=(half == 1))
                    else:
                        stt("dve", accm.t[:, ta, hs], py[half].t[:, :], G.t[:, tg, e_:e_ + 1], accm.t[:, ta, hs],
                            ALU.mult, ALU.add, [py[half].b, G.b, accb[ta]], [accb[ta]])

        def finalize(hf_, tas):
            sst = {}
            for ta in tas:
                tg = hf_ * 8 + ta
                tsl = slice(tg * 128, (tg + 1) * 128)
                pb2 = [PS.next(), PS.next()]
                ss = ss4_ring.next()
                memset("pool", ss.t[:, :], 0.0, [ss.b])
                for half in range(2):
                    hs = slice(half * 512, (half + 1) * 512)
                    mm(pb2[half].t[:, :], GT.t[:, tsl], bdn.t[:, hs], True, True, [GT.b, bdn.b], [pb2[half].b])
                    tt("dve", accm.t[:, ta, hs], accm.t[:, ta, hs], pb2[half].t[:, :], ALU.add,
                       [accb[ta], pb2[half].b], [accb[ta]])
                act(junk4.t[:, :], accm.t[:, ta, :], AF.Square, [accb[ta]], [junk4.b, ss.b], accum=ss.t[:, 0:1])
                sst[ta] = ss
            for ta in tas:
                ss = sst[ta]
                rstd_(ss.t[:, 0:1], ss.t[:, 0:1], 1.0 / 1024.0, [ss.b])
            for ta in tas:
                tg = hf_ * 8 + ta
                tsl = slice(tg * 128, (tg + 1) * 128)
                ss = sst[ta]
                stt("dve", accm.t[:, ta, :], accm.t[:, ta, :], ss.t[:, 0:1], gg2b.t[:, :], ALU.mult, ALU.mult,
                    [accb[ta], ss.b, gg2b.b], [accb[ta]])
                x1b = x1b_ring.next()
                dma("sp", x1b.t[:, :], out[tsl, :], [B_out[tg]], [x1b.b], x1b.name)
                tt("pool", accm.t[:, ta, :], accm.t[:, ta, :], x1b.t[:, :], ALU.add, [accb[ta], x1b.b], [accb[ta]])
                dma("sp", out[tsl, :], accm.t[:, ta, :], [accb[ta]], [B_out[tg]], "out_f")

        cur = cur0
        pre = None
        for si, (hf_, e_) in enumerate(seq):
            wgA, wgB, wd = cur
            have_next = si + 1 < len(seq)
            ne = seq[si + 1][1] if have_next else None
            nxtA = load_gu(ne, 0) if have_next else None
            nxtB = None
            nxtD = load_d(ne) if have_next else None
            for tb in range(2):
                tok0 = hf_ * 1024 + tb * 512
                if pre is None:
                    aT = actT_ring.next()
                    fb0 = 0
                else:
                    aT = pre
                    fb0 = NSK
                for fb in range(fb0, 8):
                    gu_fb(e_, tok0, fb, wgA, wgB, aT)
                if tb == 1 and have_next:
                    nxtB = load_gu(ne, 1)
                pre = None
                if tb == 0:
                    pre = actT_ring.next()
                    for fb in range(NSK):
                        gu_fb(e_, hf_ * 1024 + 512, fb, wgA, wgB, pre)
                elif have_next:
                    pre = actT_ring.next()
                    for fb in range(NSK):
                        gu_fb(ne, seq[si + 1][0] * 1024, fb, nxtA, None, pre)
                down(hf_, e_, tb, aT, wd)
                if e_ == 31:
                    finalize(hf_, range(tb * 4, tb * 4 + 4))
            cur = [nxtA, nxtB, nxtD]
        A.free(p4)
    A.free(p34)

    S.dead = False
    S.add("sp", lambda e: e.nop(), reads=B_out)

    S.finalize()
    sems = {e: es.enter_context(nc.semaphore("s_" + e)) for e in ENGS}
    dsems = {}
    for i, k in enumerate(sorted(S.dma_cum.keys())):
        dsems[k] = es.enter_context(nc.semaphore("d%d" % i))
    block = es.enter_context(nc.Block())
    S.emit(block, sems, dsems)
    es.close()
    build.info = dict(bmarks=getattr(S, "bmarks", []), marks=[m["pe"] for m in S.marks], peak=A.peak, nops=len(S.ops), ndsem=len(dsems),
                      per_eng={e: len(S.streams[e]) for e in ENGS},
                      flagged={e: sum(1 for o in S.streams[e] if o.flag) for e in ENGS})
    return nc, S


def make_in_maps(inputs, stage="full"):
    x = np.asarray(inputs["x"], np.float32)
    c = np.asarray(inputs["c"], np.float32)
    cf, cb = _consts()
    shared = {
        "cst_f32": cf, "cst_bf": cb,
        "w_mod": np.ascontiguousarray(inputs["w_mod"][0], np.float32),
        "b_mod": np.ascontiguousarray(inputs["b_mod"][0], np.float32),
        "g_pre_mix": np.ascontiguousarray(inputs["g_pre_mix"][0], np.float32),
        "g_post_mix": np.ascontiguousarray(inputs["g_post_mix"][0], np.float32),
        "g_pre_ffn": np.ascontiguousarray(inputs["g_pre_ffn"][0], np.float32),
        "g_post_ffn": np.ascontiguousarray(inputs["g_post_ffn"][0], np.float32),
        "w_in": np.ascontiguousarray(inputs["w_in"][0], np.float32),
        "w_gate_lr": np.ascontiguousarray(inputs["w_gate_lr"][0], np.float32),
        "b_gate": np.ascontiguousarray(inputs["b_gate"][0], np.float32),
        "g_gla": np.ascontiguousarray(inputs["g_gla"][0], np.float32),
        "w_out": np.ascontiguousarray(inputs["w_out"][0], np.float32),
    }
    if stage == "full":
        shared.update({
            "router_w": np.ascontiguousarray(inputs["router_w"][0], np.float32),
            "router_b": np.ascontiguousarray(inputs["router_b"][0], np.float32),
            "w_gate_up": np.ascontiguousarray(inputs["w_gate_up"][0], np.float32),
            "b_gate_up": np.ascontiguousarray(inputs["b_gate_up"][0], np.float32),
            "w_down": np.ascontiguousarray(inputs["w_down"][0], np.float32),
            "b_down": np.ascontiguousarray(inputs["b_down"][0], np.float32),
        })
    maps = []
    for core in range(NCORES):
        b, j = core // 4, core % 4
        end = (j + 1) * 2048
        start = end - 8192
        xeh = np.zeros((8192, 1024), np.float32)
        lo = max(start, 0)
        xeh[lo - start:, :] = x[b, lo:end, :]
        tvh = np.zeros((128, 64), np.float32)
        for t in range(64):
            if start + t * 128 >= 0:
                tvh[:, t] = 1.0
        m = dict(shared)
        m["xe"] = xeh
        m["tilevalid"] = tvh
        m["cvec"] = np.ascontiguousarray(c[b].reshape(8, 128).T)
        m["cs_tab"] = _rope_tables(end)
        maps.append(m)
    return maps


_CACHE = {}


def kernel(**inputs):
    if "nc" not in _CACHE:
        _CACHE["nc"] = build("full")[0]
    nc = _CACHE["nc"]
    maps = make_in_maps(inputs, "full")
    res = run_bass_kernel_spmd(nc, maps, core_ids=list(range(NCORES)))
    outp = np.zeros((2, 8192, 1024), np.float32)
    for core in range(NCORES):
        b, j = core // 4, core % 4
        outp[b, j * 2048:(j + 1) * 2048, :] = np.asarray(res.results[core]["out"], np.float32)
    return outp
```

```python
import numpy as np
import ml_dtypes
from contextlib import ExitStack
import concourse.bass as bass
import concourse.mybir as mybir
from concourse.bass_utils import run_bass_kernel_spmd

F32 = mybir.dt.float32
BF16 = mybir.dt.bfloat16
ALU = mybir.AluOpType
AF = mybir.ActivationFunctionType

ENGS = ("pe", "act", "dve", "pool", "sp")
EPS = 1e-6
NCORES = 8


class Buf:
    __slots__ = ("name", "w", "r", "war", "excl")

    def __init__(self, name):
        self.name = name
        self.w = {}
        self.r = {}
        self.war = []
        self.excl = False


class Op:
    __slots__ = ("eng", "fn", "dma", "pos", "deps", "flag", "cnt", "semkey", "cum")

    def __init__(self, eng, fn, dma, semkey):
        self.eng = eng
        self.fn = fn
        self.dma = dma
        self.semkey = semkey
        self.deps = []
        self.flag = False
        self.cnt = 0
        self.cum = 0
        self.pos = 0


class Sched:
    def __init__(self):
        self.ops = []
        self.streams = {e: [] for e in ENGS}
        self.dma_cum = {}
        self.last_dma = {}
        self.pending_bar = {}
        self.dead = False

    def _key(self, op):
        return ("dma", op.semkey) if op.dma else op.eng

    def add(self, eng, fn, reads=(), writes=(), dma=False, semkey=None, partial=False):
        if self.dead:
            return None
        op = Op(eng, fn, dma, semkey)
        op.pos = len(self.streams[eng])
        self.streams[eng].append(op)
        self.ops.append(op)
        if dma:
            c = self.dma_cum.get(semkey, 0) + 16
            self.dma_cum[semkey] = c
            op.cum = c
            self.last_dma[semkey] = op
        deps = []
        pb = self.pending_bar.pop(eng, None)
        if pb:
            deps.extend(pb)
        for b in reads:
            deps.extend(b.w.values())
            if b.excl:
                deps.extend(o for o in b.r.values() if o.eng != eng)
        for b in writes:
            if b.r:
                b.war = list(b.r.values()) + list(b.w.values())
                deps.extend(b.war)
            elif partial:
                deps.extend(b.war)
            else:
                deps.extend(b.w.values())
        op.deps = deps
        k = self._key(op)
        for b in reads:
            b.r[k] = op
        for b in writes:
            if b.r or not partial:
                if not b.r:
                    b.war = []
                b.w = {}
                b.r = {}
            b.w[k] = op
        return op

    def barrier(self):
        self.marks = getattr(self, "marks", [])
        self.marks.append({e: len(self.streams[e]) for e in ENGS})
        deps = []
        for e in ENGS:
            if self.streams[e]:
                last = None
                for o in reversed(self.streams[e]):
                    if not o.dma:
                        last = o
                        break
                if last is not None:
                    deps.append(last)
        deps.extend(self.last_dma.values())
        for e in ENGS:
            self.pending_bar[e] = list(deps)

    def finalize(self):
        self.waits = {}
        seen = {e: {} for e in ENGS}
        for op in self.ops:
            E = op.eng
            sv = seen[E]
            need = {}
            for d in op.deps:
                if d is op:
                    continue
                if d.dma:
                    key = ("dma", d.semkey)
                    val = d.cum
                else:
                    if d.eng == E and E in ("pe", "sp"):
                        continue
                    key = d.eng
                    val = d.pos + 1
                if sv.get(key, 0) >= val:
                    continue
                if need.get(key, (0, None))[0] < val:
                    need[key] = (val, d)
            wl = []
            for key, (val, d) in need.items():
                sv[key] = val
                if not d.dma:
                    d.flag = True
                wl.append(d)
            self.waits[id(op)] = wl
        for e in ENGS:
            c = 0
            for op in self.streams[e]:
                if not op.dma and op.flag:
                    c += 1
                    op.cnt = c

    def emit(self, block, sems, dsems):
        def run(ename, e):
            for op in self.streams[ename]:
                for d in self.waits[id(op)]:
                    if d.dma:
                        e.wait_ge(dsems[d.semkey], d.cum)
                    else:
                        e.wait_ge(sems[d.eng], d.cnt)
                ins = op.fn(e)
                if op.dma:
                    ins.then_inc(dsems[op.semkey], 16)
                elif op.flag:
                    ins.then_inc(sems[ename], 1)

        @block.sync
        def _(e):
            run("sp", e)

        @block.scalar
        def _(e):
            run("act", e)

        @block.vector
        def _(e):
            run("dve", e)

        @block.gpsimd
        def _(e):
            run("pool", e)

        @block.tensor
        def _(e):
            run("pe", e)


class TT:
    def __init__(self, t, name):
        self.t = t
        self.b = Buf(name)
        self.name = name


class Arena:
    def __init__(self, big, nbytes):
        self.big = big
        self.n = nbytes
        self.live = []
        self.peak = 0

    def alloc(self, name, shape, dt):
        esz = 4 if dt == F32 else 2
        fb = esz
        for d in shape[1:]:
            fb *= d
        size = (fb + 63) // 64 * 64
        off = 0
        for (o, sz, _) in sorted(self.live):
            if off + size <= o:
                break
            off = max(off, o + sz)
        assert off + size <= self.n, "SBUF arena overflow allocating %s (%d B); live=%d" % (
            name, size, sum(x[1] for x in self.live))
        self.live.append((off, size, name))
        self.peak = max(self.peak, off + size)
        ap = self.big[0:shape[0], off // 2:(off + fb) // 2]
        if dt == F32:
            ap = ap.bitcast(F32)
        if len(shape) == 3:
            ap = ap.rearrange("p (a b) -> p a b", a=shape[1])
        elif len(shape) == 4:
            ap = ap.rearrange("p (a b c) -> p a b c", a=shape[1], b=shape[2])
        t = TT(ap, name)
        t.off = off
        return t

    def free(self, tts):
        offs = set(t.off for t in tts)
        self.live = [x for x in self.live if x[0] not in offs]


class Ring:
    def __init__(self, items):
        self.items = items
        self.i = 0

    def next(self):
        it = self.items[self.i % len(self.items)]
        self.i += 1
        return it


def _consts():
    idx = np.arange(128)
    U = (idx[:, None] <= idx[None, :]).astype(np.float32)
    UT = U.T.copy()
    ident = np.eye(128, dtype=np.float32)
    Ls = (idx[:, None] > idx[None, :]).astype(np.float32)
    negdiag = (-0.125 * ident).astype(np.float32)
    ones = np.ones((128, 128), np.float32)
    sw = (idx // 64) * 64 + ((idx % 64) + 32) % 64
    Psw = np.zeros((128, 128), np.float32)
    Psw[sw, idx] = 1.0
    cf = np.concatenate([ident, U, Ls, negdiag, ones, Psw], axis=1)
    mask4 = np.concatenate([U, UT, U, UT], axis=1)
    negsel = np.zeros((128, 128), np.float32)
    negsel[0:64, 0] = -0.125
    negsel[64:128, 1] = -0.125
    cb = np.concatenate([ident, ones, mask4, negsel], axis=1).astype(ml_dtypes.bfloat16)
    return cf, cb


def _rope_tables(end):
    pos = (end - 4096 + np.arange(4096)).astype(np.float32)
    half = 32
    inv = (np.float32(10000.0) ** (-(np.arange(half, dtype=np.float32) / np.float32(half)))).astype(np.float32)
    ang = pos[:, None] * inv[None, :]
    c = np.cos(ang).astype(np.float32).T
    s = np.sin(ang).astype(np.float32).T
    cos_t = np.tile(c, (4, 1))
    sgn = np.where((np.arange(128) % 64) < 32, -1.0, 1.0).astype(np.float32)
    sin_t = np.tile(s, (4, 1)) * sgn[:, None]
    return np.stack([cos_t, sin_t], axis=0).astype(np.float32)


SBUF_BYTES = 206 * 1024


def build(stage="full"):
    nc = bass.Bass("TRN2", target_bir_lowering=False)
    S = Sched()

    def din(name, shape, dt=F32):
        return nc.dram_tensor(name, list(shape), dt, kind="ExternalInput").ap()

    xe = din("xe", [8192, 1024])
    tv_d = din("tilevalid", [128, 64])
    cvec = din("cvec", [128, 8])
    cs_tab = din("cs_tab", [2, 128, 4096])
    cf_d = din("cst_f32", [128, 768])
    cb_d = din("cst_bf", [128, 896], BF16)
    w_mod = din("w_mod", [1024, 6144])
    b_mod = din("b_mod", [6144])
    g_pre_mix = din("g_pre_mix", [1024])
    g_post_mix = din("g_post_mix", [1024])
    g_pre_ffn = din("g_pre_ffn", [1024])
    g_post_ffn = din("g_post_ffn", [1024])
    w_in = din("w_in", [1024, 3088])
    w_gate_lr = din("w_gate_lr", [16, 256])
    b_gate = din("b_gate", [256])
    g_gla = din("g_gla", [128])
    w_out = din("w_out", [1024, 1024])
    if stage == "full":
        router_w = din("router_w", [1024, 32])
        router_b = din("router_b", [32])
        w_gate_up = din("w_gate_up", [32, 1024, 2048])
        b_gate_up = din("b_gate_up", [32, 2048])
        w_down = din("w_down", [32, 1024, 1024])
        b_down = din("b_down", [32, 1024])
    out = nc.dram_tensor("out", [2048, 1024], F32, kind="ExternalOutput").ap()
    vscr = nc.dram_tensor("vscr", [4096, 512], BF16, kind="Internal").ap()
    ogscr = nc.dram_tensor("ogscr", [4, 128, 2048], BF16, kind="Internal").ap()
    modscr = nc.dram_tensor("modscr", [4, 128, 1024], F32, kind="Internal").ap()
    B_vscr = Buf("vscr")
    B_ogscr = Buf("ogscr")
    B_modscr = Buf("modscr")
    B_out = [Buf("out%d" % i) for i in range(16)]

    es = ExitStack()
    big = es.enter_context(nc.sbuf_tensor("big", [128, SBUF_BYTES // 2], BF16))
    A = Arena(big, SBUF_BYTES)

    def sb(scope, name, shape, dt):
        t = A.alloc(name, list(shape), dt)
        if scope is not None:
            scope.append(t)
        return t

    def ring(scope, name, shape, dt, n):
        return Ring([sb(scope, "%s%d" % (name, i), shape, dt) for i in range(n)])

    def dma(q, out_ap, in_ap, reads, writes, key, partial=False):
        S.add(q, lambda e: e.dma_start(out=out_ap, in_=in_ap), reads=reads, writes=writes,
              dma=True, semkey=key, partial=partial)

    def mm(out_ap, lhsT, rhs, start, stop, reads, writes):
        S.add("pe", lambda e: e.matmul(out_ap, lhsT=lhsT, rhs=rhs, start=start, stop=stop),
              reads=reads, writes=writes)

    def tr(out_ap, in_ap, ident_ap, reads, writes):
        S.add("pe", lambda e: e.transpose(out_ap, in_ap, ident_ap), reads=reads, writes=writes)

    def act(out_ap, in_ap, func, reads, writes, bias=None, scale=None, accum=None):
        kw = {}
        if bias is not None:
            kw["bias"] = bias
        if scale is not None:
            kw["scale"] = scale
        if accum is not None:
            kw["accum_out"] = accum
        S.add("act", lambda e: e.activation(out=out_ap, in_=in_ap, func=func, **kw), reads=reads, writes=writes)

    def tt(eng, out_ap, a, b, op, reads, writes):
        S.add(eng, lambda e: e.tensor_tensor(out=out_ap, in0=a, in1=b, op=op), reads=reads, writes=writes)

    def ts(eng, out_ap, a, s1, s2, op0, op1, reads, writes):
        if op1 is None:
            S.add(eng, lambda e: e.tensor_scalar(out=out_ap, in0=a, scalar1=s1, scalar2=None, op0=op0),
                  reads=reads, writes=writes)
        else:
            S.add(eng, lambda e: e.tensor_scalar(out=out_ap, in0=a, scalar1=s1, scalar2=s2, op0=op0, op1=op1),
                  reads=reads, writes=writes)

    def stt(eng, out_ap, a, sc, b, op0, op1, reads, writes):
        S.add(eng, lambda e: e.scalar_tensor_tensor(out=out_ap, in0=a, scalar=sc, in1=b, op0=op0, op1=op1),
              reads=reads, writes=writes)

    def cp(eng, out_ap, in_ap, reads, writes):
        if eng == "act":
            S.add("act", lambda e: e.copy(out=out_ap, in_=in_ap), reads=reads, writes=writes)
        else:
            S.add(eng, lambda e: e.tensor_copy(out=out_ap, in_=in_ap), reads=reads, writes=writes)

    def recip(out_ap, in_ap, reads, writes):
        S.add("dve", lambda e: e.reciprocal(out=out_ap, in_=in_ap), reads=reads, writes=writes)

    def ttr(out_ap, a, b, accum, reads, writes):
        S.add("dve", lambda e: e.tensor_tensor_reduce(out=out_ap, in0=a, in1=b, scale=1.0, scalar=0.0,
                                                      op0=ALU.mult, op1=ALU.add, accum_out=accum),
              reads=reads, writes=writes)

    def rstd_(ap_out, ap_in, inv_n, bufs):
        act(ap_out, ap_in, AF.Ln, bufs + [epsc.b], bufs, bias=epsc.t[:, 0:1], scale=inv_n)
        act(ap_out, ap_out, AF.Exp, bufs, bufs, scale=-0.5)

    def memset(eng, ap, val, writes):
        S.add(eng, lambda e: e.memset(ap, val), writes=writes)

    psb = []
    for i in range(8):
        t = es.enter_context(nc.psum_tensor("psb%d" % i, [128, 512], F32))
        psb.append(TT(t, "psb%d" % i))
        psb[-1].b.excl = True
    PS = Ring(psb)

    def bfview(p):
        return p.t[:, :].bitcast(BF16)

    cf = sb(None, "cf", [128, 768], F32)
    cbt = sb(None, "cb", [128, 896], BF16)
    epsc = sb(None, "epsc", [128, 1], F32)
    memset("pool", epsc.t[:, :], EPS, [epsc.b])
    c14 = sb(None, "c14", [128, 1], F32)
    memset("pool", c14.t[:, :], 14.0, [c14.b])
    tv = sb(None, "tv", [128, 64], F32)
    dma("sp", cf.t[:, :], cf_d, [], [cf.b], "cf")
    dma("sp", cbt.t[:, :], cb_d, [], [cbt.b], "cb")
    dma("sp", tv.t[:, :], tv_d, [], [tv.b], "tv")
    ident_f = cf.t[:, 0:128]
    U_f = cf.t[:, 128:256]
    Ls_f = cf.t[:, 256:384]
    negdiag = cf.t[:, 384:512]
    ones_f = cf.t[:, 512:640]
    Psw_f = cf.t[:, 640:768]
    ident_b = cbt.t[:, 0:128]
    ones_b = cbt.t[:, 128:256]
    mask4 = cbt.t[:, 256:768]
    negsel_b = cbt.t[:, 768:770]
    mT = sb(None, "mT", [128, 512], BF16)
    cp("pool", mT.t[:, 0:384], cbt.t[:, 384:768], [cbt.b], [mT.b])
    cp("pool", mT.t[:, 384:512], cbt.t[:, 256:384], [cbt.b, mT.b], [mT.b])
    mask4h = sb(None, "mask4h", [128, 512], BF16)
    cp("pool", mask4h.t[:, :], mT.t[:, :], [mT.b], [mask4h.b])
    hv = tv.t[:, 32:33]
    for off in (0, 256):
        ts("dve", mask4h.t[:, off:off + 128], mask4h.t[:, off:off + 128], hv, None, ALU.mult, None,
           [mask4h.b, tv.b], [mask4h.b])

    mix = []
    qT = sb(mix, "qT", [128, 4, 2048], BF16)
    kT = sb(mix, "kT", [128, 4, 4096], BF16)

    p1 = []
    gmod1 = sb(p1, "gmod1", [128, 1024], F32)
    shift1 = sb(p1, "shift1", [128, 1024], F32)
    p0 = []
    modb = sb(p0, "modb", [128, 6144], F32)
    bmodb = sb(p0, "bmodb", [128, 6144], F32)
    gb = [sb(p0, "gb%d" % i, [128, 1024], F32) for i in range(4)]
    cv = sb(p0, "cv", [128, 8], F32)
    ecv = sb(p0, "ecv", [128, 8], F32)
    sc = sb(p0, "sc", [128, 8], F32)
    scb = sb(p0, "scb", [128, 8, 128], BF16)
    wm = ring(p0, "wm", [128, 8, 512], BF16, 2)
    tmp = ring(p0, "mtmp", [128, 1024], F32, 3)
    dma("sp", cv.t[:, :], cvec, [], [cv.b], "cv")
    dma("sp", bmodb.t[:, :], b_mod.partition_broadcast(128), [], [bmodb.b], "bmodb")
    for i, g in enumerate((g_pre_mix, g_post_mix, g_pre_ffn, g_post_ffn)):
        dma("sp", gb[i].t[:, :], g.partition_broadcast(128), [], [gb[i].b], "gb%d" % i)
    act(ecv.t[:, :], cv.t[:, :], AF.Exp, [cv.b], [ecv.b], scale=-1.0)
    ts("dve", ecv.t[:, :], ecv.t[:, :], 1.0, None, ALU.add, None, [ecv.b], [ecv.b])
    recip(ecv.t[:, :], ecv.t[:, :], [ecv.b], [ecv.b])
    tt("dve", sc.t[:, :], cv.t[:, :], ecv.t[:, :], ALU.mult, [cv.b, ecv.b], [sc.b])
    cp("dve", scb.t[:, :, :], sc.t[:, :].unsqueeze(2).to_broadcast([128, 8, 128]), [sc.b], [scb.b])
    wmv = w_mod.rearrange("(kc p) n -> p kc n", p=128)
    for blk in range(12):
        w = wm.next()
        dma("pool", w.t[:, :, :], wmv[:, :, blk * 512:(blk + 1) * 512], [], [w.b], w.name)
        ps = PS.next()
        for kc in range(8):
            mm(ps.t[:, :], scb.t[:, kc, :], w.t[:, kc, :], kc == 0, kc == 7, [scb.b, w.b], [ps.b])
        tt("dve", modb.t[:, blk * 512:(blk + 1) * 512], ps.t[:, :], bmodb.t[:, blk * 512:(blk + 1) * 512],
           ALU.add, [ps.b, bmodb.b], [modb.b])
    sl = lambda i: modb.t[:, i * 1024:(i + 1) * 1024]
    cp("pool", shift1.t[:, :], sl(0), [modb.b], [shift1.b])
    stt("dve", gmod1.t[:, :], sl(1), 1.0, gb[0].t[:, :], ALU.add, ALU.mult, [modb.b, gb[0].b], [gmod1.b])
    t0 = tmp.next()
    tt("dve", t0.t[:, :], sl(2), gb[1].t[:, :], ALU.mult, [modb.b, gb[1].b], [t0.b])
    dma("sp", modscr[0], t0.t[:, :], [t0.b], [B_modscr], "modscr", partial=True)
    dma("sp", modscr[1], sl(3), [modb.b], [B_modscr], "modscr", partial=True)
    t1 = tmp.next()
    stt("dve", t1.t[:, :], sl(4), 1.0, gb[2].t[:, :], ALU.add, ALU.mult, [modb.b, gb[2].b], [t1.b])
    dma("sp", modscr[2], t1.t[:, :], [t1.b], [B_modscr], "modscr", partial=True)
    t2 = tmp.next()
    tt("dve", t2.t[:, :], sl(5), gb[3].t[:, :], ALU.mult, [modb.b, gb[3].b], [t2.b])
    dma("sp", modscr[3], t2.t[:, :], [t2.b], [B_modscr], "modscr", partial=True)
    S.barrier()
    if stage == "p0":
        dma("sp", out[0:128, :], gmod1.t[:, :], [gmod1.b], [B_out[0]], "dbg0")
        dma("sp", out[128:256, :], shift1.t[:, :], [shift1.b], [B_out[1]], "dbg1")
        S.dead = True
    A.free(p0)

    w_in_sb = sb(p1, "w_in", [128, 8, 3088], BF16)
    winv = w_in.rearrange("(kc p) n -> p kc n", p=128)
    for kc in range(8):
        dma("pool", w_in_sb.t[:, kc, :], winv[:, kc, :], [], [w_in_sb.b], "w_in", partial=True)
    wg17 = sb(p1, "wg17", [32, 256], F32)
    memset("pool", wg17.t[:, :], 0.0, [wg17.b])
    dma("sp", wg17.t[0:16, :], w_gate_lr, [], [wg17.b], "wg17")
    dma("sp", wg17.t[16:17, :], b_gate.rearrange("(o n) -> o n", o=1), [], [wg17.b], "wg17b")
    gglab = sb(p1, "gglab", [128, 128], F32)
    dma("sp", gglab.t[:, :], g_gla.partition_broadcast(128), [], [gglab.b], "gglab")

    Sf = sb(p1, "Sf", [128, 2, 128], F32)
    Sb = sb(p1, "Sb", [128, 2, 128], BF16)
    memset("pool", Sf.t[:, :, :], 0.0, [Sf.b])
    memset("pool", Sb.t[:, :, :], 0.0, [Sb.b])

    rn = {}
    rn["x_ring"] = ring(p1, "xt", [128, 1024], F32, 2)
    rn["junk"] = sb(p1, "junk_a", [128, 1024], BF16)
    rn["ss_ring"] = ring(p1, "ss", [128, 2], F32, 4)
    rn["hb_ring"] = ring(p1, "hb", [128, 1024], BF16, 2)
    hT_ring = ring(p1, "hT", [128, 8, 512], BF16, 2)
    cs_ring = ring(p1, "csr", [128, 2, 512], F32, 1)
    qs_ring = ring(p1, "qsb", [128, 512], F32, 2)
    rp_ring = ring(p1, "rp", [128, 512], F32, 2)
    vt_ring = ring(p1, "vt", [128, 512], BF16, 2)
    glr_ring = ring(p1, "glr", [32, 512], F32, 2)
    for g in glr_ring.items:
        memset("pool", g.t[:, :], 1.0, [g.b])
    e1_ring = ring(p1, "e1", [128, 256], F32, 2)
    sp_ring = ring(p1, "spr", [128, 256], F32, 2)
    dec_ring = ring(p1, "dec", [128, 256], F32, 2)
    ku_ring = ring(p1, "ku", [128, 256], BF16, 2)
    vg_ring = ring(p1, "vg", [128, 512], BF16, 2)
    acol_ring = ring(p1, "acol", [128, 2], F32, 2)
    eb_ring = ring(p1, "eb", [128, 256], F32, 2)
    enb_ring = ring(p1, "enb", [128, 256], F32, 2)
    qd_ring = ring(p1, "qd", [128, 2, 128], BF16, 2)
    kd_ring = ring(p1, "kd", [128, 2, 128], BF16, 2)
    attm_ring = ring(p1, "attm", [128, 4, 128], BF16, 2)
    er_ring = ring(p1, "er", [128, 512], F32, 1)
    rg_ring = ring(p1, "rg", [128, 512], F32, 2)
    ssg_ring = ring(p1, "ssg", [128, 4], F32, 2)
    on_ring = ring(p1, "on", [128, 512], F32, 1)
    og_ring = ring(p1, "og", [128, 512], BF16, 2)
    ogT_ring = ring(p1, "ogT", [128, 4, 128], BF16, 2)
    junk_s = sb(p1, "junk_s", [128, 128], BF16)

    def proj_fm(col0, ncols, hTb, tsl, ps, pcols):
        for kc in range(8):
            mm(ps.t[0:ncols, pcols], w_in_sb.t[:, kc, col0:col0 + ncols], hTb.t[:, kc, tsl], kc == 0, kc == 7,
               [w_in_sb.b, hTb.b], [ps.b])

    def proj_tm(ti, col0, ncols, hTb, ps):
        for kc in range(8):
            mm(ps.t[:, 0:ncols], hTb.t[:, kc, ti * 128:(ti + 1) * 128], w_in_sb.t[:, kc, col0:col0 + ncols],
               kc == 0, kc == 7, [w_in_sb.b, hTb.b], [ps.b])

    def rmsnorm_mod_T(xt, gm, sh, dstT, dst_cols, defer=False):
        ss = rn["ss_ring"].next()
        junk = rn["junk"]
        memset("pool", ss.t[:, :], 0.0, [ss.b])
        act(junk.t[:, :], xt.t[:, :], AF.Square, [xt.b], [junk.b, ss.b], accum=ss.t[:, 0:1])
        rstd_(ss.t[:, 1:2], ss.t[:, 0:1], 1.0 / 1024.0, [ss.b])
        stt("dve", xt.t[:, :], xt.t[:, :], ss.t[:, 1:2], gm.t[:, :], ALU.mult, ALU.mult, [xt.b, ss.b, gm.b], [xt.b])
        hb = rn["hb_ring"].next()
        tt("dve", hb.t[:, :], xt.t[:, :], sh.t[:, :], ALU.add, [xt.b, sh.b], [hb.b])
        if defer:
            return hb
        rmsnorm_T2(hb, dstT, dst_cols)

    def rmsnorm_T2(hb, dstT, dst_cols, wb=None):
        ps = PS.next()
        pv = bfview(ps)
        for kc in range(8):
            tr(pv[:, kc * 128:(kc + 1) * 128], hb.t[:, kc * 128:(kc + 1) * 128], ident_b, [hb.b, cbt.b], [ps.b])
        cp("act", dstT.t[:, :, dst_cols], pv.rearrange("p (a b) -> p a b", a=8), [ps.b],
           [wb if wb is not None else dstT.b])

    def cut(name):
        if stage == name and not S.dead:
            S.barrier()
            dma("sp", out[0:128, 0:768], cf.t[:, :], [cf.b], [B_out[0]], "dbgc")
            S.dead = True

    pending = [None]

    def back(ctx):
        acol, ku, vg = ctx["acol"], ctx["ku"], ctx["vg"]
        pu = PU.next()
        for h in range(4):
            g, hh = h // 2, h % 2
            mm(pu.t[hh * 64:(hh + 1) * 64, g * 128:(g + 1) * 128], ku.t[:, h * 64:(h + 1) * 64],
               vg.t[:, h * 128:(h + 1) * 128], True, True, [ku.b, vg.b], [pu.b])
        if ctx["kind"] == "own":
            attm, qd, rg, m0, ti = ctx["attm"], ctx["qd"], ctx["rg"], ctx["m0"], ctx["ti"]
            po = PS.next()
            for h in range(4):
                g, hh = h // 2, h % 2
                mm(po.t[:, h * 128:(h + 1) * 128], attm.t[:, h, :], vg.t[:, h * 128:(h + 1) * 128],
                   True, False, [attm.b, vg.b], [po.b])
                mm(po.t[:, h * 128:(h + 1) * 128], qd.t[hh * 64:(hh + 1) * 64, g, :],
                   Sb.t[hh * 64:(hh + 1) * 64, g, :], False, True, [qd.b, Sb.b], [po.b])
            ssg = ssg_ring.next()
            memset("pool", ssg.t[:, :], 0.0, [ssg.b])
            for h in range(4):
                act(junk_s.t[:, :], po.t[:, h * 128:(h + 1) * 128], AF.Square, [po.b], [junk_s.b, ssg.b],
                    accum=ssg.t[:, h:h + 1])
            rstd_(ssg.t[:, :], ssg.t[:, :], 1.0 / 128.0, [ssg.b])
            on = on_ring.next()
            tt("dve", on.t[:, :].rearrange("p (a b) -> p a b", a=4), po.t[:, :].rearrange("p (a b) -> p a b", a=4),
               ssg.t[:, :].unsqueeze(2).to_broadcast([128, 4, 128]), ALU.mult, [po.b, ssg.b], [on.b])
            og = og_ring.next()
            tt("pool", og.t[:, :], on.t[:, :], rg.t[:, :], ALU.mult, [on.b, rg.b], [og.b])
            pt2 = PS.next()
            pv2 = bfview(pt2)
            for h in range(4):
                tr(pv2[:, h * 128:(h + 1) * 128], og.t[:, h * 128:(h + 1) * 128], ident_b, [og.b, cbt.b], [pt2.b])
            ogT = ogT_ring.next()
            cp("act", ogT.t[:, :, :], pv2[:, 0:512].rearrange("p (a b) -> p a b", a=4), [pt2.b], [ogT.b])
            dma("sp", ogscr[:, :, m0 + ti * 128:m0 + (ti + 1) * 128].rearrange("h e t -> e h t"), ogT.t[:, :, :],
                [ogT.b], [B_ogscr], ogT.name, partial=True)
        for g in range(2):
            stt("dve", Sf.t[:, g, :], Sf.t[:, g, :], acol.t[:, g:g + 1], pu.t[:, g * 128:(g + 1) * 128],
                ALU.mult, ALU.add, [Sf.b, acol.b, pu.b], [Sf.b])
        cp("pool", Sb.t[:, :, :], Sf.t[:, :, :], [Sf.b], [Sb.b])

    hTbs = {}

    pro_pending = [None]

    def prologue_flush():
        if pro_pending[0] is not None:
            hb_, tb2, ti2 = pro_pending[0]
            rmsnorm_T2(hb_, hTbs[tb2], slice(ti2 * 128, (ti2 + 1) * 128))
            pro_pending[0] = None

    def prologue_tile(tb_, ti_, immediate=False):
        t_ = tb_ * 4 + ti_
        xt = rn["x_ring"].next()
        dma("sp", xt.t[:, :], xe[t_ * 128:(t_ + 1) * 128, :], [], [xt.b], xt.name)
        hb_ = rmsnorm_mod_T(xt, gmod1, shift1, None, None, defer=True)
        prologue_flush()
        pro_pending[0] = (hb_, tb_, ti_)
        if immediate:
            prologue_flush()

    full512 = slice(0, 512)
    PU = Ring(psb[6:8])
    PS.items = psb[0:6]
    for tb in range(16):
        kind = "prefix" if tb < 8 else ("halo" if tb < 12 else "own")
        S.bmarks = getattr(S, "bmarks", []) + [len(S.streams["pe"])]
        if tb == 0:
            hTbs[0] = hT_ring.next()
            for ti in range(4):
                prologue_tile(0, ti, immediate=(ti == 3))
        hTb = hTbs[tb]
        if tb + 1 < 16:
            hTbs[tb + 1] = hT_ring.next()
        cut("c_norm")
        n0 = (tb - 8) * 512
        m0 = (tb - 12) * 512
        if kind != "prefix":
            csr = cs_ring.next()
            dma("sp", csr.t[:, :, :], cs_tab[:, :, n0:n0 + 512].rearrange("a p n -> p a n"), [], [csr.b], csr.name)
            jobs = [(512, kT, n0)]
            if kind == "own":
                jobs.append((0, qT, m0))
            for (cbase, dst, d0) in jobs:
                for hp in range(4):
                    pa = PS.next()
                    proj_fm(cbase + hp * 128, 128, hTb, full512, pa, full512)
                    qs = qs_ring.next()
                    cp("act", qs.t[:, :], pa.t[:, :], [pa.b], [qs.b])
                    pb_ = PS.next()
                    mm(pb_.t[:, :], Psw_f, qs.t[:, :], True, True, [cf.b, qs.b], [pb_.b])
                    r1 = rp_ring.next()
                    r2 = rp_ring.next()
                    tt("pool", r1.t[:, :], qs.t[:, :], csr.t[:, 0, :], ALU.mult, [qs.b, csr.b], [r1.b])
                    tt("dve", r2.t[:, :], pb_.t[:, :], csr.t[:, 1, :], ALU.mult, [pb_.b, csr.b], [r2.b])
                    tt("pool", dst.t[:, hp, d0:d0 + 512], r1.t[:, :], r2.t[:, :], ALU.add, [r1.b, r2.b], [dst.b])
        if tb == 8:
            cut("c_rope")
        glr = glr_ring.next()
        pg = PS.next()
        proj_fm(3072, 16, hTb, full512, pg, full512)
        cp("act", glr.t[0:16, :], pg.t[0:16, :], [pg.b], [glr.b])
        for ti in range(4):
            t = tb * 4 + ti
            tsl = slice(ti * 128, (ti + 1) * 128)
            if kind != "prefix":
                pvv = PS.next()
                proj_tm(ti, 1024, 512, hTb, pvv)
                vt = vt_ring.next()
                cp("act", vt.t[:, :], pvv.t[:, :], [pvv.b], [vt.b])
                dma("sp", vscr[n0 + ti * 128:n0 + (ti + 1) * 128, :], vt.t[:, :], [vt.b], [B_vscr], vt.name,
                    partial=True)
            pgl = PS.next()
            mm(pgl.t[:, 0:256], glr.t[0:17, tsl], wg17.t[0:17, :], True, True, [glr.b, wg17.b], [pgl.b])
            e1 = e1_ring.next()
            act(e1.t[:, :], pgl.t[:, 0:256], AF.Exp, [pgl.b], [e1.b], scale=-1.0)
            spt = sp_ring.next()
            act(spt.t[:, :], e1.t[:, :], AF.Ln, [e1.b], [spt.b], bias=1.0)
            pk = PS.next()
            proj_tm(ti, 1792, 256, hTb, pk)
            pvg = PS.next()
            proj_tm(ti, 2048, 512, hTb, pvg)
            vg = vg_ring.next()
            cp("act", vg.t[:, :], pvg.t[:, :], [pvg.b], [vg.b])
            paf = PS.next()
            mm(paf.t[:, 0:256], Ls_f, spt.t[:, :], True, True, [cf.b, spt.b], [paf.b])
            dec = dec_ring.next()
            act(dec.t[:, :], paf.t[:, 0:256], AF.Exp, [paf.b], [dec.b], scale=-1.0 / 16.0)
            ku = ku_ring.next()
            stt("dve", ku.t[:, :], pk.t[:, 0:256], tv.t[:, t:t + 1], dec.t[:, :], ALU.mult, ALU.mult,
                [pk.b, tv.b, dec.b], [ku.b])
            pa_ = PS.next()
            for g in range(2):
                mm(pa_.t[:, g:g + 1], spt.t[:, g * 128:(g + 1) * 128], ones_f[:, 0:1], True, True,
                   [spt.b, cf.b], [pa_.b])
            acol = acol_ring.next()
            act(acol.t[:, :], pa_.t[:, 0:2], AF.Exp, [pa_.b], [acol.b], scale=-1.0 / 16.0)
            if kind == "own":
                pcs = PS.next()
                for g in range(2):
                    mm(pcs.t[:, g * 128:(g + 1) * 128], spt.t[:, g * 128:(g + 1) * 128], U_f, True, True,
                       [spt.b, cf.b], [pcs.b])
                eb = eb_ring.next()
                enb = enb_ring.next()
                act(eb.t[:, :], pcs.t[:, 0:256], AF.Exp, [pcs.b], [eb.b], scale=-1.0 / 16.0)
                act(enb.t[:, :], pcs.t[:, 0:256], AF.Exp, [pcs.b], [enb.b], scale=1.0 / 16.0)
                pqk = PS.next()
                for g in range(2):
                    proj_fm(1536 + g * 128, 128, hTb, tsl, pqk, slice(g * 128, (g + 1) * 128))
                    proj_fm(1792 + g * 128, 128, hTb, tsl, pqk, slice(256 + g * 128, 256 + (g + 1) * 128))
                qd = qd_ring.next()
                kd = kd_ring.next()
                stt("dve", qd.t[:, :, :], pqk.t[:, 0:256].rearrange("p (a b) -> p a b", a=2), 0.125,
                    eb.t[:, :].rearrange("p (a b) -> p a b", a=2), ALU.mult, ALU.mult, [pqk.b, eb.b], [qd.b])
                tt("dve", kd.t[:, :, :], pqk.t[:, 256:512].rearrange("p (a b) -> p a b", a=2),
                   enb.t[:, :].rearrange("p (a b) -> p a b", a=2), ALU.mult, [pqk.b, enb.b], [kd.b])
                pr = PS.next()
                proj_tm(ti, 2560, 512, hTb, pr)
                er = er_ring.next()
                act(er.t[:, :], pr.t[:, :], AF.Exp, [pr.b], [er.b], scale=-1.0)
                rg = rg_ring.next()
                tt("dve", rg.t[:, :].rearrange("p (a b) -> p a b", a=4), pr.t[:, :].rearrange("p (a b) -> p a b", a=4),
                   gglab.t[:, :].unsqueeze(1).to_broadcast([128, 4, 128]), ALU.mult, [pr.b, gglab.b], [rg.b])
                ts("dve", er.t[:, :], er.t[:, :], 1.0, None, ALU.add, None, [er.b], [er.b])
                recip(er.t[:, :], er.t[:, :], [er.b], [er.b])
                tt("pool", rg.t[:, :], rg.t[:, :], er.t[:, :], ALU.mult, [rg.b, er.b], [rg.b])
                patt = [PS.next(), PS.next()]
                for h in range(4):
                    g, hh = h // 2, h % 2
                    mm(patt[hh].t[:, g * 128:(g + 1) * 128], kd.t[hh * 64:(hh + 1) * 64, g, :],
                       qd.t[hh * 64:(hh + 1) * 64, g, :], True, True, [kd.b, qd.b], [patt[hh].b])
                attm = attm_ring.next()
                for hh in range(2):
                    tt("dve", attm.t[:, hh:4:2, :], patt[hh].t[:, 0:256].rearrange("p (a b) -> p a b", a=2),
                       U_f.unsqueeze(1).to_broadcast([128, 2, 128]), ALU.mult, [patt[hh].b, cf.b], [attm.b])
            ctx = dict(kind=kind, acol=acol, ku=ku, vg=vg)
            if kind == "own":
                ctx.update(attm=attm, qd=qd, rg=rg, m0=m0, ti=ti)
            if pending[0] is not None:
                back(pending[0])
            pending[0] = ctx
            if tb + 1 < 16:
                prologue_tile(tb + 1, ti, immediate=(ti == 3))
    back(pending[0])
    PS.items = psb
    S.barrier()
    if stage == "p1":
        dbgt = sb(None, "dbgt", [128, 1024], F32)
        for i in range(4):
            cp("dve", dbgt.t[:, :], kT.t[:, i, 2048:3072], [kT.b], [dbgt.b])
            dma("sp", out[i * 128:(i + 1) * 128, :], dbgt.t[:, :], [dbgt.b], [B_out[i]], "dbg0")
        for i in range(4):
            cp("dve", dbgt.t[:, :], qT.t[:, i, 0:1024], [qT.b], [dbgt.b])
            dma("sp", out[(4 + i) * 128:(5 + i) * 128, :], dbgt.t[:, :], [dbgt.b], [B_out[4 + i]], "dbg0")
        S.dead = True
    A.free(p1)

    p23 = []
    catT = sb(p23, "catT", [128, 8, 2048], BF16)
    w_out_sb = sb(p23, "w_out", [128, 8, 1024], BF16)
    woutv = w_out.rearrange("(kc p) n -> p kc n", p=128)
    for kc in range(8):
        dma("pool", w_out_sb.t[:, kc, :], woutv[:, kc, :], [], [w_out_sb.b], "w_out", partial=True)
    for h in range(4):
        dma("sp", catT.t[:, 4 + h, :], ogscr[h], [B_ogscr], [catT.b], "catT_ld", partial=True)
    pa = []
    vp_ring = ring(pa, "vp", [128, 3, 32, 128], BF16, 2)
    acc = sb(pa, "acc", [128, 2, 2048], F32)
    nb_ring = ring(pa, "nb", [128, 2], F32, 4)
    prod = sb(pa, "prod", [128, 2048], BF16)
    P_ring = ring(pa, "P", [128, 512], BF16, 4)
    PT_ring = ring(pa, "PT", [128, 512], BF16, 4)
    rl = sb(pa, "rl", [128, 2048], F32)
    PATS = ((0, 1), (1, 4), (2, 16))
    vps = {}

    def load_vp(hp_):
        vp_ = vp_ring.next()
        for (pi, d) in PATS:
            src = vscr[:, hp_ * 128:(hp_ + 1) * 128].rearrange("(u d) c -> d u c", d=d)
            ntile = 32 // d
            for r in range(d):
                dma("sp", vp_.t[:, pi, r * ntile:(r + 1) * ntile, :],
                    src[r].rearrange("(kt p) c -> p kt c", p=128), [B_vscr], [vp_.b], vp_.name, partial=True)
        vps[hp_] = vp_

    def mk_prod(hp_):
        tt("pool", prod.t[:, :], qT.t[:, hp_, :], kT.t[:, hp_, 2048:4096], ALU.mult, [qT.b, kT.b], [prod.b])

    load_vp(0)
    load_vp(1)
    mk_prod(0)
    for hp in range(4):
        vp = vps[hp]
        if 1 <= hp < 3:
            load_vp(hp + 1)
        memset("pool", acc.t[:, :, :], 0.0, [acc.b])
        units = []
        for (pi, d) in PATS:
            ntile = 32 // d
            for r in range(d):
                for kt in range(16 // d, ntile):
                    units.append((pi, d, r, kt))

        def stageA(u):
            pi, d, r, kt = u
            ntile = 32 // d
            st_ = {}
            q0 = r + d * kt * 128 - 2048
            k0 = r + d * (kt - 1) * 128
            qs = slice(q0, q0 + d * 127 + 1, d)
            ks = slice(k0, k0 + d * 255 + 1, d)
            pS = [PS.next(), PS.next()]
            for hh in range(2):
                mm(pS[hh].t[:, 0:256], qT.t[hh * 64:(hh + 1) * 64, hp, qs],
                   kT.t[hh * 64:(hh + 1) * 64, hp, ks], True, True, [qT.b, kT.b], [pS[hh].b])
            mm(pS[0].t[:, 256:258], prod.t[:, qs], negsel_b, True, True, [prod.b, cbt.b], [pS[0].b])
            nb = nb_ring.next()
            cp("dve", nb.t[:, 0:2], pS[0].t[:, 256:258], [pS[0].b], [nb.b])
            Pt = P_ring.next()
            for hh in range(2):
                act(Pt.t[:, hh * 256:(hh + 1) * 256], pS[hh].t[:, 0:256], AF.Exp,
                    [pS[hh].b, nb.b], [Pt.b], bias=nb.t[:, hh:hh + 1], scale=0.125)
            st_.update(Pt=Pt, qs=qs, vt0=r * ntile + kt - 1, pi=pi, first=(kt == 16 // d))
            return st_

        def stageB(st_):
            Pt = st_["Pt"]
            pT = PS.next()
            pTv = bfview(pT)
            for i in range(4):
                tr(pTv[:, i * 128:(i + 1) * 128], Pt.t[:, i * 128:(i + 1) * 128], ident_b,
                   [Pt.b, cbt.b], [pT.b])
            PT = PT_ring.next()
            mk = mask4h if st_["first"] else mT
            tt("dve", PT.t[:, :], pTv[:, 0:512], mk.t[:, :], ALU.mult, [pT.b, mk.b], [PT.b])
            st_["PT"] = PT

        def stageC(st_):
            PT, qs, vt0, pi = st_["PT"], st_["qs"], st_["vt0"], st_["pi"]
            pO = PS.next()
            for hh in range(2):
                osl = pO.t[hh * 64:(hh + 1) * 64, 0:128]
                lsl = pO.t[hh * 64:(hh + 1) * 64, 128:256]
                for kk in range(2):
                    mm(osl, vp.t[:, pi, vt0 + kk, hh * 64:(hh + 1) * 64],
                       PT.t[:, (hh * 2 + kk) * 128:(hh * 2 + kk + 1) * 128], kk == 0, kk == 1,
                       [vp.b, PT.b], [pO.b])
                for kk in range(2):
                    mm(lsl, ones_b[:, 0:64], PT.t[:, (hh * 2 + kk) * 128:(hh * 2 + kk + 1) * 128],
                       kk == 0, kk == 1, [cbt.b, PT.b], [pO.b])
            tt("dve", acc.t[:, :, qs], acc.t[:, :, qs], pO.t[:, 0:256].rearrange("p (a b) -> p a b", a=2),
               ALU.add, [acc.b, pO.b], [acc.b])

        sts = []
        nu = len(units)
        for it in range(nu + 2):
            if it < nu:
                sts.append(stageA(units[it]))
            if it == nu - 1 and hp + 1 < 4:
                mk_prod(hp + 1)
            if 0 <= it - 1 < nu:
                stageB(sts[it - 1])
            if 0 <= it - 2 < nu:
                stageC(sts[it - 2])
        recip(rl.t[:, :], acc.t[:, 1, :], [acc.b], [rl.b])
        tt("pool", catT.t[:, hp, :], acc.t[:, 0, :], rl.t[:, :], ALU.mult, [acc.b, rl.b], [catT.b])
    S.barrier()
    if stage == "p2":
        dbgt = sb(None, "dbgt", [128, 1024], F32)
        for i in range(8):
            cp("dve", dbgt.t[:, :], catT.t[:, i, 0:1024], [catT.b], [dbgt.b])
            dma("sp", out[i * 128:(i + 1) * 128, :], dbgt.t[:, :], [dbgt.b], [B_out[i]], "dbg0")
        S.dead = True
    A.free(pa)
    A.free(mix)

    p34 = []
    if stage == "full":
        h2T = sb(p34, "h2T", [128, 8, 2048], BF16)
        G = sb(p34, "G", [128, 16, 32], F32)
        GT = sb(p34, "GT", [32, 2048], BF16)
        wgu_ring = Ring([sb(p34, "wgu%d" % i, [128, 8, 1024], BF16) for i in range(2)])
        wd_ring = Ring([sb(p34, "wd0", [128, 8, 1024], BF16)])
        wguv = w_gate_up.rearrange("e (kc p) n -> e p kc n", p=128)
        wdv = w_down.rearrange("e (kc p) n -> e p kc n", p=128)

        def load_gu(e_, hx):
            wg = wgu_ring.next()
            for kc in range(0, 8, 2):
                dma("pool", wg.t[:, kc:kc + 2, :], wguv[e_, :, kc:kc + 2, hx * 1024:(hx + 1) * 1024], [], [wg.b],
                    wg.name, partial=True)
            return wg

        def load_d(e_):
            wd = wd_ring.next()
            for kc in range(0, 8, 2):
                dma("pool", wd.t[:, kc:kc + 2, :], wdv[e_, :, kc:kc + 2, :], [], [wd.b], wd.name, partial=True)
            return wd

        cur0 = [load_gu(0, 0), load_gu(0, 1), load_d(0)]
        rw = sb(p23, "rw", [128, 8, 32], BF16)
        rbb = sb(p23, "rbb", [128, 32], F32)
        dma("pool", rw.t[:, :, :], router_w.rearrange("(kc p) n -> p kc n", p=128), [], [rw.b], "rw")
        dma("sp", rbb.t[:, :], router_b.partition_broadcast(128), [], [rbb.b], "rbb")
        lg_ring = ring(p23, "lg", [128, 32], F32, 2)
        m8_ring = ring(p23, "m8", [128, 8], F32, 2)
        ex_ring = ring(p23, "ex", [128, 32], F32, 2)
        msk_ring = ring(p23, "msk", [128, 32], F32, 2)
        den_ring = ring(p23, "den", [128, 2], F32, 2)
        gtb_ring = ring(p23, "gtb", [128, 32], BF16, 2)
    mods = [sb(p23, "mods%d" % i, [128, 1024], F32) for i in range(3)]
    for i in range(3):
        dma("sp", mods[i].t[:, :], modscr[i], [B_modscr], [mods[i].b], "mods%d" % i)
    gg1, shift2, gmod2 = mods
    rn["x_ring"] = ring(p23, "xt3", [128, 1024], F32, 2)
    rn["junk"] = sb(p23, "junk_a3", [128, 1024], BF16)
    rn["ss_ring"] = ring(p23, "ss3", [128, 2], F32, 4)
    rn["hb_ring"] = ring(p23, "hb3", [128, 1024], BF16, 2)
    yt_ring = ring(p23, "yt", [128, 1024], F32, 2)
    x1_ring = ring(p23, "x1", [128, 1024], F32, 2)
    junk3 = rn["junk"]

    st3 = {}
    h2Tb = [Buf("h2T%d" % i) for i in range(16)]

    def p3A(ti):
        tsl = slice(ti * 128, (ti + 1) * 128)
        py = [PS.next(), PS.next()]
        for half in range(2):
            for kc in range(8):
                mm(py[half].t[:, :], catT.t[:, kc, tsl], w_out_sb.t[:, kc, half * 512:(half + 1) * 512],
                   kc == 0, kc == 7, [catT.b, w_out_sb.b], [py[half].b])
        ss = rn["ss_ring"].next()
        memset("pool", ss.t[:, :], 0.0, [ss.b])
        for half in range(2):
            act(junk3.t[:, 0:512], py[half].t[:, :], AF.Square, [py[half].b], [junk3.b, ss.b],
                accum=ss.t[:, half:half + 1])
        tt("dve", ss.t[:, 0:1], ss.t[:, 0:1], ss.t[:, 1:2], ALU.add, [ss.b], [ss.b])
        rstd_(ss.t[:, 0:1], ss.t[:, 0:1], 1.0 / 1024.0, [ss.b])
        yt = yt_ring.next()
        for half in range(2):
            hs = slice(half * 512, (half + 1) * 512)
            stt("dve", yt.t[:, hs], py[half].t[:, :], ss.t[:, 0:1], gg1.t[:, hs], ALU.mult, ALU.mult,
                [py[half].b, ss.b, gg1.b], [yt.b])
        xt = rn["x_ring"].next()
        dma("sp", xt.t[:, :], xe[(48 + ti) * 128:(49 + ti) * 128, :], [], [xt.b], xt.name)
        x1 = x1_ring.next()
        tt("pool", x1.t[:, :], xt.t[:, :], yt.t[:, :], ALU.add, [xt.b, yt.b], [x1.b])
        dma("sp", out[tsl, :], x1.t[:, :], [x1.b], [B_out[ti]], "out_w%d" % (ti % 4))
        st3[ti] = dict(x1=x1)

    def p3B1(ti):
        st3[ti]["hb"] = rmsnorm_mod_T(st3[ti]["x1"], gmod2, shift2, None, None, defer=True)

    def p3B2(ti):
        tsl = slice(ti * 128, (ti + 1) * 128)
        rmsnorm_T2(st3[ti]["hb"], h2T, tsl, wb=h2Tb[ti])

    def p3C(ti):
        tsl = slice(ti * 128, (ti + 1) * 128)
        pl = PS.next()
        for kc in range(8):
            mm(pl.t[:, 0:32], h2T.t[:, kc, tsl], rw.t[:, kc, :], kc == 0, kc == 7, [h2Tb[ti], rw.b], [pl.b])
        lg = lg_ring.next()
        tt("dve", lg.t[:, :], pl.t[:, 0:32], rbb.t[:, :], ALU.add, [pl.b, rbb.b], [lg.b])
        m8 = m8_ring.next()
        S.add("dve", (lambda m8=m8, lg=lg: (lambda e: e.max(out=m8.t[:, :], in_=lg.t[:, :])))(),
              reads=[lg.b], writes=[m8.b])
        msk = msk_ring.next()
        ts("dve", msk.t[:, :], lg.t[:, :], m8.t[:, 3:4], None, ALU.is_ge, None, [lg.b, m8.b], [msk.b])
        den = den_ring.next()
        memset("pool", den.t[:, :], 0.0, [den.b])
        ts("dve", den.t[:, 0:1], m8.t[:, 0:1], -1.0, None, ALU.mult, None, [m8.b], [den.b])
        ex = ex_ring.next()
        act(ex.t[:, :], lg.t[:, :], AF.Exp, [lg.b, den.b], [ex.b], bias=den.t[:, 0:1], scale=1.0)
        tt("dve", ex.t[:, :], ex.t[:, :], msk.t[:, :], ALU.mult, [ex.b, msk.b], [ex.b])
        S.add("dve", (lambda ex=ex, den=den: (lambda e: e.reduce_sum(out=den.t[:, 1:2], in_=ex.t[:, :],
                                                                   axis=mybir.AxisListType.X)))(),
              reads=[ex.b], writes=[den.b])
        recip(den.t[:, 1:2], den.t[:, 1:2], [den.b], [den.b])
        ts("dve", G.t[:, ti, :], ex.t[:, :], den.t[:, 1:2], None, ALU.mult, None, [ex.b, den.b], [G.b])
        gtb = gtb_ring.next()
        cp("pool", gtb.t[:, :], G.t[:, ti, :], [G.b], [gtb.b])
        st3[ti]["gtb"] = gtb

    def p3D(ti):
        tsl = slice(ti * 128, (ti + 1) * 128)
        gtb = st3[ti]["gtb"]
        pgt = PS.next()
        pgv = bfview(pgt)
        tr(pgv[0:32, 0:128], gtb.t[:, :], ident_b, [gtb.b, cbt.b], [pgt.b])
        cp("act", GT.t[:, tsl], pgv[0:32, 0:128], [pgt.b], [GT.b])

    for it in range(16 + 4):
        if it < 16:
            p3A(it)
        if stage == "full":
            if 0 <= it - 1 < 16:
                p3B1(it - 1)
            if 0 <= it - 2 < 16:
                p3B2(it - 2)
            if 0 <= it - 3 < 16:
                p3C(it - 3)
            if 0 <= it - 4 < 16:
                p3D(it - 4)
    S.barrier()
    A.free(p23)

    if stage == "full":
        p4 = []
        gg2b = sb(p4, "gg2b", [128, 1024], F32)
        dma("sp", gg2b.t[:, :], modscr[3], [B_modscr], [gg2b.b], "gg2b")
        bdn = sb(p4, "bdn", [32, 1024], BF16)
        dma("pool", bdn.t[:, :], b_down, [], [bdn.b], "bdn")
        bgu = sb(p4, "bgu", [128, 16, 32], F32)
        pb = []
        bgs = sb(pb, "bgs", [32, 2048], F32)
        dma("sp", bgs.t[:, :], b_gate_up, [], [bgs.b], "bgs")
        bview = bgs.t[:, :].rearrange("e (fb p s) -> e fb s p", fb=8, s=2)
        for fb in range(8):
            for s_ in range(2):
                pbt = PS.next()
                tr(pbt.t[:, 0:32], bview[:, fb, s_, :], ident_f[0:32, 0:32], [bgs.b, cf.b], [pbt.b])
                cp("act", bgu.t[:, fb * 2 + s_, :], pbt.t[:, 0:32], [pbt.b], [bgu.b])
        bl7 = sb(p4, "bl7", [128, 8, 32], F32)
        ts("dve", bl7.t[:, :, :], bgu.t[:, 1:16:2, :], -1.0, 7.0, ALU.mult, ALU.add, [bgu.b], [bl7.b])
        S.barrier()
        A.free(pb)
        accm = sb(p4, "accm", [128, 8, 1024], F32)
        accb = [Buf("accm%d" % i) for i in range(8)]
        wgu_ring.items = wgu_ring.items + [sb(p4, "wgu2", [128, 8, 1024], BF16)]
        wd_ring.items = wd_ring.items + [sb(p4, "wd1", [128, 8, 1024], BF16)]
        actT_ring = ring(p4, "actT", [128, 8, 512], BF16, 2)
        g_ring = ring(p4, "g_", [128, 512], F32, 2)
        sg_ring = ring(p4, "sg_", [128, 512], F32, 2)
        l_ring = ring(p4, "l_", [128, 512], F32, 2)
        x1b_ring = ring(p4, "x1b", [128, 1024], F32, 2)
        ss4_ring = ring(p4, "ss4", [128, 2], F32, 4)
        junk4 = sb(p4, "junk4", [128, 1024], BF16)
        seq = [(hf_, e_) for hf_ in range(2) for e_ in range(32)]
        NSK = 2

        def gu_fb(e_, tok0, fb, wgA, wgB, aT):
            wg = wgA if fb < 4 else wgB
            fl = fb % 4
            pG = PS.next()
            pL = PS.next()
            for kc in range(8):
                mm(pG.t[:, :], wg.t[:, kc, fl * 256:(fl + 1) * 256:2], h2T.t[:, kc, tok0:tok0 + 512],
                   kc == 0, kc == 7, [wg.b, h2T.b], [pG.b])
            for kc in range(8):
                mm(pL.t[:, :], wg.t[:, kc, fl * 256 + 1:(fl + 1) * 256:2], h2T.t[:, kc, tok0:tok0 + 512],
                   kc == 0, kc == 7, [wg.b, h2T.b], [pL.b])
            g_ = g_ring.next()
            ts("dve", g_.t[:, :], pG.t[:, :], bgu.t[:, fb * 2, e_:e_ + 1], 7.0, ALU.add, ALU.min,
               [pG.b, bgu.b], [g_.b])
            sg = sg_ring.next()
            act(sg.t[:, :], g_.t[:, :], AF.Sigmoid, [g_.b], [sg.b], scale=1.702)
            l_ = l_ring.next()
            act(l_.t[:, :], pL.t[:, :], AF.Relu, [pL.b, bl7.b], [l_.b], bias=bl7.t[:, fb, e_:e_ + 1], scale=-1.0)
            act(l_.t[:, :], l_.t[:, :], AF.Relu, [l_.b, c14.b], [l_.b], bias=c14.t[:, 0:1], scale=-1.0)
            tt("dve", g_.t[:, :], g_.t[:, :], sg.t[:, :], ALU.mult, [g_.b, sg.b], [g_.b])
            stt("dve", aT.t[:, fb, :], l_.t[:, :], -6.0, g_.t[:, :], ALU.add, ALU.mult, [g_.b, l_.b], [aT.b])

        def down(hf_, e_, tb, aT, wd):
            for tl in range(4):
                tg = hf_ * 8 + tb * 4 + tl
                ta = tb * 4 + tl
                py = [PS.next(), PS.next()]
                for half in range(2):
                    for fb in range(8):
                        mm(py[half].t[:, :], aT.t[:, fb, tl * 128:(tl + 1) * 128],
                           wd.t[:, fb, half * 512:(half + 1) * 512], fb == 0, fb == 7, [aT.b, wd.b], [py[half].b])
                for half in range(2):
                    hs = slice(half * 512, (half + 1) * 512)
                    if e_ == 0:
                        S.add("dve", (lambda o=accm.t[:, ta, hs], a=py[half].t[:, :], g=G.t[:, tg, e_:e_ + 1]:
                                      (lambda e: e.tensor_scalar(out=o, in0=a, scalar1=g, scalar2=None, op0=ALU.mult)))(),
                              reads=[py[half].b, G.b], writes=[accb[ta]], partial=(half == 1))
                    else:
                        stt("dve", accm.t[:, ta, hs], py[half].t[:, :], G.t[:, tg, e_:e_ + 1], accm.t[:, ta, hs],
                            ALU.mult, ALU.add, [py[half].b, G.b, accb[ta]], [accb[ta]])

        def finalize(hf_, tas):
            sst = {}
            for ta in tas:
                tg = hf_ * 8 + ta
                tsl = slice(tg * 128, (tg + 1) * 128)
                pb2 = [PS.next(), PS.next()]
                ss = ss4_ring.next()
                memset("pool", ss.t[:, :], 0.0, [ss.b])
                for half in range(2):
                    hs = slice(half * 512, (half + 1) * 512)
                    mm(pb2[half].t[:, :], GT.t[:, tsl], bdn.t[:, hs], True, True, [GT.b, bdn.b], [pb2[half].b])
                    tt("dve", accm.t[:, ta, hs], accm.t[:, ta, hs], pb2[half].t[:, :], ALU.add,
                       [accb[ta], pb2[half].b], [accb[ta]])
                act(junk4.t[:, :], accm.t[:, ta, :], AF.Square, [accb[ta]], [junk4.b, ss.b], accum=ss.t[:, 0:1])
                sst[ta] = ss
            for ta in tas:
                ss = sst[ta]
                rstd_(ss.t[:, 0:1], ss.t[:, 0:1], 1.0 / 1024.0, [ss.b])
            for ta in tas:
                tg = hf_ * 8 + ta
                tsl = slice(tg * 128, (tg + 1) * 128)
                ss = sst[ta]
                stt("dve", accm.t[:, ta, :], accm.t[:, ta, :], ss.t[:, 0:1], gg2b.t[:, :], ALU.mult, ALU.mult,
                    [accb[ta], ss.b, gg2b.b], [accb[ta]])
                x1b = x1b_ring.next()
                dma("sp", x1b.t[:, :], out[tsl, :], [B_out[tg]], [x1b.b], x1b.name)
                tt("pool", accm.t[:, ta, :], accm.t[:, ta, :], x1b.t[:, :], ALU.add, [accb[ta], x1b.b], [accb[ta]])
                dma("sp", out[tsl, :], accm.t[:, ta, :], [accb[ta]], [B_out[tg]], "out_f")

        cur = cur0
        pre = None
        for si, (hf_, e_) in enumerate(seq):
            wgA, wgB, wd = cur
            have_next = si + 1 < len(seq)
            ne = seq[si + 1][1] if have_next else None
            nxtA = load_gu(ne, 0) if have_next else None
            nxtB = None
            nxtD = load_d(ne) if have_next else None
            for tb in range(2):
                tok0 = hf_ * 1024 + tb * 512
                if pre is None:
                    aT = actT_ring.next()
                    fb0 = 0
                else:
                    aT = pre
                    fb0 = NSK
                for fb in range(fb0, 8):
                    gu_fb(e_, tok0, fb, wgA, wgB, aT)
                if tb == 1 and have_next:
                    nxtB = load_gu(ne, 1)
                pre = None
                if tb == 0:
                    pre = actT_ring.next()
                    for fb in range(NSK):
                        gu_fb(e_, hf_ * 1024 + 512, fb, wgA, wgB, pre)
                elif have_next:
                    pre = actT_ring.next()
                    for fb in range(NSK):
                        gu_fb(ne, seq[si + 1][0] * 1024, fb, nxtA, None, pre)
                down(hf_, e_, tb, aT, wd)
                if e_ == 31:
                    finalize(hf_, range(tb * 4, tb * 4 + 4))
            cur = [nxtA, nxtB, nxtD]
        A.free(p4)
    A.free(p34)

    S.dead = False
    S.add("sp", lambda e: e.nop(), reads=B_out)

    S.finalize()
    sems = {e: es.enter_context(nc.semaphore("s_" + e)) for e in ENGS}
    dsems = {}
    for i, k in enumerate(sorted(S.dma_cum.keys())):
        dsems[k] = es.enter_context(nc.semaphore("d%d" % i))
    block = es.enter_context(nc.Block())
    S.emit(block, sems, dsems)
    es.close()
    build.info = dict(bmarks=getattr(S, "bmarks", []), marks=[m["pe"] for m in S.marks], peak=A.peak, nops=len(S.ops), ndsem=len(dsems),
                      per_eng={e: len(S.streams[e]) for e in ENGS},
                      flagged={e: sum(1 for o in S.streams[e] if o.flag) for e in ENGS})
    return nc, S


def make_in_maps(inputs, stage="full"):
    x = np.asarray(inputs["x"], np.float32)
    c = np.asarray(inputs["c"], np.float32)
    cf, cb = _consts()
    shared = {
        "cst_f32": cf, "cst_bf": cb,
        "w_mod": np.ascontiguousarray(inputs["w_mod"][0], np.float32),
        "b_mod": np.ascontiguousarray(inputs["b_mod"][0], np.float32),
        "g_pre_mix": np.ascontiguousarray(inputs["g_pre_mix"][0], np.float32),
        "g_post_mix": np.ascontiguousarray(inputs["g_post_mix"][0], np.float32),
        "g_pre_ffn": np.ascontiguousarray(inputs["g_pre_ffn"][0], np.float32),
        "g_post_ffn": np.ascontiguousarray(inputs["g_post_ffn"][0], np.float32),
        "w_in": np.ascontiguousarray(inputs["w_in"][0], np.float32),
        "w_gate_lr": np.ascontiguousarray(inputs["w_gate_lr"][0], np.float32),
        "b_gate": np.ascontiguousarray(inputs["b_gate"][0], np.float32),
        "g_gla": np.ascontiguousarray(inputs["g_gla"][0], np.float32),
        "w_out": np.ascontiguousarray(inputs["w_out"][0], np.float32),
    }
    if stage == "full":
        shared.update({
            "router_w": np.ascontiguousarray(inputs["router_w"][0], np.float32),
            "router_b": np.ascontiguousarray(inputs["router_b"][0], np.float32),
            "w_gate_up": np.ascontiguousarray(inputs["w_gate_up"][0], np.float32),
            "b_gate_up": np.ascontiguousarray(inputs["b_gate_up"][0], np.float32),
            "w_down": np.ascontiguousarray(inputs["w_down"][0], np.float32),
            "b_down": np.ascontiguousarray(inputs["b_down"][0], np.float32),
        })
    maps = []
    for core in range(NCORES):
        b, j = core // 4, core % 4
        end = (j + 1) * 2048
        start = end - 8192
        xeh = np.zeros((8192, 1024), np.float32)
        lo = max(start, 0)
        xeh[lo - start:, :] = x[b, lo:end, :]
        tvh = np.zeros((128, 64), np.float32)
        for t in range(64):
            if start + t * 128 >= 0:
                tvh[:, t] = 1.0
        m = dict(shared)
        m["xe"] = xeh
        m["tilevalid"] = tvh
        m["cvec"] = np.ascontiguousarray(c[b].reshape(8, 128).T)
        m["cs_tab"] = _rope_tables(end)
        maps.append(m)
    return maps


_CACHE = {}


def kernel(**inputs):
    if "nc" not in _CACHE:
        _CACHE["nc"] = build("full")[0]
    nc = _CACHE["nc"]
    maps = make_in_maps(inputs, "full")
    res = run_bass_kernel_spmd(nc, maps, core_ids=list(range(NCORES)))
    outp = np.zeros((2, 8192, 1024), np.float32)
    for core in range(NCORES):
        b, j = core // 4, core % 4
        outp[b, j * 2048:(j + 1) * 2048, :] = np.asarray(res.results[core]["out"], np.float32)
    return outp
```

```python
import numpy as np
import ml_dtypes
from contextlib import ExitStack
import concourse.bass as bass
import concourse.mybir as mybir
from concourse.bass_utils import run_bass_kernel_spmd

F32 = mybir.dt.float32
BF16 = mybir.dt.bfloat16
ALU = mybir.AluOpType
AF = mybir.ActivationFunctionType

ENGS = ("pe", "act", "dve", "pool", "sp")
EPS = 1e-6
NCORES = 8


class Buf:
    __slots__ = ("name", "w", "r", "war", "excl")

    def __init__(self, name):
        self.name = name
        self.w = {}
        self.r = {}
        self.war = []
        self.excl = False


class Op:
    __slots__ = ("eng", "fn", "dma", "pos", "deps", "flag", "cnt", "semkey", "cum")

    def __init__(self, eng, fn, dma, semkey):
        self.eng = eng
        self.fn = fn
        self.dma = dma
        self.semkey = semkey
        self.deps = []
        self.flag = False
        self.cnt = 0
        self.cum = 0
        self.pos = 0


class Sched:
    def __init__(self):
        self.ops = []
        self.streams = {e: [] for e in ENGS}
        self.dma_cum = {}
        self.last_dma = {}
        self.pending_bar = {}
        self.dead = False

    def _key(self, op):
        return ("dma", op.semkey) if op.dma else op.eng

    def add(self, eng, fn, reads=(), writes=(), dma=False, semkey=None, partial=False):
        if self.dead:
            return None
        op = Op(eng, fn, dma, semkey)
        op.pos = len(self.streams[eng])
        self.streams[eng].append(op)
        self.ops.append(op)
        if dma:
            c = self.dma_cum.get(semkey, 0) + 16
            self.dma_cum[semkey] = c
            op.cum = c
            self.last_dma[semkey] = op
        deps = []
        pb = self.pending_bar.pop(eng, None)
        if pb:
            deps.extend(pb)
        for b in reads:
            deps.extend(b.w.values())
            if b.excl:
                deps.extend(o for o in b.r.values() if o.eng != eng)
        for b in writes:
            if b.r:
                b.war = list(b.r.values()) + list(b.w.values())
                deps.extend(b.war)
            elif partial:
                deps.extend(b.war)
            else:
                deps.extend(b.w.values())
        op.deps = deps
        k = self._key(op)
        for b in reads:
            b.r[k] = op
        for b in writes:
            if b.r or not partial:
                if not b.r:
                    b.war = []
                b.w = {}
                b.r = {}
            b.w[k] = op
        return op

    def barrier(self):
        self.marks = getattr(self, "marks", [])
        self.marks.append({e: len(self.streams[e]) for e in ENGS})
        deps = []
        for e in ENGS:
            if self.streams[e]:
                last = None
                for o in reversed(self.streams[e]):
                    if not o.dma:
                        last = o
                        break
                if last is not None:
                    deps.append(last)
        deps.extend(self.last_dma.values())
        for e in ENGS:
            self.pending_bar[e] = list(deps)

    def finalize(self):
        self.waits = {}
        seen = {e: {} for e in ENGS}
        for op in self.ops:
            E = op.eng
            sv = seen[E]
            need = {}
            for d in op.deps:
                if d is op:
                    continue
                if d.dma:
                    key = ("dma", d.semkey)
                    val = d.cum
                else:
                    if d.eng == E and E in ("pe", "sp"):
                        continue
                    key = d.eng
                    val = d.pos + 1
                if sv.get(key, 0) >= val:
                    continue
                if need.get(key, (0, None))[0] < val:
                    need[key] = (val, d)
            wl = []
            for key, (val, d) in need.items():
                sv[key] = val
                if not d.dma:
                    d.flag = True
                wl.append(d)
            self.waits[id(op)] = wl
        for e in ENGS:
            c = 0
            for op in self.streams[e]:
                if not op.dma and op.flag:
                    c += 1
                    op.cnt = c

    def emit(self, block, sems, dsems):
        def run(ename, e):
            for op in self.streams[ename]:
                for d in self.waits[id(op)]:
                    if d.dma:
                        e.wait_ge(dsems[d.semkey], d.cum)
                    else:
                        e.wait_ge(sems[d.eng], d.cnt)
                ins = op.fn(e)
                if op.dma:
                    ins.then_inc(dsems[op.semkey], 16)
                elif op.flag:
                    ins.then_inc(sems[ename], 1)

        @block.sync
        def _(e):
            run("sp", e)

        @block.scalar
        def _(e):
            run("act", e)

        @block.vector
        def _(e):
            run("dve", e)

        @block.gpsimd
        def _(e):
            run("pool", e)

        @block.tensor
        def _(e):
            run("pe", e)


class TT:
    def __init__(self, t, name):
        self.t = t
        self.b = Buf(name)
        self.name = name


class Arena:
    def __init__(self, big, nbytes):
        self.big = big
        self.n = nbytes
        self.live = []
        self.peak = 0

    def alloc(self, name, shape, dt):
        esz = 4 if dt == F32 else 2
        fb = esz
        for d in shape[1:]:
            fb *= d
        size = (fb + 63) // 64 * 64
        off = 0
        for (o, sz, _) in sorted(self.live):
            if off + size <= o:
                break
            off = max(off, o + sz)
        assert off + size <= self.n, "SBUF arena overflow allocating %s (%d B); live=%d" % (
            name, size, sum(x[1] for x in self.live))
        self.live.append((off, size, name))
        self.peak = max(self.peak, off + size)
        ap = self.big[0:shape[0], off // 2:(off + fb) // 2]
        if dt == F32:
            ap = ap.bitcast(F32)
        if len(shape) == 3:
            ap = ap.rearrange("p (a b) -> p a b", a=shape[1])
        elif len(shape) == 4:
            ap = ap.rearrange("p (a b c) -> p a b c", a=shape[1], b=shape[2])
        t = TT(ap, name)
        t.off = off
        return t

    def free(self, tts):
        offs = set(t.off for t in tts)
        self.live = [x for x in self.live if x[0] not in offs]


class Ring:
    def __init__(self, items):
        self.items = items
        self.i = 0

    def next(self):
        it = self.items[self.i % len(self.items)]
        self.i += 1
        return it


def _consts():
    idx = np.arange(128)
    U = (idx[:, None] <= idx[None, :]).astype(np.float32)
    UT = U.T.copy()
    ident = np.eye(128, dtype=np.float32)
    Ls = (idx[:, None] > idx[None, :]).astype(np.float32)
    negdiag = (-0.125 * ident).astype(np.float32)
    ones = np.ones((128, 128), np.float32)
    sw = (idx // 64) * 64 + ((idx % 64) + 32) % 64
    Psw = np.zeros((128, 128), np.float32)
    Psw[sw, idx] = 1.0
    cf = np.concatenate([ident, U, Ls, negdiag, ones, Psw], axis=1)
    mask4 = np.concatenate([U, UT, U, UT], axis=1)
    negsel = np.zeros((128, 128), np.float32)
    negsel[0:64, 0] = -0.125
    negsel[64:128, 1] = -0.125
    cb = np.concatenate([ident, ones, mask4, negsel], axis=1).astype(ml_dtypes.bfloat16)
    return cf, cb


def _rope_tables(end):
    pos = (end - 4096 + np.arange(4096)).astype(np.float32)
    half = 32
    inv = (np.float32(10000.0) ** (-(np.arange(half, dtype=np.float32) / np.float32(half)))).astype(np.float32)
    ang = pos[:, None] * inv[None, :]
    c = np.cos(ang).astype(np.float32).T
    s = np.sin(ang).astype(np.float32).T
    cos_t = np.tile(c, (4, 1))
    sgn = np.where((np.arange(128) % 64) < 32, -1.0, 1.0).astype(np.float32)
    sin_t = np.tile(s, (4, 1)) * sgn[:, None]
    return np.stack([cos_t, sin_t], axis=0).astype(np.float32)


SBUF_BYTES = 206 * 1024


def build(stage="full"):
    nc = bass.Bass("TRN2", target_bir_lowering=False)
    S = Sched()

    def din(name, shape, dt=F32):
        return nc.dram_tensor(name, list(shape), dt, kind="ExternalInput").ap()

    xe = din("xe", [8192, 1024])
    tv_d = din("tilevalid", [128, 64])
    cvec = din("cvec", [128, 8])
    cs_tab = din("cs_tab", [2, 128, 4096])
    cf_d = din("cst_f32", [128, 768])
    cb_d = din("cst_bf", [128, 896], BF16)
    w_mod = din("w_mod", [1024, 6144])
    b_mod = din("b_mod", [6144])
    g_pre_mix = din("g_pre_mix", [1024])
    g_post_mix = din("g_post_mix", [1024])
    g_pre_ffn = din("g_pre_ffn", [1024])
    g_post_ffn = din("g_post_ffn", [1024])
    w_in = din("w_in", [1024, 3088])
    w_gate_lr = din("w_gate_lr", [16, 256])
    b_gate = din("b_gate", [256])
    g_gla = din("g_gla", [128])
    w_out = din("w_out", [1024, 1024])
    if stage == "full":
        router_w = din("router_w", [1024, 32])
        router_b = din("router_b", [32])
        w_gate_up = din("w_gate_up", [32, 1024, 2048])
        b_gate_up = din("b_gate_up", [32, 2048])
        w_down = din("w_down", [32, 1024, 1024])
        b_down = din("b_down", [32, 1024])
    out = nc.dram_tensor("out", [2048, 1024], F32, kind="ExternalOutput").ap()
    vscr = nc.dram_tensor("vscr", [4096, 512], BF16, kind="Internal").ap()
    ogscr = nc.dram_tensor("ogscr", [4, 128, 2048], BF16, kind="Internal").ap()
    modscr = nc.dram_tensor("modscr", [4, 128, 1024], F32, kind="Internal").ap()
    B_vscr = Buf("vscr")
    B_ogscr = Buf("ogscr")
    B_modscr = Buf("modscr")
    B_out = [Buf("out%d" % i) for i in range(16)]

    es = ExitStack()
    big = es.enter_context(nc.sbuf_tensor("big", [128, SBUF_BYTES // 2], BF16))
    A = Arena(big, SBUF_BYTES)

    def sb(scope, name, shape, dt):
        t = A.alloc(name, list(shape), dt)
        if scope is not None:
            scope.append(t)
        return t

    def ring(scope, name, shape, dt, n):
        return Ring([sb(scope, "%s%d" % (name, i), shape, dt) for i in range(n)])

    def dma(q, out_ap, in_ap, reads, writes, key, partial=False):
        S.add(q, lambda e: e.dma_start(out=out_ap, in_=in_ap), reads=reads, writes=writes,
              dma=True, semkey=key, partial=partial)

    def mm(out_ap, lhsT, rhs, start, stop, reads, writes):
        S.add("pe", lambda e: e.matmul(out_ap, lhsT=lhsT, rhs=rhs, start=start, stop=stop),
              reads=reads, writes=writes)

    def tr(out_ap, in_ap, ident_ap, reads, writes):
        S.add("pe", lambda e: e.transpose(out_ap, in_ap, ident_ap), reads=reads, writes=writes)

    def act(out_ap, in_ap, func, reads, writes, bias=None, scale=None, accum=None):
        kw = {}
        if bias is not None:
            kw["bias"] = bias
        if scale is not None:
            kw["scale"] = scale
        if accum is not None:
            kw["accum_out"] = accum
        S.add("act", lambda e: e.activation(out=out_ap, in_=in_ap, func=func, **kw), reads=reads, writes=writes)

    def tt(eng, out_ap, a, b, op, reads, writes):
        S.add(eng, lambda e: e.tensor_tensor(out=out_ap, in0=a, in1=b, op=op), reads=reads, writes=writes)

    def ts(eng, out_ap, a, s1, s2, op0, op1, reads, writes):
        if op1 is None:
            S.add(eng, lambda e: e.tensor_scalar(out=out_ap, in0=a, scalar1=s1, scalar2=None, op0=op0),
                  reads=reads, writes=writes)
        else:
            S.add(eng, lambda e: e.tensor_scalar(out=out_ap, in0=a, scalar1=s1, scalar2=s2, op0=op0, op1=op1),
                  reads=reads, writes=writes)

    def stt(eng, out_ap, a, sc, b, op0, op1, reads, writes):
        S.add(eng, lambda e: e.scalar_tensor_tensor(out=out_ap, in0=a, scalar=sc, in1=b, op0=op0, op1=op1),
              reads=reads, writes=writes)

    def cp(eng, out_ap, in_ap, reads, writes):
        if eng == "act":
            S.add("act", lambda e: e.copy(out=out_ap, in_=in_ap), reads=reads, writes=writes)
        else:
            S.add(eng, lambda e: e.tensor_copy(out=out_ap, in_=in_ap), reads=reads, writes=writes)

    def recip(out_ap, in_ap, reads, writes):
        S.add("dve", lambda e: e.reciprocal(out=out_ap, in_=in_ap), reads=reads, writes=writes)

    def ttr(out_ap, a, b, accum, reads, writes):
        S.add("dve", lambda e: e.tensor_tensor_reduce(out=out_ap, in0=a, in1=b, scale=1.0, scalar=0.0,
                                                      op0=ALU.mult, op1=ALU.add, accum_out=accum),
              reads=reads, writes=writes)

    def rstd_(ap_out, ap_in, inv_n, bufs):
        act(ap_out, ap_in, AF.Ln, bufs + [epsc.b], bufs, bias=epsc.t[:, 0:1], scale=inv_n)
        act(ap_out, ap_out, AF.Exp, bufs, bufs, scale=-0.5)

    def memset(eng, ap, val, writes):
        S.add(eng, lambda e: e.memset(ap, val), writes=writes)

    psb = []
    for i in range(8):
        t = es.enter_context(nc.psum_tensor("psb%d" % i, [128, 512], F32))
        psb.append(TT(t, "psb%d" % i))
        psb[-1].b.excl = True
    PS = Ring(psb)

    def bfview(p):
        return p.t[:, :].bitcast(BF16)

    cf = sb(None, "cf", [128, 768], F32)
    cbt = sb(None, "cb", [128, 896], BF16)
    epsc = sb(None, "epsc", [128, 1], F32)
    memset("pool", epsc.t[:, :], EPS, [epsc.b])
    c14 = sb(None, "c14", [128, 1], F32)
    memset("pool", c14.t[:, :], 14.0, [c14.b])
    tv = sb(None, "tv", [128, 64], F32)
    dma("sp", cf.t[:, :], cf_d, [], [cf.b], "cf")
    dma("sp", cbt.t[:, :], cb_d, [], [cbt.b], "cb")
    dma("sp", tv.t[:, :], tv_d, [], [tv.b], "tv")
    ident_f = cf.t[:, 0:128]
    U_f = cf.t[:, 128:256]
    Ls_f = cf.t[:, 256:384]
    negdiag = cf.t[:, 384:512]
    ones_f = cf.t[:, 512:640]
    Psw_f = cf.t[:, 640:768]
    ident_b = cbt.t[:, 0:128]
    ones_b = cbt.t[:, 128:256]
    mask4 = cbt.t[:, 256:768]
    negsel_b = cbt.t[:, 768:770]
    mT = sb(None, "mT", [128, 512], BF16)
    cp("pool", mT.t[:, 0:384], cbt.t[:, 384:768], [cbt.b], [mT.b])
    cp("pool", mT.t[:, 384:512], cbt.t[:, 256:384], [cbt.b, mT.b], [mT.b])
    mask4h = sb(None, "mask4h", [128, 512], BF16)
    cp("pool", mask4h.t[:, :], mT.t[:, :], [mT.b], [mask4h.b])
    hv = tv.t[:, 32:33]
    for off in (0, 256):
        ts("dve", mask4h.t[:, off:off + 128], mask4h.t[:, off:off + 128], hv, None, ALU.mult, None,
           [mask4h.b, tv.b], [mask4h.b])

    mix = []
    qT = sb(mix, "qT", [128, 4, 2048], BF16)
    kT = sb(mix, "kT", [128, 4, 4096], BF16)

    p1 = []
    gmod1 = sb(p1, "gmod1", [128, 1024], F32)
    shift1 = sb(p1, "shift1", [128, 1024], F32)
    p0 = []
    modb = sb(p0, "modb", [128, 6144], F32)
    bmodb = sb(p0, "bmodb", [128, 6144], F32)
    gb = [sb(p0, "gb%d" % i, [128, 1024], F32) for i in range(4)]
    cv = sb(p0, "cv", [128, 8], F32)
    ecv = sb(p0, "ecv", [128, 8], F32)
    sc = sb(p0, "sc", [128, 8], F32)
    scb = sb(p0, "scb", [128, 8, 128], BF16)
    wm = ring(p0, "wm", [128, 8, 512], BF16, 2)
    tmp = ring(p0, "mtmp", [128, 1024], F32, 3)
    dma("sp", cv.t[:, :], cvec, [], [cv.b], "cv")
    dma("sp", bmodb.t[:, :], b_mod.partition_broadcast(128), [], [bmodb.b], "bmodb")
    for i, g in enumerate((g_pre_mix, g_post_mix, g_pre_ffn, g_post_ffn)):
        dma("sp", gb[i].t[:, :], g.partition_broadcast(128), [], [gb[i].b], "gb%d" % i)
    act(ecv.t[:, :], cv.t[:, :], AF.Exp, [cv.b], [ecv.b], scale=-1.0)
    ts("dve", ecv.t[:, :], ecv.t[:, :], 1.0, None, ALU.add, None, [ecv.b], [ecv.b])
    recip(ecv.t[:, :], ecv.t[:, :], [ecv.b], [ecv.b])
    tt("dve", sc.t[:, :], cv.t[:, :], ecv.t[:, :], ALU.mult, [cv.b, ecv.b], [sc.b])
    cp("dve", scb.t[:, :, :], sc.t[:, :].unsqueeze(2).to_broadcast([128, 8, 128]), [sc.b], [scb.b])
    wmv = w_mod.rearrange("(kc p) n -> p kc n", p=128)
    for blk in range(12):
        w = wm.next()
        dma("pool", w.t[:, :, :], wmv[:, :, blk * 512:(blk + 1) * 512], [], [w.b], w.name)
        ps = PS.next()
        for kc in range(8):
            mm(ps.t[:, :], scb.t[:, kc, :], w.t[:, kc, :], kc == 0, kc == 7, [scb.b, w.b], [ps.b])
        tt("dve", modb.t[:, blk * 512:(blk + 1) * 512], ps.t[:, :], bmodb.t[:, blk * 512:(blk + 1) * 512],
           ALU.add, [ps.b, bmodb.b], [modb.b])
    sl = lambda i: modb.t[:, i * 1024:(i + 1) * 1024]
    cp("pool", shift1.t[:, :], sl(0), [modb.b], [shift1.b])
    stt("dve", gmod1.t[:, :], sl(1), 1.0, gb[0].t[:, :], ALU.add, ALU.mult, [modb.b, gb[0].b], [gmod1.b])
    t0 = tmp.next()
    tt("dve", t0.t[:, :], sl(2), gb[1].t[:, :], ALU.mult, [modb.b, gb[1].b], [t0.b])
    dma("sp", modscr[0], t0.t[:, :], [t0.b], [B_modscr], "modscr", partial=True)
    dma("sp", modscr[1], sl(3), [modb.b], [B_modscr], "modscr", partial=True)
    t1 = tmp.next()
    stt("dve", t1.t[:, :], sl(4), 1.0, gb[2].t[:, :], ALU.add, ALU.mult, [modb.b, gb[2].b], [t1.b])
    dma("sp", modscr[2], t1.t[:, :], [t1.b], [B_modscr], "modscr", partial=True)
    t2 = tmp.next()
    tt("dve", t2.t[:, :], sl(5), gb[3].t[:, :], ALU.mult, [modb.b, gb[3].b], [t2.b])
    dma("sp", modscr[3], t2.t[:, :], [t2.b], [B_modscr], "modscr", partial=True)
    S.barrier()
    if stage == "p0":
        dma("sp", out[0:128, :], gmod1.t[:, :], [gmod1.b], [B_out[0]], "dbg0")
        dma("sp", out[128:256, :], shift1.t[:, :], [shift1.b], [B_out[1]], "dbg1")
        S.dead = True
    A.free(p0)

    w_in_sb = sb(p1, "w_in", [128, 8, 3088], BF16)
    winv = w_in.rearrange("(kc p) n -> p kc n", p=128)
    for kc in range(8):
        dma("pool", w_in_sb.t[:, kc, :], winv[:, kc, :], [], [w_in_sb.b], "w_in", partial=True)
    wg17 = sb(p1, "wg17", [32, 256], F32)
    memset("pool", wg17.t[:, :], 0.0, [wg17.b])
    dma("sp", wg17.t[0:16, :], w_gate_lr, [], [wg17.b], "wg17")
    dma("sp", wg17.t[16:17, :], b_gate.rearrange("(o n) -> o n", o=1), [], [wg17.b], "wg17b")
    gglab = sb(p1, "gglab", [128, 128], F32)
    dma("sp", gglab.t[:, :], g_gla.partition_broadcast(128), [], [gglab.b], "gglab")

    Sf = sb(p1, "Sf", [128, 2, 128], F32)
    Sb = sb(p1, "Sb", [128, 2, 128], BF16)
    memset("pool", Sf.t[:, :, :], 0.0, [Sf.b])
    memset("pool", Sb.t[:, :, :], 0.0, [Sb.b])

    rn = {}
    rn["x_ring"] = ring(p1, "xt", [128, 1024], F32, 2)
    rn["junk"] = sb(p1, "junk_a", [128, 1024], BF16)
    rn["ss_ring"] = ring(p1, "ss", [128, 2], F32, 4)
    rn["hb_ring"] = ring(p1, "hb", [128, 1024], BF16, 2)
    hT_ring = ring(p1, "hT", [128, 8, 512], BF16, 2)
    cs_ring = ring(p1, "csr", [128, 2, 512], F32, 1)
    qs_ring = ring(p1, "qsb", [128, 512], F32, 2)
    rp_ring = ring(p1, "rp", [128, 512], F32, 2)
    vt_ring = ring(p1, "vt", [128, 512], BF16, 2)
    glr_ring = ring(p1, "glr", [32, 512], F32, 2)
    for g in glr_ring.items:
        memset("pool", g.t[:, :], 1.0, [g.b])
    e1_ring = ring(p1, "e1", [128, 256], F32, 2)
    sp_ring = ring(p1, "spr", [128, 256], F32, 2)
    dec_ring = ring(p1, "dec", [128, 256], F32, 2)
    ku_ring = ring(p1, "ku", [128, 256], BF16, 2)
    vg_ring = ring(p1, "vg", [128, 512], BF16, 2)
    acol_ring = ring(p1, "acol", [128, 2], F32, 2)
    eb_ring = ring(p1, "eb", [128, 256], F32, 2)
    enb_ring = ring(p1, "enb", [128, 256], F32, 2)
    qd_ring = ring(p1, "qd", [128, 2, 128], BF16, 2)
    kd_ring = ring(p1, "kd", [128, 2, 128], BF16, 2)
    attm_ring = ring(p1, "attm", [128, 4, 128], BF16, 2)
    er_ring = ring(p1, "er", [128, 512], F32, 1)
    rg_ring = ring(p1, "rg", [128, 512], F32, 2)
    ssg_ring = ring(p1, "ssg", [128, 4], F32, 2)
    on_ring = ring(p1, "on", [128, 512], F32, 1)
    og_ring = ring(p1, "og", [128, 512], BF16, 2)
    ogT_ring = ring(p1, "ogT", [128, 4, 128], BF16, 2)
    junk_s = sb(p1, "junk_s", [128, 128], BF16)

    def proj_fm(col0, ncols, hTb, tsl, ps, pcols):
        for kc in range(8):
            mm(ps.t[0:ncols, pcols], w_in_sb.t[:, kc, col0:col0 + ncols], hTb.t[:, kc, tsl], kc == 0, kc == 7,
               [w_in_sb.b, hTb.b], [ps.b])

    def proj_tm(ti, col0, ncols, hTb, ps):
        for kc in range(8):
            mm(ps.t[:, 0:ncols], hTb.t[:, kc, ti * 128:(ti + 1) * 128], w_in_sb.t[:, kc, col0:col0 + ncols],
               kc == 0, kc == 7, [w_in_sb.b, hTb.b], [ps.b])

    def rmsnorm_mod_T(xt, gm, sh, dstT, dst_cols, defer=False):
        ss = rn["ss_ring"].next()
        junk = rn["junk"]
        memset("pool", ss.t[:, :], 0.0, [ss.b])
        act(junk.t[:, :], xt.t[:, :], AF.Square, [xt.b], [junk.b, ss.b], accum=ss.t[:, 0:1])
        rstd_(ss.t[:, 1:2], ss.t[:, 0:1], 1.0 / 1024.0, [ss.b])
        stt("dve", xt.t[:, :], xt.t[:, :], ss.t[:, 1:2], gm.t[:, :], ALU.mult, ALU.mult, [xt.b, ss.b, gm.b], [xt.b])
        hb = rn["hb_ring"].next()
        tt("dve", hb.t[:, :], xt.t[:, :], sh.t[:, :], ALU.add, [xt.b, sh.b], [hb.b])
        if defer:
            return hb
        rmsnorm_T2(hb, dstT, dst_cols)

    def rmsnorm_T2(hb, dstT, dst_cols, wb=None):
        ps = PS.next()
        pv = bfview(ps)
        for kc in range(8):
            tr(pv[:, kc * 128:(kc + 1) * 128], hb.t[:, kc * 128:(kc + 1) * 128], ident_b, [hb.b, cbt.b], [ps.b])
        cp("act", dstT.t[:, :, dst_cols], pv.rearrange("p (a b) -> p a b", a=8), [ps.b],
           [wb if wb is not None else dstT.b])

    def cut(name):
        if stage == name and not S.dead:
            S.barrier()
            dma("sp", out[0:128, 0:768], cf.t[:, :], [cf.b], [B_out[0]], "dbgc")
            S.dead = True

    pending = [None]

    def back(ctx):
        acol, ku, vg = ctx["acol"], ctx["ku"], ctx["vg"]
        pu = PU.next()
        for h in range(4):
            g, hh = h // 2, h % 2
            mm(pu.t[hh * 64:(hh + 1) * 64, g * 128:(g + 1) * 128], ku.t[:, h * 64:(h + 1) * 64],
               vg.t[:, h * 128:(h + 1) * 128], True, True, [ku.b, vg.b], [pu.b])
        if ctx["kind"] == "own":
            attm, qd, rg, m0, ti = ctx["attm"], ctx["qd"], ctx["rg"], ctx["m0"], ctx["ti"]
            po = PS.next()
            for h in range(4):
                g, hh = h // 2, h % 2
                mm(po.t[:, h * 128:(h + 1) * 128], attm.t[:, h, :], vg.t[:, h * 128:(h + 1) * 128],
                   True, False, [attm.b, vg.b], [po.b])
                mm(po.t[:, h * 128:(h + 1) * 128], qd.t[hh * 64:(hh + 1) * 64, g, :],
                   Sb.t[hh * 64:(hh + 1) * 64, g, :], False, True, [qd.b, Sb.b], [po.b])
            ssg = ssg_ring.next()
            memset("pool", ssg.t[:, :], 0.0, [ssg.b])
            for h in range(4):
                act(junk_s.t[:, :], po.t[:, h * 128:(h + 1) * 128], AF.Square, [po.b], [junk_s.b, ssg.b],
                    accum=ssg.t[:, h:h + 1])
            rstd_(ssg.t[:, :], ssg.t[:, :], 1.0 / 128.0, [ssg.b])
            on = on_ring.next()
            tt("dve", on.t[:, :].rearrange("p (a b) -> p a b", a=4), po.t[:, :].rearrange("p (a b) -> p a b", a=4),
               ssg.t[:, :].unsqueeze(2).to_broadcast([128, 4, 128]), ALU.mult, [po.b, ssg.b], [on.b])
            og = og_ring.next()
            tt("pool", og.t[:, :], on.t[:, :], rg.t[:, :], ALU.mult, [on.b, rg.b], [og.b])
            pt2 = PS.next()
            pv2 = bfview(pt2)
            for h in range(4):
                tr(pv2[:, h * 128:(h + 1) * 128], og.t[:, h * 128:(h + 1) * 128], ident_b, [og.b, cbt.b], [pt2.b])
            ogT = ogT_ring.next()
            cp("act", ogT.t[:, :, :], pv2[:, 0:512].rearrange("p (a b) -> p a b", a=4), [pt2.b], [ogT.b])
            dma("sp", ogscr[:, :, m0 + ti * 128:m0 + (ti + 1) * 128].rearrange("h e t -> e h t"), ogT.t[:, :, :],
                [ogT.b], [B_ogscr], ogT.name, partial=True)
        for g in range(2):
            stt("dve", Sf.t[:, g, :], Sf.t[:, g, :], acol.t[:, g:g + 1], pu.t[:, g * 128:(g + 1) * 128],
                ALU.mult, ALU.add, [Sf.b, acol.b, pu.b], [Sf.b])
        cp("pool", Sb.t[:, :, :], Sf.t[:, :, :], [Sf.b], [Sb.b])

    hTbs = {}

    pro_pending = [None]

    def prologue_flush():
        if pro_pending[0] is not None:
            hb_, tb2, ti2 = pro_pending[0]
            rmsnorm_T2(hb_, hTbs[tb2], slice(ti2 * 128, (ti2 + 1) * 128))
            pro_pending[0] = None

    def prologue_tile(tb_, ti_, immediate=False):
        t_ = tb_ * 4 + ti_
        xt = rn["x_ring"].next()
        dma("sp", xt.t[:, :], xe[t_ * 128:(t_ + 1) * 128, :], [], [xt.b], xt.name)
        hb_ = rmsnorm_mod_T(xt, gmod1, shift1, None, None, defer=True)
        prologue_flush()
        pro_pending[0] = (hb_, tb_, ti_)
        if immediate:
            prologue_flush()

    full512 = slice(0, 512)
    PU = Ring(psb[6:8])
    PS.items = psb[0:6]
    for tb in range(16):
        kind = "prefix" if tb < 8 else ("halo" if tb < 12 else "own")
        S.bmarks = getattr(S, "bmarks", []) + [len(S.streams["pe"])]
        if tb == 0:
            hTbs[0] = hT_ring.next()
            for ti in range(4):
                prologue_tile(0, ti, immediate=(ti == 3))
        hTb = hTbs[tb]
        if tb + 1 < 16:
            hTbs[tb + 1] = hT_ring.next()
        cut("c_norm")
        n0 = (tb - 8) * 512
        m0 = (tb - 12) * 512
        if kind != "prefix":
            csr = cs_ring.next()
            dma("sp", csr.t[:, :, :], cs_tab[:, :, n0:n0 + 512].rearrange("a p n -> p a n"), [], [csr.b], csr.name)
            jobs = [(512, kT, n0)]
            if kind == "own":
                jobs.append((0, qT, m0))
            for (cbase, dst, d0) in jobs:
                for hp in range(4):
                    pa = PS.next()
                    proj_fm(cbase + hp * 128, 128, hTb, full512, pa, full512)
                    qs = qs_ring.next()
                    cp("act", qs.t[:, :], pa.t[:, :], [pa.b], [qs.b])
                    pb_ = PS.next()
                    mm(pb_.t[:, :], Psw_f, qs.t[:, :], True, True, [cf.b, qs.b], [pb_.b])
                    r1 = rp_ring.next()
                    r2 = rp_ring.next()
                    tt("pool", r1.t[:, :], qs.t[:, :], csr.t[:, 0, :], ALU.mult, [qs.b, csr.b], [r1.b])
                    tt("dve", r2.t[:, :], pb_.t[:, :], csr.t[:, 1, :], ALU.mult, [pb_.b, csr.b], [r2.b])
                    tt("pool", dst.t[:, hp, d0:d0 + 512], r1.t[:, :], r2.t[:, :], ALU.add, [r1.b, r2.b], [dst.b])
        if tb == 8:
            cut("c_rope")
        glr = glr_ring.next()
        pg = PS.next()
        proj_fm(3072, 16, hTb, full512, pg, full512)
        cp("act", glr.t[0:16, :], pg.t[0:16, :], [pg.b], [glr.b])
        for ti in range(4):
            t = tb * 4 + ti
            tsl = slice(ti * 128, (ti + 1) * 128)
            if kind != "prefix":
                pvv = PS.next()
                proj_tm(ti, 1024, 512, hTb, pvv)
                vt = vt_ring.next()
                cp("act", vt.t[:, :], pvv.t[:, :], [pvv.b], [vt.b])
                dma("sp", vscr[n0 + ti * 128:n0 + (ti + 1) * 128, :], vt.t[:, :], [vt.b], [B_vscr], vt.name,
                    partial=True)
            pgl = PS.next()
            mm(pgl.t[:, 0:256], glr.t[0:17, tsl], wg17.t[0:17, :], True, True, [glr.b, wg17.b], [pgl.b])
            e1 = e1_ring.next()
            act(e1.t[:, :], pgl.t[:, 0:256], AF.Exp, [pgl.b], [e1.b], scale=-1.0)
            spt = sp_ring.next()
            act(spt.t[:, :], e1.t[:, :], AF.Ln, [e1.b], [spt.b], bias=1.0)
            pk = PS.next()
            proj_tm(ti, 1792, 256, hTb, pk)
            pvg = PS.next()
            proj_tm(ti, 2048, 512, hTb, pvg)
            vg = vg_ring.next()
            cp("act", vg.t[:, :], pvg.t[:, :], [pvg.b], [vg.b])
            paf = PS.next()
            mm(paf.t[:, 0:256], Ls_f, spt.t[:, :], True, True, [cf.b, spt.b], [paf.b])
            dec = dec_ring.next()
            act(dec.t[:, :], paf.t[:, 0:256], AF.Exp, [paf.b], [dec.b], scale=-1.0 / 16.0)
            ku = ku_ring.next()
            stt("dve", ku.t[:, :], pk.t[:, 0:256], tv.t[:, t:t + 1], dec.t[:, :], ALU.mult, ALU.mult,
                [pk.b, tv.b, dec.b], [ku.b])
            pa_ = PS.next()
            for g in range(2):
                mm(pa_.t[:, g:g + 1], spt.t[:, g * 128:(g + 1) * 128], ones_f[:, 0:1], True, True,
                   [spt.b, cf.b], [pa_.b])
            acol = acol_ring.next()
            act(acol.t[:, :], pa_.t[:, 0:2], AF.Exp, [pa_.b], [acol.b], scale=-1.0 / 16.0)
            if kind == "own":
                pcs = PS.next()
                for g in range(2):
                    mm(pcs.t[:, g * 128:(g + 1) * 128], spt.t[:, g * 128:(g + 1) * 128], U_f, True, True,
                       [spt.b, cf.b], [pcs.b])
                eb = eb_ring.next()
                enb = enb_ring.next()
                act(eb.t[:, :], pcs.t[:, 0:256], AF.Exp, [pcs.b], [eb.b], scale=-1.0 / 16.0)
                act(enb.t[:, :], pcs.t[:, 0:256], AF.Exp, [pcs.b], [enb.b], scale=1.0 / 16.0)
                pqk = PS.next()
                for g in range(2):
                    proj_fm(1536 + g * 128, 128, hTb, tsl, pqk, slice(g * 128, (g + 1) * 128))
                    proj_fm(1792 + g * 128, 128, hTb, tsl, pqk, slice(256 + g * 128, 256 + (g + 1) * 128))
                qd = qd_ring.next()
                kd = kd_ring.next()
                stt("dve", qd.t[:, :, :], pqk.t[:, 0:256].rearrange("p (a b) -> p a b", a=2), 0.125,
                    eb.t[:, :].rearrange("p (a b) -> p a b", a=2), ALU.mult, ALU.mult, [pqk.b, eb.b], [qd.b])
                tt("dve", kd.t[:, :, :], pqk.t[:, 256:512].rearrange("p (a b) -> p a b", a=2),
                   enb.t[:, :].rearrange("p (a b) -> p a b", a=2), ALU.mult, [pqk.b, enb.b], [kd.b])
                pr = PS.next()
                proj_tm(ti, 2560, 512, hTb, pr)
                er = er_ring.next()
                act(er.t[:, :], pr.t[:, :], AF.Exp, [pr.b], [er.b], scale=-1.0)
                rg = rg_ring.next()
                tt("dve", rg.t[:, :].rearrange("p (a b) -> p a b", a=4), pr.t[:, :].rearrange("p (a b) -> p a b", a=4),
                   gglab.t[:, :].unsqueeze(1).to_broadcast([128, 4, 128]), ALU.mult, [pr.b, gglab.b], [rg.b])
                ts("dve", er.t[:, :], er.t[:, :], 1.0, None, ALU.add, None, [er.b], [er.b])
                recip(er.t[:, :], er.t[:, :], [er.b], [er.b])
                tt("pool", rg.t[:, :], rg.t[:, :], er.t[:, :], ALU.mult, [rg.b, er.b], [rg.b])
                patt = [PS.next(), PS.next()]
                for h in range(4):
                    g, hh = h // 2, h % 2
                    mm(patt[hh].t[:, g * 128:(g + 1) * 128], kd.t[hh * 64:(hh + 1) * 64, g, :],
                       qd.t[hh * 64:(hh + 1) * 64, g, :], True, True, [kd.b, qd.b], [patt[hh].b])
                attm = attm_ring.next()
                for hh in range(2):
                    tt("dve", attm.t[:, hh:4:2, :], patt[hh].t[:, 0:256].rearrange("p (a b) -> p a b", a=2),
                       U_f.unsqueeze(1).to_broadcast([128, 2, 128]), ALU.mult, [patt[hh].b, cf.b], [attm.b])
            ctx = dict(kind=kind, acol=acol, ku=ku, vg=vg)
            if kind == "own":
                ctx.update(attm=attm, qd=qd, rg=rg, m0=m0, ti=ti)
            if pending[0] is not None:
                back(pending[0])
            pending[0] = ctx
            if tb + 1 < 16:
                prologue_tile(tb + 1, ti, immediate=(ti == 3))
    back(pending[0])
    PS.items = psb
    S.barrier()
    if stage == "p1":
        dbgt = sb(None, "dbgt", [128, 1024], F32)
        for i in range(4):
            cp("dve", dbgt.t[:, :], kT.t[:, i, 2048:3072], [kT.b], [dbgt.b])
            dma("sp", out[i * 128:(i + 1) * 128, :], dbgt.t[:, :], [dbgt.b], [B_out[i]], "dbg0")
        for i in range(4):
            cp("dve", dbgt.t[:, :], qT.t[:, i, 0:1024], [qT.b], [dbgt.b])
            dma("sp", out[(4 + i) * 128:(5 + i) * 128, :], dbgt.t[:, :], [dbgt.b], [B_out[4 + i]], "dbg0")
        S.dead = True
    A.free(p1)

    p23 = []
    catT = sb(p23, "catT", [128, 8, 2048], BF16)
    w_out_sb = sb(p23, "w_out", [128, 8, 1024], BF16)
    woutv = w_out.rearrange("(kc p) n -> p kc n", p=128)
    for kc in range(8):
        dma("pool", w_out_sb.t[:, kc, :], woutv[:, kc, :], [], [w_out_sb.b], "w_out", partial=True)
    for h in range(4):
        dma("sp", catT.t[:, 4 + h, :], ogscr[h], [B_ogscr], [catT.b], "catT_ld", partial=True)
    pa = []
    vp_ring = ring(pa, "vp", [128, 3, 32, 128], BF16, 2)
    acc = sb(pa, "acc", [128, 2, 2048], F32)
    nb_ring = ring(pa, "nb", [128, 2], F32, 4)
    prod = sb(pa, "prod", [128, 2048], BF16)
    P_ring = ring(pa, "P", [128, 512], BF16, 4)
    PT_ring = ring(pa, "PT", [128, 512], BF16, 4)
    rl = sb(pa, "rl", [128, 2048], F32)
    PATS = ((0, 1), (1, 4), (2, 16))
    vps = {}

    def load_vp(hp_):
        vp_ = vp_ring.next()
        for (pi, d) in PATS:
            src = vscr[:, hp_ * 128:(hp_ + 1) * 128].rearrange("(u d) c -> d u c", d=d)
            ntile = 32 // d
            for r in range(d):
                dma("sp", vp_.t[:, pi, r * ntile:(r + 1) * ntile, :],
                    src[r].rearrange("(kt p) c -> p kt c", p=128), [B_vscr], [vp_.b], vp_.name, partial=True)
        vps[hp_] = vp_

    def mk_prod(hp_):
        tt("pool", prod.t[:, :], qT.t[:, hp_, :], kT.t[:, hp_, 2048:4096], ALU.mult, [qT.b, kT.b], [prod.b])

    load_vp(0)
    load_vp(1)
    mk_prod(0)
    for hp in range(4):
        vp = vps[hp]
        if 1 <= hp < 3:
            load_vp(hp + 1)
        memset("pool", acc.t[:, :, :], 0.0, [acc.b])
        units = []
        for (pi, d) in PATS:
            ntile = 32 // d
            for r in range(d):
                for kt in range(16 // d, ntile):
                    units.append((pi, d, r, kt))

        def stageA(u):
            pi, d, r, kt = u
            ntile = 32 // d
            st_ = {}
            q0 = r + d * kt * 128 - 2048
            k0 = r + d * (kt - 1) * 128
            qs = slice(q0, q0 + d * 127 + 1, d)
            ks = slice(k0, k0 + d * 255 + 1, d)
            pS = [PS.next(), PS.next()]
            for hh in range(2):
                mm(pS[hh].t[:, 0:256], qT.t[hh * 64:(hh + 1) * 64, hp, qs],
                   kT.t[hh * 64:(hh + 1) * 64, hp, ks], True, True, [qT.b, kT.b], [pS[hh].b])
            mm(pS[0].t[:, 256:258], prod.t[:, qs], negsel_b, True, True, [prod.b, cbt.b], [pS[0].b])
            nb = nb_ring.next()
            cp("dve", nb.t[:, 0:2], pS[0].t[:, 256:258], [pS[0].b], [nb.b])
            Pt = P_ring.next()
            for hh in range(2):
                act(Pt.t[:, hh * 256:(hh + 1) * 256], pS[hh].t[:, 0:256], AF.Exp,
                    [pS[hh].b, nb.b], [Pt.b], bias=nb.t[:, hh:hh + 1], scale=0.125)
            st_.update(Pt=Pt, qs=qs, vt0=r * ntile + kt - 1, pi=pi, first=(kt == 16 // d))
            return st_

        def stageB(st_):
            Pt = st_["Pt"]
            pT = PS.next()
            pTv = bfview(pT)
            for i in range(4):
                tr(pTv[:, i * 128:(i + 1) * 128], Pt.t[:, i * 128:(i + 1) * 128], ident_b,
                   [Pt.b, cbt.b], [pT.b])
            PT = PT_ring.next()
            mk = mask4h if st_["first"] else mT
            tt("dve", PT.t[:, :], pTv[:, 0:512], mk.t[:, :], ALU.mult, [pT.b, mk.b], [PT.b])
            st_["PT"] = PT

        def stageC(st_):
            PT, qs, vt0, pi = st_["PT"], st_["qs"], st_["vt0"], st_["pi"]
            pO = PS.next()
            for hh in range(2):
                osl = pO.t[hh * 64:(hh + 1) * 64, 0:128]
                lsl = pO.t[hh * 64:(hh + 1) * 64, 128:256]
                for kk in range(2):
                    mm(osl, vp.t[:, pi, vt0 + kk, hh * 64:(hh + 1) * 64],
                       PT.t[:, (hh * 2 + kk) * 128:(hh * 2 + kk + 1) * 128], kk == 0, kk == 1,
                       [vp.b, PT.b], [pO.b])
                for kk in range(2):
                    mm(lsl, ones_b[:, 0:64], PT.t[:, (hh * 2 + kk) * 128:(hh * 2 + kk + 1) * 128],
                       kk == 0, kk == 1, [cbt.b, PT.b], [pO.b])
            tt("dve", acc.t[:, :, qs], acc.t[:, :, qs], pO.t[:, 0:256].rearrange("p (a b) -> p a b", a=2),
               ALU.add, [acc.b, pO.b], [acc.b])

        sts = []
        nu = len(units)
        for it in range(nu + 2):
            if it < nu:
                sts.append(stageA(units[it]))
            if it == nu - 1 and hp + 1 < 4:
                mk_prod(hp + 1)
            if 0 <= it - 1 < nu:
                stageB(sts[it - 1])
            if 0 <= it - 2 < nu:
                stageC(sts[it - 2])
        recip(rl.t[:, :], acc.t[:, 1, :], [acc.b], [rl.b])
        tt("pool", catT.t[:, hp, :], acc.t[:, 0, :], rl.t[:, :], ALU.mult, [acc.b, rl.b], [catT.b])
    S.barrier()
    if stage == "p2":
        dbgt = sb(None, "dbgt", [128, 1024], F32)
        for i in range(8):
            cp("dve", dbgt.t[:, :], catT.t[:, i, 0:1024], [catT.b], [dbgt.b])
            dma("sp", out[i * 128:(i + 1) * 128, :], dbgt.t[:, :], [dbgt.b], [B_out[i]], "dbg0")
        S.dead = True
    A.free(pa)
    A.free(mix)

    p34 = []
    if stage == "full":
        h2T = sb(p34, "h2T", [128, 8, 2048], BF16)
        G = sb(p34, "G", [128, 16, 32], F32)
        GT = sb(p34, "GT", [32, 2048], BF16)
        wgu_ring = Ring([sb(p34, "wgu%d" % i, [128, 8, 1024], BF16) for i in range(2)])
        wd_ring = Ring([sb(p34, "wd0", [128, 8, 1024], BF16)])
        wguv = w_gate_up.rearrange("e (kc p) n -> e p kc n", p=128)
        wdv = w_down.rearrange("e (kc p) n -> e p kc n", p=128)

        def load_gu(e_, hx):
            wg = wgu_ring.next()
            for kc in range(0, 8, 2):
                dma("pool", wg.t[:, kc:kc + 2, :], wguv[e_, :, kc:kc + 2, hx * 1024:(hx + 1) * 1024], [], [wg.b],
                    wg.name, partial=True)
            return wg

        def load_d(e_):
            wd = wd_ring.next()
            for kc in range(0, 8, 2):
                dma("pool", wd.t[:, kc:kc + 2, :], wdv[e_, :, kc:kc + 2, :], [], [wd.b], wd.name, partial=True)
            return wd

        cur0 = [load_gu(0, 0), load_gu(0, 1), load_d(0)]
        rw = sb(p23, "rw", [128, 8, 32], BF16)
        rbb = sb(p23, "rbb", [128, 32], F32)
        dma("pool", rw.t[:, :, :], router_w.rearrange("(kc p) n -> p kc n", p=128), [], [rw.b], "rw")
        dma("sp", rbb.t[:, :], router_b.partition_broadcast(128), [], [rbb.b], "rbb")
        lg_ring = ring(p23, "lg", [128, 32], F32, 2)
        m8_ring = ring(p23, "m8", [128, 8], F32, 2)
        ex_ring = ring(p23, "ex", [128, 32], F32, 2)
        msk_ring = ring(p23, "msk", [128, 32], F32, 2)
        den_ring = ring(p23, "den", [128, 2], F32, 2)
        gtb_ring = ring(p23, "gtb", [128, 32], BF16, 2)
    mods = [sb(p23, "mods%d" % i, [128, 1024], F32) for i in range(3)]
    for i in range(3):
        dma("sp", mods[i].t[:, :], modscr[i], [B_modscr], [mods[i].b], "mods%d" % i)
    gg1, shift2, gmod2 = mods
    rn["x_ring"] = ring(p23, "xt3", [128, 1024], F32, 2)
    rn["junk"] = sb(p23, "junk_a3", [128, 1024], BF16)
    rn["ss_ring"] = ring(p23, "ss3", [128, 2], F32, 4)
    rn["hb_ring"] = ring(p23, "hb3", [128, 1024], BF16, 2)
    yt_ring = ring(p23, "yt", [128, 1024], F32, 2)
    x1_ring = ring(p23, "x1", [128, 1024], F32, 3)
    junk3 = rn["junk"]

    st3 = {}
    h2Tb = [Buf("h2T%d" % i) for i in range(16)]

    def p3A(ti):
        tsl = slice(ti * 128, (ti + 1) * 128)
        py = [PS.next(), PS.next()]
        for half in range(2):
            for kc in range(8):
                mm(py[half].t[:, :], catT.t[:, kc, tsl], w_out_sb.t[:, kc, half * 512:(half + 1) * 512],
                   kc == 0, kc == 7, [catT.b, w_out_sb.b], [py[half].b])
        ss = rn["ss_ring"].next()
        memset("pool", ss.t[:, :], 0.0, [ss.b])
        for half in range(2):
            act(junk3.t[:, 0:512], py[half].t[:, :], AF.Square, [py[half].b], [junk3.b, ss.b],
                accum=ss.t[:, half:half + 1])
        tt("dve", ss.t[:, 0:1], ss.t[:, 0:1], ss.t[:, 1:2], ALU.add, [ss.b], [ss.b])
        rstd_(ss.t[:, 0:1], ss.t[:, 0:1], 1.0 / 1024.0, [ss.b])
        yt = yt_ring.next()
        for half in range(2):
            hs = slice(half * 512, (half + 1) * 512)
            stt("dve", yt.t[:, hs], py[half].t[:, :], ss.t[:, 0:1], gg1.t[:, hs], ALU.mult, ALU.mult,
                [py[half].b, ss.b, gg1.b], [yt.b])
        xt = rn["x_ring"].next()
        dma("sp", xt.t[:, :], xe[(48 + ti) * 128:(49 + ti) * 128, :], [], [xt.b], xt.name)
        x1 = x1_ring.next()
        tt("pool", x1.t[:, :], xt.t[:, :], yt.t[:, :], ALU.add, [xt.b, yt.b], [x1.b])
        dma("sp", out[tsl, :], x1.t[:, :], [x1.b], [B_out[ti]], "out_w%d" % (ti % 4))
        st3[ti] = dict(x1=x1)

    def p3B1(ti):
        st3[ti]["hb"] = rmsnorm_mod_T(st3[ti]["x1"], gmod2, shift2, None, None, defer=True)

    def p3B2(ti):
        tsl = slice(ti * 128, (ti + 1) * 128)
        rmsnorm_T2(st3[ti]["hb"], h2T, tsl, wb=h2Tb[ti])

    def p3C(ti):
        tsl = slice(ti * 128, (ti + 1) * 128)
        pl = PS.next()
        for kc in range(8):
            mm(pl.t[:, 0:32], h2T.t[:, kc, tsl], rw.t[:, kc, :], kc == 0, kc == 7, [h2Tb[ti], rw.b], [pl.b])
        lg = lg_ring.next()
        tt("dve", lg.t[:, :], pl.t[:, 0:32], rbb.t[:, :], ALU.add, [pl.b, rbb.b], [lg.b])
        m8 = m8_ring.next()
        S.add("dve", (lambda m8=m8, lg=lg: (lambda e: e.max(out=m8.t[:, :], in_=lg.t[:, :])))(),
              reads=[lg.b], writes=[m8.b])
        msk = msk_ring.next()
        ts("dve", msk.t[:, :], lg.t[:, :], m8.t[:, 3:4], None, ALU.is_ge, None, [lg.b, m8.b], [msk.b])
        den = den_ring.next()
        memset("pool", den.t[:, :], 0.0, [den.b])
        ts("dve", den.t[:, 0:1], m8.t[:, 0:1], -1.0, None, ALU.mult, None, [m8.b], [den.b])
        ex = ex_ring.next()
        act(ex.t[:, :], lg.t[:, :], AF.Exp, [lg.b, den.b], [ex.b], bias=den.t[:, 0:1], scale=1.0)
        tt("dve", ex.t[:, :], ex.t[:, :], msk.t[:, :], ALU.mult, [ex.b, msk.b], [ex.b])
        S.add("dve", (lambda ex=ex, den=den: (lambda e: e.reduce_sum(out=den.t[:, 1:2], in_=ex.t[:, :],
                                                                   axis=mybir.AxisListType.X)))(),
              reads=[ex.b], writes=[den.b])
        recip(den.t[:, 1:2], den.t[:, 1:2], [den.b], [den.b])
        ts("dve", G.t[:, ti, :], ex.t[:, :], den.t[:, 1:2], None, ALU.mult, None, [ex.b, den.b], [G.b])
        gtb = gtb_ring.next()
        cp("pool", gtb.t[:, :], G.t[:, ti, :], [G.b], [gtb.b])
        st3[ti]["gtb"] = gtb

    def p3D(ti):
        tsl = slice(ti * 128, (ti + 1) * 128)
        gtb = st3[ti]["gtb"]
        pgt = PS.next()
        pgv = bfview(pgt)
        tr(pgv[0:32, 0:128], gtb.t[:, :], ident_b, [gtb.b, cbt.b], [pgt.b])
        cp("act", GT.t[:, tsl], pgv[0:32, 0:128], [pgt.b], [GT.b])

    for it in range(16 + 5):
        if stage == "full":
            if 0 <= it - 2 < 16:
                p3B1(it - 2)
            if 0 <= it - 3 < 16:
                p3B2(it - 3)
            if 0 <= it - 4 < 16:
                p3C(it - 4)
            if 0 <= it - 5 < 16:
                p3D(it - 5)
        if it < 16:
            p3A(it)
    S.barrier()
    A.free(p23)

    if stage == "full":
        p4 = []
        gg2b = sb(p4, "gg2b", [128, 1024], F32)
        dma("sp", gg2b.t[:, :], modscr[3], [B_modscr], [gg2b.b], "gg2b")
        bdn = sb(p4, "bdn", [32, 1024], BF16)
        dma("pool", bdn.t[:, :], b_down, [], [bdn.b], "bdn")
        bgu = sb(p4, "bgu", [128, 16, 32], F32)
        pb = []
        bgs = sb(pb, "bgs", [32, 2048], F32)
        dma("sp", bgs.t[:, :], b_gate_up, [], [bgs.b], "bgs")
        bview = bgs.t[:, :].rearrange("e (fb p s) -> e fb s p", fb=8, s=2)
        for fb in range(8):
            for s_ in range(2):
                pbt = PS.next()
                tr(pbt.t[:, 0:32], bview[:, fb, s_, :], ident_f[0:32, 0:32], [bgs.b, cf.b], [pbt.b])
                cp("act", bgu.t[:, fb * 2 + s_, :], pbt.t[:, 0:32], [pbt.b], [bgu.b])
        bl7 = sb(p4, "bl7", [128, 8, 32], F32)
        ts("dve", bl7.t[:, :, :], bgu.t[:, 1:16:2, :], -1.0, 7.0, ALU.mult, ALU.add, [bgu.b], [bl7.b])
        S.barrier()
        A.free(pb)
        accm = sb(p4, "accm", [128, 8, 1024], F32)
        accb = [Buf("accm%d" % i) for i in range(8)]
        wgu_ring.items = wgu_ring.items + [sb(p4, "wgu2", [128, 8, 1024], BF16)]
        wd_ring.items = wd_ring.items + [sb(p4, "wd1", [128, 8, 1024], BF16)]
        actT_ring = ring(p4, "actT", [128, 8, 512], BF16, 2)
        g_ring = ring(p4, "g_", [128, 512], F32, 2)
        sg_ring = ring(p4, "sg_", [128, 512], F32, 2)
        l_ring = ring(p4, "l_", [128, 512], F32, 2)
        x1b_ring = ring(p4, "x1b", [128, 1024], F32, 2)
        ss4_ring = ring(p4, "ss4", [128, 2], F32, 4)
        junk4 = sb(p4, "junk4", [128, 1024], BF16)
        seq = [(hf_, e_) for hf_ in range(2) for e_ in range(32)]
        NSK = 2

        def gu_fb(e_, tok0, fb, wgA, wgB, aT):
            wg = wgA if fb < 4 else wgB
            fl = fb % 4
            pG = PS.next()
            pL = PS.next()
            for kc in range(8):
                mm(pG.t[:, :], wg.t[:, kc, fl * 256:(fl + 1) * 256:2], h2T.t[:, kc, tok0:tok0 + 512],
                   kc == 0, kc == 7, [wg.b, h2T.b], [pG.b])
            for kc in range(8):
                mm(pL.t[:, :], wg.t[:, kc, fl * 256 + 1:(fl + 1) * 256:2], h2T.t[:, kc, tok0:tok0 + 512],
                   kc == 0, kc == 7, [wg.b, h2T.b], [pL.b])
            g_ = g_ring.next()
            ts("dve", g_.t[:, :], pG.t[:, :], bgu.t[:, fb * 2, e_:e_ + 1], 7.0, ALU.add, ALU.min,
               [pG.b, bgu.b], [g_.b])
            sg = sg_ring.next()
            act(sg.t[:, :], g_.t[:, :], AF.Sigmoid, [g_.b], [sg.b], scale=1.702)
            l_ = l_ring.next()
            act(l_.t[:, :], pL.t[:, :], AF.Relu, [pL.b, bl7.b], [l_.b], bias=bl7.t[:, fb, e_:e_ + 1], scale=-1.0)
            act(l_.t[:, :], l_.t[:, :], AF.Relu, [l_.b, c14.b], [l_.b], bias=c14.t[:, 0:1], scale=-1.0)
            tt("dve", g_.t[:, :], g_.t[:, :], sg.t[:, :], ALU.mult, [g_.b, sg.b], [g_.b])
            stt("dve", aT.t[:, fb, :], l_.t[:, :], -6.0, g_.t[:, :], ALU.add, ALU.mult, [g_.b, l_.b], [aT.b])

        def down(hf_, e_, tb, aT, wd):
            for tl in range(4):
                tg = hf_ * 8 + tb * 4 + tl
                ta = tb * 4 + tl
                py = [PS.next(), PS.next()]
                for half in range(2):
                    for fb in range(8):
                        mm(py[half].t[:, :], aT.t[:, fb, tl * 128:(tl + 1) * 128],
                           wd.t[:, fb, half * 512:(half + 1) * 512], fb == 0, fb == 7, [aT.b, wd.b], [py[half].b])
                for half in range(2):
                    hs = slice(half * 512, (half + 1) * 512)
                    if e_ == 0:
                        S.add("dve", (lambda o=accm.t[:, ta, hs], a=py[half].t[:, :], g=G.t[:, tg, e_:e_ + 1]:
                                      (lambda e: e.tensor_scalar(out=o, in0=a, scalar1=g, scalar2=None, op0=ALU.mult)))(),
                              reads=[py[half].b, G.b], writes=[accb[ta]], partial=(half == 1))
                    else:
                        stt("dve", accm.t[:, ta, hs], py[half].t[:, :], G.t[:, tg, e_:e_ + 1], accm.t[:, ta, hs],
                            ALU.mult, ALU.add, [py[half].b, G.b, accb[ta]], [accb[ta]])

        def finalize(hf_, tas):
            sst = {}
            for ta in tas:
                tg = hf_ * 8 + ta
                tsl = slice(tg * 128, (tg + 1) * 128)
                pb2 = [PS.next(), PS.next()]
                ss = ss4_ring.next()
                memset("pool", ss.t[:, :], 0.0, [ss.b])
                for half in range(2):
                    hs = slice(half * 512, (half + 1) * 512)
                    mm(pb2[half].t[:, :], GT.t[:, tsl], bdn.t[:, hs], True, True, [GT.b, bdn.b], [pb2[half].b])
                    tt("dve", accm.t[:, ta, hs], accm.t[:, ta, hs], pb2[half].t[:, :], ALU.add,
                       [accb[ta], pb2[half].b], [accb[ta]])
                act(junk4.t[:, :], accm.t[:, ta, :], AF.Square, [accb[ta]], [junk4.b, ss.b], accum=ss.t[:, 0:1])
                sst[ta] = ss
            for ta in tas:
                ss = sst[ta]
                rstd_(ss.t[:, 0:1], ss.t[:, 0:1], 1.0 / 1024.0, [ss.b])
            for ta in tas:
                tg = hf_ * 8 + ta
                tsl = slice(tg * 128, (tg + 1) * 128)
                ss = sst[ta]
                stt("dve", accm.t[:, ta, :], accm.t[:, ta, :], ss.t[:, 0:1], gg2b.t[:, :], ALU.mult, ALU.mult,
                    [accb[ta], ss.b, gg2b.b], [accb[ta]])
                x1b = x1b_ring.next()
                dma("sp", x1b.t[:, :], out[tsl, :], [B_out[tg]], [x1b.b], x1b.name)
                tt("pool", accm.t[:, ta, :], accm.t[:, ta, :], x1b.t[:, :], ALU.add, [accb[ta], x1b.b], [accb[ta]])
                dma("sp", out[tsl, :], accm.t[:, ta, :], [accb[ta]], [B_out[tg]], "out_f")

        cur = cur0
        pre = None
        for si, (hf_, e_) in enumerate(seq):
            wgA, wgB, wd = cur
            have_next = si + 1 < len(seq)
            ne = seq[si + 1][1] if have_next else None
            nxtA = load_gu(ne, 0) if have_next else None
            nxtB = None
            nxtD = load_d(ne) if have_next else None
            for tb in range(2):
                tok0 = hf_ * 1024 + tb * 512
                if pre is None:
                    aT = actT_ring.next()
                    fb0 = 0
                else:
                    aT = pre
                    fb0 = NSK
                for fb in range(fb0, 8):
                    gu_fb(e_, tok0, fb, wgA, wgB, aT)
                if tb == 1 and have_next:
                    nxtB = load_gu(ne, 1)
                pre = None
                if tb == 0:
                    pre = actT_ring.next()
                    for fb in range(NSK):
                        gu_fb(e_, hf_ * 1024 + 512, fb, wgA, wgB, pre)
                elif have_next:
                    pre = actT_ring.next()
                    for fb in range(NSK):
                        gu_fb(ne, seq[si + 1][0] * 1024, fb, nxtA, None, pre)
                down(hf_, e_, tb, aT, wd)
                if e_ == 31:
                    finalize(hf_, range(tb * 4, tb * 4 + 4))
            cur = [nxtA, nxtB, nxtD]
        A.free(p4)
    A.free(p34)

    S.dead = False
    S.add("sp", lambda e: e.nop(), reads=B_out)

    S.finalize()
    sems = {e: es.enter_context(nc.semaphore("s_" + e)) for e in ENGS}
    dsems = {}
    for i, k in enumerate(sorted(S.dma_cum.keys())):
        dsems[k] = es.enter_context(nc.semaphore("d%d" % i))
    block = es.enter_context(nc.Block())
    S.emit(block, sems, dsems)
    es.close()
    build.info = dict(bmarks=getattr(S, "bmarks", []), marks=[m["pe"] for m in S.marks], peak=A.peak, nops=len(S.ops), ndsem=len(dsems),
                      per_eng={e: len(S.streams[e]) for e in ENGS},
                      flagged={e: sum(1 for o in S.streams[e] if o.flag) for e in ENGS})
    return nc, S


def make_in_maps(inputs, stage="full"):
    x = np.asarray(inputs["x"], np.float32)
    c = np.asarray(inputs["c"], np.float32)
    cf, cb = _consts()
    shared = {
        "cst_f32": cf, "cst_bf": cb,
        "w_mod": np.ascontiguousarray(inputs["w_mod"][0], np.float32),
        "b_mod": np.ascontiguousarray(inputs["b_mod"][0], np.float32),
        "g_pre_mix": np.ascontiguousarray(inputs["g_pre_mix"][0], np.float32),
        "g_post_mix": np.ascontiguousarray(inputs["g_post_mix"][0], np.float32),
        "g_pre_ffn": np.ascontiguousarray(inputs["g_pre_ffn"][0], np.float32),
        "g_post_ffn": np.ascontiguousarray(inputs["g_post_ffn"][0], np.float32),
        "w_in": np.ascontiguousarray(inputs["w_in"][0], np.float32),
        "w_gate_lr": np.ascontiguousarray(inputs["w_gate_lr"][0], np.float32),
        "b_gate": np.ascontiguousarray(inputs["b_gate"][0], np.float32),
        "g_gla": np.ascontiguousarray(inputs["g_gla"][0], np.float32),
        "w_out": np.ascontiguousarray(inputs["w_out"][0], np.float32),
    }
    if stage == "full":
        shared.update({
            "router_w": np.ascontiguousarray(inputs["router_w"][0], np.float32),
            "router_b": np.ascontiguousarray(inputs["router_b"][0], np.float32),
            "w_gate_up": np.ascontiguousarray(inputs["w_gate_up"][0], np.float32),
            "b_gate_up": np.ascontiguousarray(inputs["b_gate_up"][0], np.float32),
            "w_down": np.ascontiguousarray(inputs["w_down"][0], np.float32),
            "b_down": np.ascontiguousarray(inputs["b_down"][0], np.float32),
        })
    maps = []
    for core in range(NCORES):
        b, j = core // 4, core % 4
        end = (j + 1) * 2048
        start = end - 8192
        xeh = np.zeros((8192, 1024), np.float32)
        lo = max(start, 0)
        xeh[lo - start:, :] = x[b, lo:end, :]
        tvh = np.zeros((128, 64), np.float32)
        for t in range(64):
            if start + t * 128 >= 0:
                tvh[:, t] = 1.0
        m = dict(shared)
        m["xe"] = xeh
        m["tilevalid"] = tvh
        m["cvec"] = np.ascontiguousarray(c[b].reshape(8, 128).T)
        m["cs_tab"] = _rope_tables(end)
        maps.append(m)
    return maps


_CACHE = {}


def kernel(**inputs):
    if "nc" not in _CACHE:
        _CACHE["nc"] = build("full")[0]
    nc = _CACHE["nc"]
    maps = make_in_maps(inputs, "full")
    res = run_bass_kernel_spmd(nc, maps, core_ids=list(range(NCORES)))
    outp = np.zeros((2, 8192, 1024), np.float32)
    for core in range(NCORES):
        b, j = core // 4, core % 4
        outp[b, j * 2048:(j + 1) * 2048, :] = np.asarray(res.results[core]["out"], np.float32)
    return outp
```
